# Optimizing a Trainium2 kernel written in Bass

```python
import functools
import jax, jax.numpy as jnp
from jax import lax
import numpy as np

D_MODEL = 2048
BATCH = 4
SEQ = 2048
DEPTH = 1
DEC_BATCH = 8
DEC_SEQ = 1
PAST_LEN = 16384
PAGE_SIZE = 128

RET_HEADS = 8
RET_QK_DIM = 128
RET_V_DIM = 256
RET_CHUNK = 128
ROPE_BASE = 10000.0
ATT_HEADS = 16
ATT_KV_HEADS = 4
HEAD_DIM = 128
GROUP = ATT_HEADS // ATT_KV_HEADS
IDX_HEADS = 16
IDX_DIM = 64
IDX_W_SCALE = (IDX_HEADS ** -0.5) * (IDX_DIM ** -0.5)
TOPK_MAX = 256
Q_BLOCK = 128
D_FF = 5632
EPS = 1e-6

RET_QK_W = RET_HEADS * RET_QK_DIM
RET_V_W = RET_HEADS * RET_V_DIM
ATT_Q_W = ATT_HEADS * HEAD_DIM
ATT_KV_W = ATT_KV_HEADS * HEAD_DIM
IDX_Q_W = IDX_HEADS * IDX_DIM
SPLITS = (RET_QK_W, RET_QK_W, RET_V_W, RET_V_W,
          ATT_Q_W, ATT_KV_W, ATT_KV_W,
          IDX_Q_W, IDX_DIM, IDX_HEADS,
          D_MODEL, D_MODEL)
IN_W = sum(SPLITS)

kernel_name = "retention_dsa_gated_macaron_step"


def rms_norm(x, g):
    xf = x.astype(jnp.float32)
    y = xf * lax.rsqrt(jnp.mean(xf * xf, axis=-1, keepdims=True) + EPS)
    return (y * g.astype(jnp.float32)).astype(x.dtype)


def swiglu(x, w1, w2):
    gate, up = jnp.split(x @ w1, 2, axis=-1)
    return (jax.nn.silu(gate) * up) @ w2


def split_cols(a):
    points, acc = [], 0
    for w in SPLITS[:-1]:
        acc += w
        points.append(acc)
    return jnp.split(a, points, axis=-1)


def rotary(x, pos):
    half = x.shape[-1] // 2
    inv = ROPE_BASE ** (-jnp.arange(half, dtype=jnp.float32) / half)
    ang = pos.astype(jnp.float32)[:, None] * inv[None, :]
    cos = jnp.cos(ang)[None, :, None, :]
    sin = jnp.sin(ang)[None, :, None, :]
    xf = x.astype(jnp.float32)
    x1, x2 = xf[..., :half], xf[..., half:]
    return jnp.concatenate([x1 * cos - x2 * sin, x1 * sin + x2 * cos], axis=-1).astype(x.dtype)


def retention_chunk(state, q, k, v, log_g):
    c = q.shape[1]
    i = jnp.arange(c, dtype=jnp.float32)
    rel = i[:, None] - i[None, :]
    causal = rel >= 0
    dmat = jnp.where(causal[None], jnp.exp(log_g[:, None, None] * jnp.where(causal, rel, 0.0)[None]), 0.0)
    scores = jnp.einsum('bihd,bjhd->bhij', q, k) * dmat[None]
    inner = jnp.einsum('bhij,bjhe->bihe', scores, v)
    q_dec = jnp.exp(log_g[None, :] * (i[:, None] + 1.0))
    cross = jnp.einsum('bihd,bhde->bihe', q * q_dec[None, :, :, None], state)
    k_dec = jnp.exp(log_g[None, :] * (c - 1.0 - i)[:, None])
    new_state = (jnp.exp(log_g * c)[None, :, None, None] * state
                 + jnp.einsum('bjhd,bjhe->bhde', k * k_dec[None, :, :, None], v))
    return new_state, inner + cross


def retention(q, k, v, g, state0, pos, chunk, gn, w_out):
    b, t = q.shape[:2]
    dt = q.dtype
    q = rotary(q.reshape(b, t, RET_HEADS, RET_QK_DIM), pos).astype(jnp.float32)
    k = rotary(k.reshape(b, t, RET_HEADS, RET_QK_DIM), pos).astype(jnp.float32) * (RET_QK_DIM ** -0.5)
    v = v.reshape(b, t, RET_HEADS, RET_V_DIM).astype(jnp.float32)
    log_g = jnp.log1p(-jnp.exp2(-5.0 - jnp.arange(RET_HEADS, dtype=jnp.float32)))
    nc = t // chunk
    blocks = lambda a: a.reshape(b, nc, chunk, *a.shape[2:]).swapaxes(0, 1)
    step = lambda s, xs: retention_chunk(s, xs[0], xs[1], xs[2], log_g)
    final, o = lax.scan(step, state0.astype(jnp.float32), (blocks(q), blocks(k), blocks(v)))
    o = o.swapaxes(0, 1).reshape(b, t, RET_HEADS, RET_V_DIM)
    o = rms_norm(o, gn).reshape(b, t, RET_V_W).astype(dt) * jax.nn.silu(g)
    return o @ w_out, final.astype(state0.dtype)


def indexer_select(qi, wi, ki, q_pos, n_keep):
    s = jax.nn.relu(jnp.einsum('bthd,bsd->bths', qi, ki).astype(jnp.float32))
    s = jnp.einsum('bths,bth->bts', s, wi.astype(jnp.float32))
    key_pos = jnp.arange(ki.shape[1])
    visible = key_pos[None, :] <= q_pos[:, None]
    s = jnp.where(visible[None], s, -jnp.inf)
    _, idx = lax.top_k(s, n_keep)
    valid = idx <= q_pos[None, :, None]
    return idx, valid


def attend_selected(q, k_sel, v_sel, valid):
    b, t = q.shape[:2]
    s = jnp.einsum('btkgd,btskd->btkgs', q, k_sel).astype(jnp.float32)
    s = jnp.where(valid[:, :, None, None, :], s, -jnp.inf)
    p = jax.nn.softmax(s, axis=-1).astype(v_sel.dtype)
    o = jnp.einsum('btkgs,btskd->btkgd', p, v_sel)
    return o.reshape(b, t, ATT_Q_W)


_take = jax.vmap(lambda a, i: a[i])


def prompt_sparse_attention(q, k, v, qi, wi, ki):
    b, t = q.shape[:2]
    nb = t // Q_BLOCK
    n_keep = min(TOPK_MAX, t // 4)
    to_blocks = lambda a: a.reshape(b, nb, Q_BLOCK, *a.shape[2:]).swapaxes(0, 1)

    def one_block(xs):
        qb, qib, wib, t0 = xs
        q_pos = t0 + jnp.arange(Q_BLOCK)
        idx, valid = indexer_select(qib, wib, ki, q_pos, n_keep)
        return attend_selected(qb, _take(k, idx), _take(v, idx), valid)

    out = lax.map(one_block, (to_blocks(q), to_blocks(qi), to_blocks(wi), jnp.arange(nb) * Q_BLOCK))
    return out.swapaxes(0, 1).reshape(b, t, ATT_Q_W)


def decode_sparse_attention(q, k, v, qi, wi, ki, pool_k, pool_v, pool_ki, page_table, layer):
    db, t = q.shape[:2]
    past = page_table.shape[1] * PAGE_SIZE
    past_ki = pool_ki[layer, page_table].reshape(db, past, IDX_DIM).astype(ki.dtype)
    ki_all = jnp.concatenate([past_ki, ki], axis=1)
    n_keep = min(TOPK_MAX, (past + t) // 4)
    q_pos = past + jnp.arange(t)
    idx, valid = indexer_select(qi, wi, ki_all, q_pos, n_keep)
    in_past = (idx < past)[..., None, None]
    pidx = jnp.minimum(idx, past - 1)
    phys = jax.vmap(lambda pt, i: pt[i])(page_table, pidx // PAGE_SIZE)
    off = pidx % PAGE_SIZE
    nidx = jnp.clip(idx - past, 0, t - 1)
    select = lambda pool, new: jnp.where(in_past, pool[layer, phys, off].astype(new.dtype), _take(new, nidx))
    return attend_selected(q, select(pool_k, k), select(pool_v, v), valid)


def layer_forward(x, pos, ret_state0, ret_chunk, sparse_attn, p):
    (ffn1_norm, ffn1_w1, ffn1_w2, mix_norm, w_in, q_norm, k_norm, ret_norm,
     w_ret_out, w_att_out, w_o, ffn2_norm, ffn2_w1, ffn2_w2) = p
    b, t, _ = x.shape
    x = x + 0.5 * swiglu(rms_norm(x, ffn1_norm), ffn1_w1, ffn1_w2)
    h = rms_norm(x, mix_norm)
    rq, rk, rv, rg, aq, ak, av, iq, ik, iw, g_ret, g_att = split_cols(h @ w_in)
    ret_out, ret_state = retention(rq, rk, rv, rg, ret_state0, pos, ret_chunk, ret_norm, w_ret_out)
    aq = rms_norm(aq.reshape(b, t, ATT_HEADS, HEAD_DIM), q_norm) * (HEAD_DIM ** -0.5)
    ak = rms_norm(ak.reshape(b, t, ATT_KV_HEADS, HEAD_DIM), k_norm)
    av = av.reshape(b, t, ATT_KV_HEADS, HEAD_DIM)
    iq = iq.reshape(b, t, IDX_HEADS, IDX_DIM)
    iw = iw * IDX_W_SCALE
    att = sparse_attn(aq.reshape(b, t, ATT_KV_HEADS, GROUP, HEAD_DIM), ak, av, iq, iw, ik)
    att_out = att @ w_att_out
    merged = jax.nn.sigmoid(g_ret) * ret_out + jax.nn.sigmoid(g_att) * att_out
    x = x + merged @ w_o
    x = x + 0.5 * swiglu(rms_norm(x, ffn2_norm), ffn2_w1, ffn2_w2)
    return x, ret_state, ak, av, ik


def setup_inputs(seed: int = 0) -> dict:
    key = jax.random.key(seed)
    ks = jax.random.split(key, 32)
    n_pages = PAST_LEN // PAGE_SIZE
    used = DEC_BATCH * n_pages
    n_phys = used + max(1, used // 4)
    nrm = lambda k, shape, scale: jax.random.normal(k, shape, jnp.float32) * scale
    gain = lambda k, n: 1.0 + 0.02 * jax.random.normal(k, (DEPTH, n), jnp.float32)
    page_table = jax.random.permutation(ks[0], n_phys)[:used].reshape(DEC_BATCH, n_pages).astype(jnp.int32)
    return {
        "x_prompt": nrm(ks[1], (BATCH, SEQ, D_MODEL), 1.0),
        "x_sample": nrm(ks[2], (DEC_BATCH, DEC_SEQ, D_MODEL), 1.0),
        "state_ret": nrm(ks[3], (DEPTH, DEC_BATCH, RET_HEADS, RET_QK_DIM, RET_V_DIM), 0.1),
        "cache_k": nrm(ks[4], (DEPTH, n_phys, PAGE_SIZE, ATT_KV_HEADS, HEAD_DIM), 1.0),
        "cache_v": nrm(ks[5], (DEPTH, n_phys, PAGE_SIZE, ATT_KV_HEADS, HEAD_DIM), 1.0),
        "cache_idx_k": nrm(ks[6], (DEPTH, n_phys, PAGE_SIZE, IDX_DIM), 1.0),
        "page_table": page_table,
        "ffn1_norm": gain(ks[7], D_MODEL),
        "ffn1_w1": nrm(ks[8], (DEPTH, D_MODEL, 2 * D_FF), D_MODEL ** -0.5),
        "ffn1_w2": nrm(ks[9], (DEPTH, D_FF, D_MODEL), D_FF ** -0.5),
        "mix_norm": gain(ks[10], D_MODEL),
        "w_in": nrm(ks[11], (DEPTH, D_MODEL, IN_W), D_MODEL ** -0.5),
        "q_norm": gain(ks[12], HEAD_DIM),
        "k_norm": gain(ks[13], HEAD_DIM),
        "ret_norm": gain(ks[14], RET_V_DIM),
        "w_ret_out": nrm(ks[15], (DEPTH, RET_V_W, D_MODEL), RET_V_W ** -0.5),
        "w_att_out": nrm(ks[16], (DEPTH, ATT_Q_W, D_MODEL), ATT_Q_W ** -0.5),
        "w_o": nrm(ks[17], (DEPTH, D_MODEL, D_MODEL), D_MODEL ** -0.5),
        "ffn2_norm": gain(ks[18], D_MODEL),
        "ffn2_w1": nrm(ks[19], (DEPTH, D_MODEL, 2 * D_FF), D_MODEL ** -0.5),
        "ffn2_w2": nrm(ks[20], (DEPTH, D_FF, D_MODEL), D_FF ** -0.5),
    }


def reference(x_prompt, x_sample, state_ret, cache_k, cache_v, cache_idx_k, page_table,
              ffn1_norm, ffn1_w1, ffn1_w2, mix_norm, w_in, q_norm, k_norm, ret_norm,
              w_ret_out, w_att_out, w_o, ffn2_norm, ffn2_w1, ffn2_w2):
    b, tp = x_prompt.shape[:2]
    ts = x_sample.shape[1]
    past = page_table.shape[1] * PAGE_SIZE
    pos_p = jnp.arange(tp)
    pos_s = past + jnp.arange(ts)
    weights = (ffn1_norm, ffn1_w1, ffn1_w2, mix_norm, w_in, q_norm, k_norm, ret_norm,
               w_ret_out, w_att_out, w_o, ffn2_norm, ffn2_w1, ffn2_w2)
    yp, ys = x_prompt, x_sample
    sp_l, kp_l, vp_l, ip_l, ss_l, ks_l, vs_l, is_l = [], [], [], [], [], [], [], []
    for l in range(DEPTH):
        p = tuple(w[l] for w in weights)
        zero_state = jnp.zeros((b, RET_HEADS, RET_QK_DIM, RET_V_DIM), x_prompt.dtype)
        yp, sp, kp, vp, ip = layer_forward(yp, pos_p, zero_state, min(RET_CHUNK, tp),
                                           prompt_sparse_attention, p)
        dec_attn = functools.partial(decode_sparse_attention, pool_k=cache_k, pool_v=cache_v,
                                     pool_ki=cache_idx_k, page_table=page_table, layer=l)
        ys, ss, ks_, vs, is_ = layer_forward(ys, pos_s, state_ret[l], ts, dec_attn, p)
        sp_l.append(sp); kp_l.append(kp); vp_l.append(vp); ip_l.append(ip)
        ss_l.append(ss); ks_l.append(ks_); vs_l.append(vs); is_l.append(is_)
    ret_state_prompt = jnp.stack(sp_l)
    k_prompt = jnp.stack(kp_l)
    v_prompt = jnp.stack(vp_l)
    idx_k_prompt = jnp.stack(ip_l)
    ret_state_sample = jnp.stack(ss_l)
    k_sample = jnp.stack(ks_l)
    v_sample = jnp.stack(vs_l)
    idx_k_sample = jnp.stack(is_l)
    return (yp, ys, ret_state_prompt, k_prompt, v_prompt, idx_k_prompt,
            ret_state_sample, k_sample, v_sample, idx_k_sample)
```

```python
import contextlib
import numpy as np
import concourse.bass as bass
import concourse.mybir as mybir
from concourse.bass_utils import run_bass_kernel_spmd

F32 = mybir.dt.float32
BF16 = mybir.dt.bfloat16
I32 = mybir.dt.int32
AF = mybir.ActivationFunctionType
ALU = mybir.AluOpType
AX = mybir.AxisListType

PE, ACT, DVE, POOL, SP = "pe", "act", "dve", "pool", "sp"


class Cfg:
    D = 2048
    DFF = 5632
    SEQ = 2048
    TB = 512
    NPAGES = 128
    NPHYS = 1280
    RH = 8
    EPS = 1e-6
    TOPK = 256
    NIT = 20
    WITH_SAMPLE = True
    NCORES = 8
    STAGE = 99


O_RQ, O_RK, O_RV, O_RG = 0, 1024, 2048, 4096
O_AQ, O_AK, O_AV = 6144, 8192, 8704
O_IQ, O_IK, O_IW = 9216, 10240, 10304
O_GR, O_GA = 10320, 12368
IN_W = 14416
IDX_W_SCALE = (16 ** -0.5) * (64 ** -0.5)
NEG = -30000.0
ARENA = 47104
GRAN = 512


class Sched:
    NDMA = 24

    def __init__(self):
        self.ops = {e: [] for e in (PE, ACT, DVE, POOL, SP)}
        self.epoch = 0
        self.count = {}
        self.waited = {e: {} for e in (PE, ACT, DVE, POOL, SP)}
        self.last_w = {}
        self.readers = {}
        self.dma_i = 0
        self.dma_cnt = [0] * self.NDMA
        self.semkeys = []
        self.keyfn = None

    def new_epoch(self):
        self.epoch += 1

    def _key(self, eng):
        k = (eng, self.epoch)
        if k not in self.count:
            self.count[k] = 0
            self.semkeys.append(k)
        return k

    def _norm(self, items):
        out = []
        for it in items:
            if isinstance(it, (str, tuple)):
                out.append(it)
            elif isinstance(it, list):
                out.extend(self._norm(it))
            else:
                out.extend(self.keyfn(it))
        return out

    def _deps(self, reads, writes):
        deps = {}
        for r in reads:
            if r in self.last_w:
                sk, v = self.last_w[r]
                deps[sk] = max(deps.get(sk, 0), v)
        for w in writes:
            if w in self.last_w:
                sk, v = self.last_w[w]
                deps[sk] = max(deps.get(sk, 0), v)
            for sk, v in self.readers.get(w, ()):
                deps[sk] = max(deps.get(sk, 0), v)
        return deps

    def _waits(self, eng, deps, skip=None):
        waits = []
        for sk, v in deps.items():
            if skip is not None and sk == skip:
                continue
            if self.waited[eng].get(sk, 0) >= v:
                continue
            self.waited[eng][sk] = v
            waits.append((sk, v))
        return waits

    def _commit(self, me, reads, writes):
        for r in reads:
            lst = self.readers.setdefault(r, [])
            lst[:] = [x for x in lst if x[0] != me[0]] + [me]
        for w in writes:
            self.last_w[w] = me
            self.readers[w] = []

    def op(self, eng, emit, reads=(), writes=()):
        reads, writes = self._norm(reads), self._norm(writes)
        sk = self._key(eng)
        deps = self._deps(reads, writes)
        waits = self._waits(eng, deps, skip=sk if eng == PE else None)
        self.count[sk] += 1
        me = (sk, self.count[sk])
        self.ops[eng].append((waits, emit, (sk, 1), self.count[sk]))
        self._commit(me, reads, writes)
        return me

    def dma(self, eng, emit, reads=(), writes=()):
        reads, writes = self._norm(reads), self._norm(writes)
        i = self.dma_i % self.NDMA
        self.dma_i += 1
        sk = ("dma", i)
        if sk not in self.semkeys:
            self.semkeys.append(sk)
        deps = self._deps(reads, writes)
        if self.dma_cnt[i] > 0:
            deps[sk] = max(deps.get(sk, 0), 16 * self.dma_cnt[i])
        waits = self._waits(eng, deps)
        self.dma_cnt[i] += 1
        me = (sk, 16 * self.dma_cnt[i])
        self.ops[eng].append((waits, emit, (sk, 16), None))
        self._commit(me, reads, writes)
        return me

    def finish(self, eng=SP):
        deps = {("dma", i): 16 * c for i, c in enumerate(self.dma_cnt) if c > 0}
        waits = self._waits(eng, deps)
        self.ops[eng].append((waits, None, None, None))

    def emit(self, nc):
        sems = {}
        needed = {}
        for eng_ops in self.ops.values():
            for waits, _, _, _ in eng_ops:
                for sk, v in waits:
                    if sk[0] != "dma":
                        needed.setdefault(sk, set()).add(v)
        rank = {sk: {v: i + 1 for i, v in enumerate(sorted(vs))} for sk, vs in needed.items()}
        with contextlib.ExitStack() as st:
            for i, sk in enumerate(self.semkeys):
                sems[sk] = st.enter_context(nc.semaphore("s%d" % i))
            block = st.enter_context(nc.Block())

            def run(eng_name):
                def f(e):
                    for waits, emit, inc, idx in self.ops[eng_name]:
                        for sk, v in waits:
                            e.wait_ge(sems[sk], v if sk[0] == "dma" else rank[sk][v])
                        if emit is None:
                            continue
                        ins = emit(e)
                        if idx is None:
                            ins.then_inc(sems[inc[0]], inc[1])
                        elif idx in needed.get(inc[0], ()):
                            ins.then_inc(sems[inc[0]], 1)
                return f

            block.tensor(run(PE))
            block.scalar(run(ACT))
            block.vector(run(DVE))
            block.gpsimd(run(POOL))
            block.sync(run(SP))


class Ring:
    def __init__(self, items):
        self.items = items
        self.i = 0

    def next(self):
        it = self.items[self.i % len(self.items)]
        self.i += 1
        return it


def build_program(cfg):
    D, DFF, SEQ, TB = cfg.D, cfg.DFF, cfg.SEQ, cfg.TB
    KT = D // 128
    KTF = DFF // 128
    NTB = TB // 128
    NKEY = SEQ
    NKB = NKEY // 128
    nc = bass.Bass("TRN2", target_bir_lowering=False)
    S = Sched()

    def din(name, shape, dt=F32):
        return nc.dram_tensor(name, list(shape), dt, kind="ExternalInput").ap()

    def dout(name, shape, dt=F32):
        return nc.dram_tensor(name, list(shape), dt, kind="ExternalOutput").ap()

    NPRE = SEQ // 2
    NOWN = SEQ // 2
    xo = din("xo", [NOWN, D])
    xpre = din("xpre", [NPRE, D])
    flag_d = din("flag", [128, 1])
    negflag_d = din("negflag", [128, 1])
    xs = din("xs", [1, D])
    st_s = din("st_s", [cfg.RH, 128, 256])
    if cfg.WITH_SAMPLE:
        cache_k = din("cache_k", [cfg.NPHYS * 128, 512])
        cache_v = din("cache_v", [cfg.NPHYS * 128, 512])
        cache_ik = din("cache_ik", [cfg.NPHYS * 4, 2048])
        ptab = din("ptab", [128, 1], I32)
    w_f1a = din("ffn1_w1", [D, 2 * DFF])
    w_f1b = din("ffn1_w2", [DFF, D])
    w_f2a = din("ffn2_w1", [D, 2 * DFF])
    w_f2b = din("ffn2_w2", [DFF, D])
    w_in = din("w_in", [D, IN_W])
    w_ro = din("w_ret_out", [D, D])
    w_ao = din("w_att_out", [D, D])
    w_o = din("w_o", [D, D])
    g_f1 = din("ffn1_norm", [1, D])
    g_mx = din("mix_norm", [1, D])
    g_f2 = din("ffn2_norm", [1, D])
    g_q = din("q_norm", [1, 128])
    g_k = din("k_norm", [1, 128])
    g_r = din("ret_norm", [1, 256])
    c_rope = din("c_rope", [SEQ + 1, 4, cfg.RH, 64])
    c_ident = din("c_ident", [128, 128])
    c_caus = din("c_caus", [128, 128])
    c_negu = din("c_negu", [128, 128])
    c_iota = din("c_iota", [128, 256])
    c_lstr = din("c_lstr", [128, 128])
    c_jcol = din("c_jcol", [128, 128])
    c_dslot = din("c_dslot", [128, 2])
    c_pow2 = din("c_pow2", [128, 32])

    y_p = dout("y_p", [NOWN, D])
    y_s = dout("y_s", [1, D])
    rs_p = dout("rs_p", [cfg.RH, 128, 256])
    k_p = dout("k_p", [NOWN, 512])
    v_p = dout("v_p", [NOWN, 512])
    ik_p = dout("ik_p", [NOWN, 64])
    rs_s = dout("rs_s", [cfg.RH, 128, 256])
    k_s = dout("k_s", [1, 512])
    v_s = dout("v_s", [1, 512])
    ik_s = dout("ik_s", [1, 64])
    sc_x = nc.dram_tensor("sc_x", [TB, D], F32, kind="Internal").ap()
    sc_gr = nc.dram_tensor("sc_gr", [TB, D], BF16, kind="Internal").ap()
    sc_ga = nc.dram_tensor("sc_ga", [TB, D], BF16, kind="Internal").ap()
    NSLOT = 128
    wcache = nc.dram_tensor("wcache", [NSLOT, 128, 8192], BF16, kind="Internal").ap()
    wslots = {}

    with contextlib.ExitStack() as ctx:
        def sb(name, shape, dt):
            return ctx.enter_context(nc.sbuf_tensor(name, list(shape), dt))

        def pt(name, shape, dt):
            return ctx.enter_context(nc.psum_tensor(name, list(shape), dt))

        arena = sb("arena", [128, ARENA], BF16)

        def keyfn(ap):
            t_ = getattr(ap, "tensor", None)
            name = t_.name if t_ is not None else ap.name
            if name != "arena":
                return [name]
            esz = 2 if ap.dtype == BF16 else 4
            rowlen = (ARENA * 2) // esz
            off = int(ap.offset) % rowlen
            ext = sum((n - 1) * abs(s) for s, n in list(ap.ap)[1:]) + 1
            lo = (off * esz) // (2 * GRAN)
            hi = ((off + ext) * esz - 1) // (2 * GRAN)
            return [("A", g) for g in range(lo, hi + 1)]

        S.keyfn = keyfn

        def AV(off, n, dt=BF16):
            assert off % 2 == 0 and off + n <= ARENA, (off, n)
            v = arena[:, off:off + n]
            return v if dt == BF16 else v.bitcast(dt)

        gain_bc = sb("gain_bc", [128, D], F32)
        ident_f = sb("ident_f", [128, 128], F32)
        ident_b = sb("ident_b", [128, 128], BF16)
        ones_b = sb("ones_b", [128, 128], BF16)
        caus = sb("caus", [128, 128], F32)
        negu = sb("negu", [128, 128], F32)
        gq_bc = sb("gq_bc", [128, 128], F32)
        gk_bc = sb("gk_bc", [128, 128], F32)
        gr_bc = sb("gr_bc", [128, 256], F32)
        kT = sb("kT", [128, 4, NKEY], BF16)
        Vc = sb("Vc", [128, NKB, 512], BF16)
        ikT2 = sb("ikT2", [128, NKEY], BF16)
        state_f = sb("state_f", [128, cfg.RH, 256], F32)
        state_b = sb("state_b", [128, cfg.RH, 256], BF16)
        wsc = sb("wsc", [128, NTB, 16], F32)
        thr = sb("thr", [128, 8], F32)
        pow2 = sb("pow2", [128, 32], F32)
        w_all = sb("w_all", [128, 32], F32)
        wring = Ring([sb("wb%d" % i, [128, 8192], BF16) for i in range(2)])
        xn_ring = Ring([sb("xn%d" % i, [128, D], BF16) for i in range(1)])
        f32s_ring = Ring([sb("fs%d" % i, [128, 512], F32) for i in range(3)])
        bfs_ring = Ring([sb("bs%d" % i, [128, 512], BF16) for i in range(3)])
        stat = sb("stat", [128, 64], F32)
        stat_i = [0]

        psL = Ring([pt("psL%d" % i, [128, 512], F32) for i in range(4)])
        psT = Ring([pt("psT%d" % i, [128, 1024], BF16) for i in range(2)])
        psM = [pt("psM%d" % i, [128, 512], F32) for i in range(2)]

        def stat_cols(n):
            if stat_i[0] + n > 64:
                stat_i[0] = 0
            a = stat_i[0]
            stat_i[0] += n
            return stat[:, a:a + n], ["stat%d" % i for i in range(a // 8, (a + n - 1) // 8 + 1)]

        deferred = []

        def flush_deferred(keep=0):
            while len(deferred) > keep:
                deferred.pop(0)()

        def cload(dst, src, eng=SP):
            S.dma(eng, lambda e: e.dma_start(out=dst, in_=src), writes=[dst])

        flagt = sb("flagt", [128, 16], F32)
        flag_sb = flagt[:, 0:1]
        negflag_sb = flagt[:, 8:9]
        cload(flag_sb, flag_d)
        cload(negflag_sb, negflag_d)
        cload(ident_f[:, :], c_ident)
        cload(pow2[:, :], c_pow2)
        cload(caus[:, :], c_caus)
        cload(negu[:, :], c_negu)
        cload(gk_bc[:, :], g_k.partition_broadcast(128))
        cload(gq_bc[:, :], g_q.partition_broadcast(128))
        cload(gr_bc[:, :], g_r.partition_broadcast(128))
        S.op(DVE, lambda e: e.tensor_copy(out=ident_b[:, :], in_=ident_f[:, :]), reads=[ident_f], writes=[ident_b])
        S.op(DVE, lambda e: e.memset(ones_b[:, :], 1.0), writes=[ones_b])
        S.op(DVE, lambda e: e.tensor_scalar(out=gq_bc[:, :], in0=gq_bc[:, :], scalar1=128 ** -0.5, scalar2=None,
                                            op0=ALU.mult), reads=[gq_bc], writes=[gq_bc])
        SFK = ["state_f%d" % h for h in range(cfg.RH)]
        SBK = ["state_b%d" % h for h in range(cfg.RH)]
        S.op(DVE, lambda e: e.memset(state_f[:, :, :], 0.0), writes=SFK)
        S.op(DVE, lambda e: e.memset(state_b[:, :, :], 0.0), writes=SBK)

        def rstd_from_ss(ss_ap, ss_keys, n, tp, inv_n):
            ms, msk = stat_cols(n)
            sd, sdk = stat_cols(n)
            rs, rsk = stat_cols(n)
            S.op(DVE, lambda e: e.tensor_scalar(out=ms[:tp], in0=ss_ap, scalar1=inv_n, scalar2=cfg.EPS,
                                                op0=ALU.mult, op1=ALU.add), reads=ss_keys, writes=msk)
            S.op(ACT, lambda e: e.activation(out=sd[:tp], in_=ms[:tp], func=AF.Sqrt), reads=msk, writes=sdk)
            S.op(DVE, lambda e: e.reciprocal(out=rs[:tp], in_=sd[:tp]), reads=sdk, writes=rsk)
            return rs, rsk

        def transpose_to(src_ap, tp, ncols, dst_fn):
            nj_all = ncols // 128
            j0 = 0
            while j0 < nj_all:
                nj = min(8, nj_all - j0)
                ps = psT.next()

                def emit_t(e, ps=ps, j0=j0, nj=nj):
                    ins = None
                    for j in range(nj):
                        ins = e.transpose(out=ps[:, j * 128:j * 128 + tp],
                                          in_=src_ap[:tp, (j0 + j) * 128:(j0 + j + 1) * 128],
                                          identity=ident_b[:tp, :tp])
                    return ins
                S.op(PE, emit_t, reads=[src_ap[:tp, :], ident_b], writes=[ps])
                dst = dst_fn(j0, nj)
                src_ps = ps[:, 0:nj * 128].rearrange("p (j t) -> p j t", j=nj)[:, :, 0:tp]
                S.op(ACT, lambda e, dst=dst, src_ps=src_ps: e.copy(out=dst, in_=src_ps), reads=[ps], writes=[dst])
                j0 += nj

        class Lay:
            pass

        def layout(T):
            L = Lay()
            tp = min(T, 128)
            nt = (T + 127) // 128
            L.tp, L.nt, L.T = tp, nt, T
            if T > 1:
                X0, A0, H0 = 0, 16384, 24576
                L.x_tm = AV(X0, 16384, F32).rearrange("p (t d) -> p t d", t=NTB)
                L.actT = AV(A0, 8192).rearrange("p (k t) -> p k t", k=KT)
                L.hT = AV(H0, KTF * TB).rearrange("p (k t) -> p k t", k=KTF)
                L.aqT = AV(0, 8192).rearrange("p (k t) -> p k t", k=16)
                L.iqT = AV(8192, 4096).rearrange("p (k t) -> p k t", k=8)
                L.diagW = AV(12288, 2048).rearrange("p (h t) -> p h t", h=16)
                L.mb = AV(14336, 2048)
                L.attT = AV(H0, 8192).rearrange("p (k t) -> p k t", k=16)
                L.S_sb = AV(32768, 4096, F32)
                L.junk = AV(36864, 2048)
                L.Rring = Ring([AV(38912 + 512 * i, 512) for i in range(4)])
                L.pTring = Ring([AV(40960 + 512 * i, 512) for i in range(3)])
                L.rden = AV(42496, 1024, F32)
                L.mb2 = AV(43520, 2048)
                L.ogT = AV(32768, 8192).rearrange("p (k t) -> p k t", k=16)
                L.qrT = AV(0, 2048).rearrange("p (h t) -> p h t", h=4)
                L.krT = AV(2048, 2048).rearrange("p (h t) -> p h t", h=4)
                L.ktm = AV(4096, 2048).rearrange("p (t c) -> p t c", t=NTB)
                L.vtm = AV(6144, 4096).rearrange("p (t c) -> p t c", t=NTB)
                L.sgtm = AV(10240, 4096).rearrange("p (t c) -> p t c", t=NTB)
                L.rope = Ring([AV(14336 + 1024 * i, 1024, F32).rearrange("p (a h f) -> p a h f", a=2, h=4) for i in range(2)])
                L.o_raw = AV(40960, 2048, F32).rearrange("p (h e) -> p h e", h=4)
                L.G = AV(43008, 1024)
                L.og_tm = AV(44032, 1024)
                L.sTm = Ring([AV(45056 + 128 * i, 128) for i in range(4)])
                L.m_tm = AV(0, 8192).rearrange("p (t d) -> p t d", t=NTB)
                L.gtr = Ring([AV(8192 + 512 * i, 512) for i in range(3)])
            else:
                b = [20480]

                def al(n):
                    o = b[0]
                    b[0] += (n + 63) // 64 * 64
                    return o
                L.x_tm = AV(al(4096), 4096, F32).rearrange("p (t d) -> p t d", t=1)
                L.actT = AV(al(16), 16).rearrange("p (k t) -> p k t", k=KT)
                L.hT = AV(al(KTF), KTF).rearrange("p (k t) -> p k t", k=KTF)
                L.aqT = AV(al(16), 16).rearrange("p (k t) -> p k t", k=16)
                L.attT = AV(al(16), 16).rearrange("p (k t) -> p k t", k=16)
                L.ogT = AV(al(16), 16).rearrange("p (k t) -> p k t", k=16)
                L.qrT = AV(al(4), 4).rearrange("p (h t) -> p h t", h=4)
                L.krT = AV(al(4), 4).rearrange("p (h t) -> p h t", h=4)
                L.ktm = AV(al(512), 512).rearrange("p (t c) -> p t c", t=1)
                L.vtm = AV(al(1024), 1024).rearrange("p (t c) -> p t c", t=1)
                L.sgtm = AV(al(1024), 1024).rearrange("p (t c) -> p t c", t=1)
                L.rope = Ring([AV(al(1024), 1024, F32).rearrange("p (a h f) -> p a h f", a=2, h=4) for i in range(2)])
                L.o_raw = AV(al(2048), 2048, F32).rearrange("p (h e) -> p h e", h=4)
                L.G = AV(al(1024), 1024)
                L.og_tm = AV(al(1024), 1024)
                L.sTm = Ring([AV(al(128), 128) for i in range(4)])
                L.m_tm = AV(al(2048), 2048).rearrange("p (t d) -> p t d", t=1)
                L.gtr = Ring([AV(al(512), 512) for i in range(3)])
                L.iqTs = AV(al(16), 16)
                L.ikTs = AV(al(2), 2)
                L.wcol = AV(al(2), 2)
                L.akTs = AV(al(4), 4)
                L.kn_s = AV(al(512), 512)
                L.v_sb = AV(al(512), 512)
                L.ik_sb = AV(al(64), 64)
                L.iw_s = AV(al(32), 32, F32)
                assert b[0] <= ARENA
            return L

        def rmsnorm_transpose(L, gain_dram):
            tp, nt = L.tp, L.nt
            S.dma(SP, lambda e: e.dma_start(out=gain_bc[:, :], in_=gain_dram.partition_broadcast(128)),
                  writes=[gain_bc])
            for t in range(nt):
                xn = xn_ring.next()
                ss, ssk = stat_cols(1)
                xt = L.x_tm[:tp, t, :]
                S.op(DVE, lambda e, xt=xt, xn=xn, ss=ss: e.scalar_tensor_tensor(
                    out=xn[:tp, :], in0=xt, scalar=1.0, in1=xt, op0=ALU.mult, op1=ALU.mult, accum_out=ss[:tp]),
                    reads=[xt], writes=[xn] + ssk)
                rs, rsk = rstd_from_ss(ss[:tp], ssk, 1, tp, 1.0 / D)
                S.op(DVE, lambda e, xt=xt, xn=xn, rs=rs: e.scalar_tensor_tensor(
                    out=xn[:tp, :], in0=xt, scalar=rs[:tp], in1=gain_bc[:tp, :], op0=ALU.mult, op1=ALU.mult),
                    reads=[xt, gain_bc] + rsk, writes=[xn])
                transpose_to(xn, tp, D, lambda j0, nj, t=t: L.actT[:, j0:j0 + nj, t * 128:t * 128 + tp])

        def linear(L, act_ap, kt, w_dram, chunks, consumer, wkey):
            tp, nt = L.tp, L.nt
            wv = w_dram.rearrange("(k p) n -> p k n", p=128)
            for ci, blocks in enumerate(chunks):
                ncols = sum(n for _, n in blocks)
                kk_max = 8192 // ncols
                ksplits = [(k0, min(kk_max, kt - k0)) for k0 in range(0, kt, kk_max)]
                pss = [psL.next() for _ in range(nt)] if len(ksplits) > 1 else None
                for si, (k0, kk) in enumerate(ksplits):
                    wb = getattr(L, "wring", wring).next()
                    wbv = wb[:, 0:kk * ncols].rearrange("p (k n) -> p k n", k=kk)
                    skey = (wkey, ci, si)
                    nel = kk * ncols
                    if skey not in wslots:
                        slot = len(wslots)
                        assert slot < NSLOT
                        wslots[skey] = slot
                        off = 0
                        for (c0, n) in blocks:
                            S.dma(POOL, lambda e, wbv=wbv, off=off, n=n, c0=c0, k0=k0, kk=kk: e.dma_start(
                                out=wbv[:, :, off:off + n], in_=wv[:, k0:k0 + kk, c0:c0 + n]), writes=[wb])
                            off += n
                        S.dma(SP, lambda e, wb=wb, slot=slot, nel=nel: e.dma_start(
                            out=wcache[slot, :, 0:nel], in_=wb[:, 0:nel]), reads=[wb], writes=["wc%d" % slot])
                    else:
                        slot = wslots[skey]
                        S.dma(POOL, lambda e, wb=wb, slot=slot, nel=nel: e.dma_start(
                            out=wb[:, 0:nel], in_=wcache[slot, :, 0:nel]), reads=["wc%d" % slot], writes=[wb])
                    for t in range(nt):
                        flush_deferred(keep=2)
                        ps = psL.next() if pss is None else pss[t]

                        def emit_mm(e, ps=ps, t=t, k0=k0, kk=kk, wbv=wbv, ncols=ncols, si=si):
                            ins = None
                            for k in range(kk):
                                ins = e.matmul(ps[:tp, 0:ncols],
                                               lhsT=act_ap[:, k0 + k, t * 128:t * 128 + tp],
                                               rhs=wbv[:, k, :],
                                               start=(si == 0 and k == 0),
                                               stop=(si == len(ksplits) - 1 and k == kk - 1))
                            return ins
                        S.op(PE, emit_mm, reads=[wb, act_ap[:, :, t * 128:t * 128 + tp]], writes=[ps])
                        if si == len(ksplits) - 1:
                            consumer(ci, t, ps[:tp, 0:ncols], ps)
            flush_deferred(0)

        def ffn(L, gain_dram, w1, w2, wk):
            tp = L.tp
            rmsnorm_transpose(L, gain_dram)
            nch = DFF // 256

            def cons_h(ci, t, ps, pst):
                sg = f32s_ring.next()
                hb = bfs_ring.next()
                S.op(ACT, lambda e: e.activation(out=sg[:tp, 0:256], in_=ps[:, 0:256], func=AF.Silu),
                     reads=[pst], writes=[sg])
                S.op(DVE, lambda e: e.tensor_tensor(out=hb[:tp, 0:256], in0=ps[:, 256:512], in1=sg[:tp, 0:256],
                                                    op=ALU.mult), reads=[pst, sg], writes=[hb])
                deferred.append(lambda: transpose_to(
                    hb, tp, 256, lambda j0, nj: L.hT[:, 2 * ci + j0:2 * ci + j0 + nj, t * 128:t * 128 + tp]))

            linear(L, L.actT, KT, w1, [[(s * 256, 256), (DFF + s * 256, 256)] for s in range(nch)], cons_h, wk + "a")

            def cons_y(ci, t, ps, pst):
                xs_ = L.x_tm[:tp, t, ci * 256:(ci + 1) * 256]
                S.op(DVE, lambda e: e.scalar_tensor_tensor(out=xs_, in0=ps, scalar=0.5, in1=xs_,
                                                           op0=ALU.mult, op1=ALU.add),
                     reads=[pst, xs_], writes=[xs_])

            linear(L, L.hT, KTF, w2, [[(n * 256, 256)] for n in range(D // 256)], cons_y, wk + "b")

        def headnorm(tp, ps, pst, gbc):
            raw = f32s_ring.next()
            sq = f32s_ring.next()
            S.op(ACT, lambda e: e.copy(out=raw[:tp, :], in_=ps), reads=[pst], writes=[raw])
            S.op(DVE, lambda e: e.tensor_tensor(out=sq[:tp, :], in0=raw[:tp, :], in1=raw[:tp, :], op=ALU.mult),
                 reads=[raw], writes=[sq])
            ss, ssk = stat_cols(4)
            S.op(DVE, lambda e: e.tensor_reduce(out=ss[:tp], in_=sq[:tp, :].rearrange("p (h d) -> p h d", h=4),
                                                axis=AX.X, op=ALU.add), reads=[sq], writes=ssk)
            rs, rsk = rstd_from_ss(ss[:tp], ssk, 4, tp, 1.0 / 128)
            S.op(DVE, lambda e: e.tensor_tensor(
                out=sq[:tp, :].rearrange("p (h d) -> p h d", h=4),
                in0=raw[:tp, :].rearrange("p (h d) -> p h d", h=4),
                in1=rs[:tp].unsqueeze(2).to_broadcast([tp, 4, 128]), op=ALU.mult),
                reads=[raw] + rsk, writes=[sq])
            S.op(DVE, lambda e: e.tensor_tensor(
                out=raw[:tp, :].rearrange("p (h d) -> p h d", h=4),
                in0=sq[:tp, :].rearrange("p (h d) -> p h d", h=4),
                in1=gbc[:tp, :].unsqueeze(1).to_broadcast([tp, 4, 128]), op=ALU.mult),
                reads=[sq, gbc], writes=[raw])
            return raw

        def mixer(L, tok0, row0, is_sample, prefix=False):
            tp, nt, T = L.tp, L.nt, L.T
            rmsnorm_transpose(L, g_mx)
            for t in range(nt if not prefix else 0):
                xt = L.x_tm[:tp, t, :]
                S.dma(SP, lambda e, t=t, xt=xt: e.dma_start(out=sc_x[t * 128:t * 128 + tp, :], in_=xt),
                      reads=[xt], writes=["sc_x%d" % t])
            k_out, v_out, ik_out = (k_s, v_s, ik_s) if is_sample else (k_p, v_p, ik_p)

            def rows(t):
                return slice(row0 + t * 128, row0 + t * 128 + tp)

            def cons_gate(dst):
                def f(ci, t, ps, pst):
                    gb = bfs_ring.next()
                    S.op(ACT, lambda e: e.activation(out=gb[:tp, :], in_=ps, func=AF.Sigmoid), reads=[pst], writes=[gb])
                    S.dma(SP, lambda e: e.dma_start(out=dst[t * 128:t * 128 + tp, ci * 512:(ci + 1) * 512], in_=gb[:tp, :]),
                          reads=[gb], writes=["%s_%d_%d" % (dst.tensor.name, t, ci)])
                return f
            if not prefix:
                linear(L, L.actT, KT, w_in, [[(O_GR + c * 512, 512)] for c in range(4)], cons_gate(sc_gr), "gr")
                linear(L, L.actT, KT, w_in, [[(O_GA + c * 512, 512)] for c in range(4)], cons_gate(sc_ga), "ga")

            def cons_ak(ci, t, ps, pst):
                kn = headnorm(tp, ps, pst, gk_bc)
                if not prefix:
                    S.dma(SP, lambda e: e.dma_start(out=k_out[rows(t), :], in_=kn[:tp, :]), reads=[kn])
                kb = bfs_ring.next()
                S.op(ACT, lambda e: e.copy(out=kb[:tp, :], in_=kn[:tp, :]), reads=[kn], writes=[kb])
                if is_sample:
                    S.op(DVE, lambda e: e.tensor_copy(out=L.kn_s[:1, :], in_=kn[:1, :]), reads=[kn], writes=[L.kn_s])
                    deferred.append(lambda: transpose_to(kb, tp, 512, lambda j0, nj: L.akTs[:, j0:j0 + nj].unsqueeze(2)))
                else:
                    deferred.append(lambda: transpose_to(
                        kb, tp, 512, lambda j0, nj: kT[:, j0:j0 + nj, tok0 + t * 128:tok0 + t * 128 + tp]))

            def cons_av(ci, t, ps, pst):
                raw = f32s_ring.next()
                S.op(ACT, lambda e: e.copy(out=raw[:tp, :], in_=ps), reads=[pst], writes=[raw])
                if not prefix:
                    S.dma(SP, lambda e: e.dma_start(out=v_out[rows(t), :], in_=raw[:tp, :]), reads=[raw])
                dstv = L.v_sb[:1, :] if is_sample else Vc[:tp, tok0 // 128 + t, :]
                S.op(DVE, lambda e: e.tensor_copy(out=dstv, in_=raw[:tp, :]), reads=[raw], writes=[dstv])

            def cons_ik(ci, t, ps, pst):
                raw = f32s_ring.next()
                S.op(ACT, lambda e: e.copy(out=raw[:tp, 0:80], in_=ps), reads=[pst], writes=[raw])
                if not prefix:
                    S.dma(SP, lambda e: e.dma_start(out=ik_out[rows(t), :], in_=raw[:tp, 0:64]), reads=[raw])
                if is_sample:
                    S.op(DVE, lambda e: e.tensor_copy(out=L.ik_sb[:1, :], in_=raw[:1, 0:64]), reads=[raw], writes=[L.ik_sb])
                    S.op(DVE, lambda e: e.tensor_scalar(out=L.iw_s[:1, 0:16], in0=raw[:1, 64:80], scalar1=IDX_W_SCALE,
                                                        scalar2=None, op0=ALU.mult), reads=[raw], writes=[L.iw_s])
                else:
                    ib = bfs_ring.next()
                    S.op(DVE, lambda e: e.tensor_copy(out=ib[:tp, 0:64], in_=raw[:tp, 0:64]), reads=[raw], writes=[ib])
                    S.op(DVE, lambda e: e.tensor_copy(out=ib[:tp, 64:128], in_=raw[:tp, 0:64]), reads=[raw], writes=[ib])
                    S.op(DVE, lambda e: e.tensor_scalar(out=wsc[:tp, t, :], in0=raw[:tp, 64:80], scalar1=IDX_W_SCALE,
                                                        scalar2=None, op0=ALU.mult), reads=[raw], writes=[wsc])
                    deferred.append(lambda: transpose_to(
                        ib, tp, 128, lambda j0, nj: ikT2[:, tok0 + t * 128:tok0 + t * 128 + tp].unsqueeze(1)))

            linear(L, L.actT, KT, w_in, [[(O_AK, 512)]], cons_ak, "ak")
            linear(L, L.actT, KT, w_in, [[(O_AV, 512)]], cons_av, "av")
            linear(L, L.actT, KT, w_in, [[(O_IK, 80)]], cons_ik, "ik")

            def cons_aq(ci, t, ps, pst):
                qn = headnorm(tp, ps, pst, gq_bc)
                qb = bfs_ring.next()
                S.op(ACT, lambda e: e.copy(out=qb[:tp, :], in_=qn[:tp, :]), reads=[qn], writes=[qb])
                deferred.append(lambda: transpose_to(
                    qb, tp, 512, lambda j0, nj: L.aqT[:, 4 * ci + j0:4 * ci + j0 + nj, t * 128:t * 128 + tp]))
            if not prefix:
                linear(L, L.actT, KT, w_in, [[(O_AQ + c * 512, 512)] for c in range(4)], cons_aq, "aq")

            def cons_iq(ci, t, ps, pst):
                qb = bfs_ring.next()
                S.op(ACT, lambda e: e.copy(out=qb[:tp, :], in_=ps), reads=[pst], writes=[qb])
                if is_sample:
                    def tr():
                        psx = psT.next()

                        def em(e):
                            ins = None
                            for j in range(8):
                                ins = e.transpose(out=psx[0:64, 2 * j:2 * j + 1], in_=qb[:1, j * 64:(j + 1) * 64],
                                                  identity=ident_b[:1, :1])
                            return ins
                        S.op(PE, em, reads=[qb, ident_b], writes=[psx])
                        dst = L.iqTs[0:64, 8 * ci:8 * ci + 8]
                        S.op(ACT, lambda e: e.copy(out=dst, in_=psx[0:64, 0:16].rearrange("p (j two) -> p j two", two=2)[:, :, 0]),
                             reads=[psx], writes=[dst])
                    deferred.append(tr)
                else:
                    deferred.append(lambda: transpose_to(
                        qb, tp, 512, lambda j0, nj: L.iqT[:, 4 * ci + j0:4 * ci + j0 + nj, t * 128:t * 128 + tp]))
            if not prefix:
                linear(L, L.actT, KT, w_in, [[(O_IQ + c * 512, 512)] for c in range(2)], cons_iq, "iq")

            if prefix:
                pass
            elif is_sample:
                decode_dsa(L)
            else:
                prompt_dsa_all(L, tok0)

            for hg in range(2):
                retention_group(L, tok0, hg, is_sample, prefix)
            if prefix:
                return
            if is_sample:
                S.dma(SP, lambda e: e.dma_start(out=rs_s.rearrange("h d e -> d h e"), in_=state_f[:, :, :]),
                      reads=SFK)

            def cons_ao(ci, t, ps, pst):
                gt = L.gtr.next()
                S.dma(SP, lambda e: e.dma_start(out=gt[:tp, :], in_=sc_ga[t * 128:t * 128 + tp, ci * 512:(ci + 1) * 512]),
                      reads=["sc_ga_%d_%d" % (t, ci)], writes=[gt])
                dst = L.m_tm[:tp, t, ci * 512:(ci + 1) * 512]
                S.op(DVE, lambda e: e.tensor_tensor(out=dst, in0=ps, in1=gt[:tp, :], op=ALU.mult),
                     reads=[pst, gt], writes=[dst])
            linear(L, L.attT, KT, w_ao, [[(c * 512, 512)] for c in range(4)], cons_ao, "ao")

            def cons_ro(ci, t, ps, pst):
                gt = L.gtr.next()
                S.dma(SP, lambda e: e.dma_start(out=gt[:tp, :], in_=sc_gr[t * 128:t * 128 + tp, ci * 512:(ci + 1) * 512]),
                      reads=["sc_gr_%d_%d" % (t, ci)], writes=[gt])
                tmp = f32s_ring.next()
                dst = L.m_tm[:tp, t, ci * 512:(ci + 1) * 512]
                S.op(DVE, lambda e: e.tensor_tensor(out=tmp[:tp, :], in0=ps, in1=gt[:tp, :], op=ALU.mult),
                     reads=[pst, gt], writes=[tmp])
                S.op(DVE, lambda e: e.tensor_tensor(out=dst, in0=tmp[:tp, :], in1=dst, op=ALU.add),
                     reads=[tmp, dst], writes=[dst])
            linear(L, L.ogT, KT, w_ro, [[(c * 512, 512)] for c in range(4)], cons_ro, "ro")
            for t in range(nt):
                transpose_to(L.m_tm[:, t, :], tp, D, lambda j0, nj, t=t: L.actT[:, j0:j0 + nj, t * 128:t * 128 + tp])
            for t in range(nt):
                xt = L.x_tm[:tp, t, :]
                S.dma(SP, lambda e, t=t, xt=xt: e.dma_start(out=xt, in_=sc_x[t * 128:t * 128 + tp, :]),
                      reads=["sc_x%d" % t], writes=[xt])

            def cons_o(ci, t, ps, pst):
                xs_ = L.x_tm[:tp, t, ci * 512:(ci + 1) * 512]
                S.op(DVE, lambda e: e.tensor_tensor(out=xs_, in0=ps, in1=xs_, op=ALU.add), reads=[pst, xs_], writes=[xs_])
            linear(L, L.actT, KT, w_o, [[(c * 512, 512)] for c in range(4)], cons_o, "wo")

        def retention_group(L, tok0, hg, is_sample, prefix=False):
            tp, nt = L.tp, L.nt
            pos0 = SEQ if is_sample else tok0

            def cons_rot(which):
                def f(ci, t, ps, pst):
                    raw = f32s_ring.next()
                    tmp = f32s_ring.next()
                    ob = bfs_ring.next()
                    rp = L.rope.next()
                    S.dma(SP, lambda e: e.dma_start(
                        out=rp[:tp, :, :, :],
                        in_=c_rope[pos0 + t * 128:pos0 + t * 128 + tp, 2 * which:2 * which + 2, 4 * hg:4 * hg + 4, :]),
                        writes=[rp[:tp, :, :, :]])
                    S.op(ACT, lambda e: e.copy(out=raw[:tp, :], in_=ps), reads=[pst], writes=[raw])
                    x = raw[:tp, :].rearrange("p (h two f) -> p h two f", h=4, two=2)
                    x1, x2 = x[:, :, 0, :], x[:, :, 1, :]
                    C = rp[:tp, 0, :, :]
                    Sn = rp[:tp, 1, :, :]
                    tv = tmp[:tp, :].rearrange("p (a h f) -> p a h f", a=2, h=4)
                    o = ob[:tp, :].rearrange("p (h two f) -> p h two f", h=4, two=2)
                    rk = [raw, rp[:tp, :, :, :]]
                    S.op(DVE, lambda e: e.tensor_tensor(out=tv[:, 0], in0=x1, in1=C, op=ALU.mult), reads=rk, writes=[tmp])
                    S.op(DVE, lambda e: e.tensor_tensor(out=tv[:, 1], in0=x2, in1=Sn, op=ALU.mult), reads=rk, writes=[tmp])
                    S.op(DVE, lambda e: e.tensor_tensor(out=o[:, :, 0, :], in0=tv[:, 0], in1=tv[:, 1], op=ALU.subtract),
                         reads=[tmp], writes=[ob])
                    S.op(DVE, lambda e: e.tensor_tensor(out=tv[:, 0], in0=x1, in1=Sn, op=ALU.mult), reads=rk + [ob], writes=[tmp])
                    S.op(DVE, lambda e: e.tensor_tensor(out=tv[:, 1], in0=x2, in1=C, op=ALU.mult), reads=rk, writes=[tmp])
                    S.op(DVE, lambda e: e.tensor_tensor(out=o[:, :, 1, :], in0=tv[:, 0], in1=tv[:, 1], op=ALU.add),
                         reads=[tmp], writes=[ob])
                    if which == 1:
                        kd = L.ktm[:tp, t, :]
                        S.op(ACT, lambda e: e.copy(out=kd, in_=ob[:tp, :]), reads=[ob], writes=[kd])
                    dstT = L.qrT if which == 0 else L.krT
                    if not prefix:
                        deferred.append(lambda: transpose_to(
                            ob, tp, 512, lambda j0, nj: dstT[:, j0:j0 + nj, t * 128:t * 128 + tp]))
                return f
            if not prefix:
                linear(L, L.actT, KT, w_in, [[(O_RQ + hg * 512, 512)]], cons_rot(0), "rq%d" % hg)
            linear(L, L.actT, KT, w_in, [[(O_RK + hg * 512, 512)]], cons_rot(1), "rk%d" % hg)

            def cons_rv(ci, t, ps, pst):
                dst = L.vtm[:tp, t, ci * 512:(ci + 1) * 512]
                S.op(ACT, lambda e: e.copy(out=dst, in_=ps), reads=[pst], writes=[dst])
            linear(L, L.actT, KT, w_in, [[(O_RV + hg * 1024 + c * 512, 512)] for c in range(2)], cons_rv, "rv%d" % hg)

            def cons_rg(ci, t, ps, pst):
                dst = L.sgtm[:tp, t, ci * 512:(ci + 1) * 512]
                S.op(ACT, lambda e: e.activation(out=dst, in_=ps, func=AF.Silu), reads=[pst], writes=[dst])
            if not prefix:
                linear(L, L.actT, KT, w_in, [[(O_RG + hg * 1024 + c * 512, 512)] for c in range(2)], cons_rg, "rg%d" % hg)

            log_g = [float(np.log1p(-np.exp2(-5.0 - h))) for h in range(cfg.RH)]
            def ret_tile(t):
                ts = slice(t * 128, t * 128 + tp)
                sms = []
                for hl in range(4 if not prefix else 0):
                    ps_s = psL.next()
                    S.op(PE, lambda e, ps_s=ps_s, hl=hl, ts=ts: e.matmul(ps_s[:tp, 0:tp], lhsT=L.krT[:, hl, ts], rhs=L.qrT[:, hl, ts],
                                                               start=True, stop=True),
                         reads=[L.krT[:, hl, ts], L.qrT[:, hl, ts]], writes=[ps_s])
                    sm = L.sTm.next()
                    S.op(DVE, lambda e, ps_s=ps_s, sm=sm: e.tensor_tensor(out=sm[:tp, 0:tp], in0=ps_s[:tp, 0:tp],
                                                                           in1=caus[:tp, 0:tp], op=ALU.mult),
                         reads=[ps_s, caus], writes=[sm])
                    sms.append(sm)
                for hl in range(4):
                    h = 4 * hg + hl
                    ps_kv = psM[hl % 2]
                    vs_ = L.vtm[:tp, t, hl * 256:(hl + 1) * 256]
                    if not prefix:
                        sm = sms[hl]
                        ps_o = psL.next()

                    if not prefix:
                        def em_o(e, ps_o=ps_o, sm=sm, vs_=vs_, hl=hl, h=h, ts=ts):
                            e.matmul(ps_o[:tp, 0:256], lhsT=sm[:tp, 0:tp], rhs=vs_, start=True, stop=False)
                            return e.matmul(ps_o[:tp, 0:256], lhsT=L.qrT[:, hl, ts], rhs=state_b[:, h, :], start=False, stop=True)
                        S.op(PE, em_o, reads=[sm, vs_, L.qrT[:, hl, ts], "state_b%d" % h], writes=[ps_o])
                    kslice = L.ktm[:tp, t, hl * 128:(hl + 1) * 128]
                    S.op(PE, lambda e, ps_kv=ps_kv, kslice=kslice, vs_=vs_: e.matmul(
                        ps_kv[:, 0:256], lhsT=kslice, rhs=vs_, start=True, stop=True),
                        reads=[kslice, vs_], writes=[ps_kv])
                    if not prefix:
                        orw = L.o_raw[:tp, hl, :]
                        S.op(ACT, lambda e, orw=orw, ps_o=ps_o: e.copy(out=orw, in_=ps_o[:tp, 0:256]), reads=[ps_o], writes=[orw])
                    sf = state_f[:, h, :]
                    gC = float(np.exp(log_g[h] * tp))
                    S.op(DVE, lambda e, sf=sf, ps_kv=ps_kv: e.tensor_tensor(out=sf, in0=sf, in1=ps_kv[:, 0:256], op=ALU.add),
                         reads=[ps_kv, "state_f%d" % h], writes=["state_f%d" % h])
                    S.op(DVE, lambda e, sf=sf, gC=gC: e.tensor_scalar(out=sf, in0=sf, scalar1=gC, scalar2=None, op0=ALU.mult),
                         reads=["state_f%d" % h], writes=["state_f%d" % h])
                    S.op(ACT, lambda e, sf=sf, h=h: e.copy(out=state_b[:, h, :], in_=sf),
                         reads=["state_f%d" % h], writes=["state_b%d" % h])
                if prefix:
                    return
                ss, ssk = stat_cols(4)
                for hl in range(4):
                    orw = L.o_raw[:tp, hl, :]
                    S.op(DVE, lambda e, orw=orw, hl=hl: e.scalar_tensor_tensor(
                        out=L.og_tm[:tp, hl * 256:(hl + 1) * 256], in0=orw, scalar=1.0, in1=orw,
                        op0=ALU.mult, op1=ALU.mult, accum_out=ss[:tp, hl:hl + 1]),
                        reads=[orw], writes=[L.og_tm[:tp, :]] + ssk)
                rs, rsk = rstd_from_ss(ss[:tp], ssk, 4, tp, 1.0 / 256)
                Gv = L.G[:tp, :].rearrange("p (h e) -> p h e", h=4)
                S.op(DVE, lambda e: e.tensor_tensor(
                    out=Gv, in0=L.sgtm[:tp, t, :].rearrange("p (h e) -> p h e", h=4),
                    in1=gr_bc[:tp, :].unsqueeze(1).to_broadcast([tp, 4, 256]), op=ALU.mult),
                    reads=[L.sgtm[:tp, t, :], gr_bc], writes=[L.G[:tp, :]])
                orall = L.o_raw[:tp, :, :]
                S.op(DVE, lambda e: e.tensor_tensor(out=orall, in0=orall,
                                                    in1=rs[:tp].unsqueeze(2).to_broadcast([tp, 4, 256]), op=ALU.mult),
                     reads=[orall] + rsk, writes=[orall])
                S.op(DVE, lambda e: e.tensor_tensor(out=L.og_tm[:tp, :].rearrange("p (h e) -> p h e", h=4),
                                                    in0=orall, in1=Gv, op=ALU.mult),
                     reads=[orall, L.G[:tp, :]], writes=[L.og_tm[:tp, :]])
                transpose_to(L.og_tm, tp, 1024, lambda j0, nj, t=t: L.ogT[:, 8 * hg + j0:8 * hg + j0 + nj, t * 128:t * 128 + tp])

            for t in range(nt):
                ret_tile(t)

        def bisect_init(tp):
            T_ = lambda i: thr[:tp, i:i + 1]
            k_ = lambda i: "thr%d" % i
            S.op(DVE, lambda e: e.tensor_tensor(out=T_(2), in0=T_(0), in1=T_(1), op=ALU.subtract),
                 reads=[k_(0), k_(1)], writes=[k_(2)])
            S.op(DVE, lambda e: e.tensor_scalar(out=T_(2), in0=T_(2), scalar1=1.0001, scalar2=1e-20,
                                                op0=ALU.mult, op1=ALU.add), reads=[k_(2)], writes=[k_(2)])
            S.op(DVE, lambda e: e.tensor_scalar(out=w_all[:tp, :], in0=pow2[:tp, :], scalar1=T_(2), scalar2=None, op0=ALU.mult),
                 reads=[k_(2), pow2], writes=[w_all])
            S.op(DVE, lambda e: e.tensor_tensor(out=T_(5), in0=T_(1), in1=w_all[:tp, 1:2], op=ALU.add),
                 reads=[k_(1), w_all], writes=[k_(5)])

        def bisect_iters(tp, count_fn, K, i0, i1):
            T_ = lambda i: thr[:tp, i:i + 1]
            k_ = lambda i: "thr%d" % i
            for i in range(i0, i1):
                count_fn()
                S.op(DVE, lambda e: e.tensor_scalar(out=T_(7), in0=T_(6), scalar1=K - 0.5, scalar2=0.5,
                                                    op0=ALU.is_ge, op1=ALU.subtract),
                     reads=[k_(6)], writes=[k_(7)])
                S.op(DVE, lambda e, i=i: e.scalar_tensor_tensor(out=T_(5), in0=T_(7), scalar=w_all[:tp, i + 1:i + 2], in1=T_(5),
                                                                op0=ALU.mult, op1=ALU.add),
                     reads=[k_(7), k_(5), w_all], writes=[k_(5)])

        def bisect_finish(tp):
            n = cfg.NIT
            S.op(DVE, lambda e: e.tensor_tensor(out=thr[:tp, 3:4], in0=thr[:tp, 5:6], in1=w_all[:tp, n + 1:n + 2], op=ALU.subtract),
                 reads=["thr5", w_all], writes=["thr3"])

        def bisect(S_ap, tp, count_fn, K):
            bisect_init(tp)
            bisect_iters(tp, count_fn, K, 0, cfg.NIT)
            bisect_finish(tp)

        def dsa_index(L, tok0, t):
            tp = L.tp
            ts = slice(t * 128, t * 128 + tp)
            nk = tok0 + (t + 1) * 128
            for h in range(16):
                S.op(DVE, lambda e, h=h: e.tensor_scalar(out=L.diagW[:tp, h, 0:tp], in0=ident_b[:tp, 0:tp],
                                                         scalar1=wsc[:tp, t, h:h + 1], scalar2=None, op0=ALU.mult),
                     reads=[ident_b, wsc], writes=[L.diagW[:tp, h, 0:tp]])
            def idx_chunk(kc, c0):
                cw = min(512, nk - c0)
                S_ps = psM[kc % 2]
                pend = []
                for h in range(16):
                    r0 = (h % 2) * 64
                    Pps = psL.next()
                    lq = L.iqT[r0:r0 + 64, h // 2, ts]
                    rk_ = ikT2[r0:r0 + 64, c0:c0 + cw]
                    S.op(PE, lambda e, Pps=Pps, lq=lq, rk_=rk_: e.matmul(Pps[:tp, 0:cw], lhsT=lq, rhs=rk_, start=True, stop=True),
                         reads=[lq, ikT2], writes=[Pps])
                    Rb = L.Rring.next()
                    S.op(ACT, lambda e, Pps=Pps, Rb=Rb: e.activation(out=Rb[:tp, 0:cw], in_=Pps[:tp, 0:cw], func=AF.Relu),
                         reads=[Pps], writes=[Rb])

                    def dg(h=h, Rb=Rb, S_ps=S_ps, cw=cw):
                        S.op(PE, lambda e: e.matmul(S_ps[:tp, 0:cw], lhsT=L.diagW[:tp, h, 0:tp], rhs=Rb[:tp, 0:cw],
                                                    start=(h == 0), stop=(h == 15)),
                             reads=[L.diagW[:tp, h, 0:tp], Rb], writes=[S_ps])
                    pend.append(dg)
                    if len(pend) > 2:
                        pend.pop(0)()
                while pend:
                    pend.pop(0)()
                dstS = L.S_sb[:tp, c0:c0 + cw]
                S.op(ACT, lambda e, dstS=dstS, S_ps=S_ps: e.copy(out=dstS, in_=S_ps[:tp, 0:cw]), reads=[S_ps], writes=[dstS])

            for kc, c0 in enumerate(range(0, nk, 512)):
                idx_chunk(kc, c0)
            Sall = L.S_sb[:tp, 0:nk]
            S.op(DVE, lambda e: e.tensor_reduce(out=thr[:tp, 0:1], in_=Sall, axis=AX.X, op=ALU.max), reads=[Sall], writes=["thr0"])
            S.op(DVE, lambda e: e.tensor_reduce(out=thr[:tp, 1:2], in_=Sall, axis=AX.X, op=ALU.min), reads=[Sall], writes=["thr1"])
            pre_ = L.S_sb[:tp, 0:NPRE]
            S.op(DVE, lambda e: e.tensor_scalar(out=pre_, in0=pre_, scalar1=negflag_sb[:tp, 0:1], scalar2=None, op0=ALU.add),
                 reads=[pre_, negflag_sb], writes=[pre_])
            dg_ = L.S_sb[:tp, nk - 128:nk]
            S.op(DVE, lambda e: e.tensor_tensor(out=dg_, in0=dg_, in1=negu[:tp, :], op=ALU.add), reads=[dg_, negu], writes=[dg_])

            def count_fn():
                S.op(DVE, lambda e: e.tensor_scalar(out=L.junk[:tp, 0:nk], in0=Sall, scalar1=thr[:tp, 5:6], scalar2=0.0,
                                                    op0=ALU.is_ge, op1=ALU.add, accum_out=thr[:tp, 6:7]),
                     reads=[Sall, "thr5"], writes=[L.junk[:tp, 0:nk], "thr6"])
            bisect_init(tp)
            return count_fn, Sall

        def dsa_mask(L, tok0, t, Sall, mbuf):
            tp = L.tp
            nk = tok0 + (t + 1) * 128
            bisect_finish(tp)
            mbv = mbuf[:tp, 0:nk]
            S.op(DVE, lambda e: e.tensor_scalar(out=mbv, in0=Sall, scalar1=thr[:tp, 3:4], scalar2=NEG,
                                                op0=ALU.is_lt, op1=ALU.mult), reads=[Sall, "thr3"], writes=[mbv])

        def dsa_attend(L, tok0, t, g, mbuf):
            tp = L.tp
            ts = slice(t * 128, t * 128 + tp)
            nk = tok0 + (t + 1) * 128
            nkb = nk // 128
            identrep = ident_b[:tp, 0:tp].unsqueeze(1).to_broadcast([tp, 4, tp])
            def att_group(g):
                oT, den = psM[0], psM[1]
                pend = []
                qv = L.aqT[:, 4 * g:4 * g + 4, ts]
                for kb in range(nkb):
                    sps = psL.next()
                    spv = sps[:, 0:4 * tp].rearrange("p (h t) -> p h t", h=4)
                    kslice = kT[:, g, kb * 128:(kb + 1) * 128]
                    mslice = mbuf[:tp, kb * 128:(kb + 1) * 128]

                    def em_s(e, spv=spv, kslice=kslice, mslice=mslice):
                        e.matmul(spv, lhsT=kslice, rhs=qv, start=True, stop=False)
                        return e.matmul(spv, lhsT=mslice, rhs=identrep, start=False, stop=True)
                    S.op(PE, em_s, reads=[kT, qv, mslice, ident_b], writes=[sps])
                    pT_ = L.pTring.next()
                    S.op(ACT, lambda e, sps=sps, pT_=pT_: e.activation(out=pT_[:, 0:4 * tp], in_=sps[:, 0:4 * tp], func=AF.Exp),
                         reads=[sps], writes=[pT_])

                    def pv(kb=kb, pT_=pT_):
                        vsl = Vc[:, kb, g * 128:(g + 1) * 128]

                        def em(e):
                            e.matmul(oT[:, 0:4 * tp], lhsT=vsl, rhs=pT_[:, 0:4 * tp], start=(kb == 0), stop=(kb == nkb - 1))
                            return e.matmul(den[:, 0:4 * tp], lhsT=ones_b[:, :], rhs=pT_[:, 0:4 * tp],
                                            start=(kb == 0), stop=(kb == nkb - 1))
                        S.op(PE, em, reads=[Vc, pT_, ones_b], writes=[oT, den])
                    pend.append(pv)
                    if len(pend) > 1:
                        pend.pop(0)()
                while pend:
                    pend.pop(0)()
                S.op(DVE, lambda e: e.reciprocal(out=L.rden[:, 0:4 * tp], in_=den[:, 0:4 * tp]), reads=[den], writes=[L.rden])
                dsta = L.attT[:, 4 * g:4 * g + 4, ts]
                S.op(DVE, lambda e, dsta=dsta: e.tensor_tensor(
                    out=dsta, in0=oT[:, 0:4 * tp].rearrange("p (h t) -> p h t", h=4),
                    in1=L.rden[:, 0:4 * tp].rearrange("p (h t) -> p h t", h=4), op=ALU.mult),
                    reads=[oT, L.rden], writes=[dsta])

            att_group(g)

        def prompt_dsa_all(L, tok0):
            tp, nt = L.tp, L.nt
            mbufs = [L.mb, L.mb2]
            nsl = 4
            per = (cfg.NIT + nsl - 1) // nsl
            cur = dsa_index(L, tok0, 0)
            for t in range(nt):
                count_fn, Sall = cur
                for sl in range(nsl):
                    bisect_iters(tp, count_fn, cfg.TOPK, sl * per, min(cfg.NIT, (sl + 1) * per))
                    if t > 0:
                        dsa_attend(L, tok0, t - 1, sl, mbufs[(t - 1) % 2])
                dsa_mask(L, tok0, t, Sall, mbufs[t % 2])
                if t + 1 < nt:
                    cur = dsa_index(L, tok0, t + 1)
            for g in range(4):
                dsa_attend(L, tok0, nt - 1, g, mbufs[(nt - 1) % 2])

        def decode_dsa(L):
            D0 = 0
            kidx_g = AV(D0, 4096, F32)
            kidxTq = AV(4096, 4096)
            Rr = Ring([AV(8192 + 512 * i, 512) for i in range(4)])
            b = [10240]

            def al(n, dt=BF16):
                o = b[0]
                b[0] += (n * (2 if dt != BF16 else 1) + 63) // 64 * 64
                assert b[0] <= 20480
                return AV(o, n * (2 if dt != BF16 else 1), dt)
            Sg = al(130, F32)
            junkg = al(130)
            rsc = al(128, F32)
            dest = al(128, F32)
            mk = al(128, F32)
            Er = Ring([al(256, F32) for _ in range(3)])
            kg = al(1024, F32)
            vg = al(1024, F32)
            kgb = al(1024)
            vgb = al(1024)
            kgT = al(1024)
            pts = al(8, F32)
            idx4 = sb("idx4", [128, 1], I32)
            ptsb = sb("ptsb", [128, 1], I32)
            rowi = sb("rowi", [128, 2], I32)
            small = sb("dsm", [128, 64], F32)
            smallb = sb("dsmb", [128, 64], BF16)
            iota_d = sb("iota_d", [128, 256], F32)
            lstr = sb("lstr", [128, 128], BF16)
            jcol = sb("jcol", [128, 128], F32)
            dslot = sb("dslot", [128, 2], F32)
            rhs3 = sb("rhs3", [128, 128, 2], F32)
            cload(iota_d[:, :], c_iota)
            cload(jcol[:, :], c_jcol)
            cload(dslot[:, :], c_dslot)
            cload(mk[:, 0:128], c_lstr)
            S.op(DVE, lambda e: e.tensor_copy(out=lstr[:, :], in_=mk[:, 0:128]), reads=[mk], writes=[lstr])
            idx4b = sb("idx4b", [128, 1], I32)
            rowib = sb("rowib", [128, 2], I32)
            for tt_ in (idx4, idx4b, rowi, rowib):
                S.op(DVE, lambda e, tt_=tt_: e.memset(tt_[:, :], 0), writes=[tt_])
            cload(ptsb[:, :], ptab)
            S.op(DVE, lambda e: e.tensor_copy(out=pts[:, 0:1], in_=ptsb[:, :]), reads=[ptsb], writes=[pts])
            pw = psL.next()
            S.op(PE, lambda e: e.transpose(out=pw[0:16, 0:1], in_=L.iw_s[:1, 0:16], identity=ident_f[:1, :1]),
                 reads=[L.iw_s, ident_f], writes=[pw])
            S.op(ACT, lambda e: e.copy(out=L.wcol[0:16, 0:1], in_=pw[0:16, 0:1]), reads=[pw], writes=[L.wcol])
            px = psT.next()
            S.op(PE, lambda e: e.transpose(out=px[0:64, 0:1], in_=L.ik_sb[:1, 0:64], identity=ident_b[:1, :1]),
                 reads=[L.ik_sb, ident_b], writes=[px])
            S.op(ACT, lambda e: e.copy(out=L.ikTs[0:64, 0:1], in_=px[0:64, 0:1]), reads=[px], writes=[L.ikTs])
            psG = psM[0]
            for q4 in range(4):
                S.op(DVE, lambda e, q4=q4: e.tensor_scalar(out=idx4[:, :], in0=pts[:, 0:1], scalar1=4.0, scalar2=float(q4),
                                                           op0=ALU.mult, op1=ALU.add), reads=[pts], writes=[idx4])
                S.op(DVE, lambda e: e.tensor_copy(out=idx4b[:, :], in_=idx4[:, :]), reads=[idx4], writes=[idx4b])
                S.dma(POOL, lambda e: e.indirect_dma_start(
                    out=kidx_g[:, :], out_offset=None, in_=cache_ik[:, :],
                    in_offset=bass.IndirectOffsetOnAxis(ap=idx4b[:, :], axis=0),
                    bounds_check=cfg.NPHYS * 4 - 1, oob_is_err=False), reads=[idx4b], writes=[kidx_g])
                for jb in range(8):
                    pk = psL.next()

                    def em(e, pk=pk, jb=jb):
                        ins = None
                        for jj in range(4):
                            j = jb * 4 + jj
                            ins = e.transpose(out=pk[0:64, jj * 128:(jj + 1) * 128], in_=kidx_g[:, j * 64:(j + 1) * 64],
                                              identity=ident_f[:, :])
                        return ins
                    S.op(PE, em, reads=[kidx_g, ident_f], writes=[pk])
                    dstk = kidxTq[0:64, jb * 512:(jb + 1) * 512]
                    S.op(ACT, lambda e, pk=pk, dstk=dstk: e.copy(out=dstk, in_=pk[0:64, :]), reads=[pk], writes=[dstk])
                for c in range(8):
                    Pp = psL.next()
                    rk_ = kidxTq[0:64, c * 512:(c + 1) * 512]
                    S.op(PE, lambda e, Pp=Pp, rk_=rk_: e.matmul(Pp[0:16, :], lhsT=L.iqTs[0:64, 0:16], rhs=rk_, start=True, stop=True),
                         reads=[L.iqTs, rk_], writes=[Pp])
                    Rb = Rr.next()
                    S.op(ACT, lambda e, Pp=Pp, Rb=Rb: e.activation(out=Rb[0:16, :], in_=Pp[0:16, :], func=AF.Relu),
                         reads=[Pp], writes=[Rb])

                    def em2(e, Rb=Rb, c=c, q4=q4):
                        ins = None
                        for jj in range(4):
                            j = q4 * 32 + c * 4 + jj
                            ins = e.matmul(psG[:, j:j + 1], lhsT=Rb[0:16, jj * 128:(jj + 1) * 128], rhs=L.wcol[0:16, 0:1],
                                           start=True, stop=True)
                        return ins
                    S.op(PE, em2, reads=[Rb, L.wcol], writes=[psG])
            S.op(ACT, lambda e: e.copy(out=Sg[:, 0:128], in_=psG[:, 0:128]), reads=[psG], writes=[Sg])
            S.op(DVE, lambda e: e.memset(Sg[:, 128:130], -1e30), writes=[Sg])
            pself = psL.next()
            S.op(PE, lambda e: e.matmul(pself[0:16, 0:1], lhsT=L.iqTs[0:64, 0:16], rhs=L.ikTs[0:64, 0:1], start=True, stop=True),
                 reads=[L.iqTs, L.ikTs], writes=[pself])
            S.op(ACT, lambda e: e.activation(out=smallb[0:16, 0:1], in_=pself[0:16, 0:1], func=AF.Relu), reads=[pself], writes=[smallb])
            pself2 = psL.next()
            S.op(PE, lambda e: e.matmul(pself2[0:1, 0:1], lhsT=smallb[0:16, 0:1], rhs=L.wcol[0:16, 0:1], start=True, stop=True),
                 reads=[smallb, L.wcol], writes=[pself2])
            S.op(ACT, lambda e: e.copy(out=Sg[0:1, 128:129], in_=pself2[0:1, 0:1]), reads=[pself2], writes=[Sg])
            S.op(DVE, lambda e: e.tensor_reduce(out=small[:, 0:1], in_=Sg[:, 0:128], axis=AX.X, op=ALU.min), reads=[Sg], writes=[small])
            S.op(DVE, lambda e: e.tensor_reduce(out=small[:, 1:2], in_=Sg[:, 0:128], axis=AX.X, op=ALU.max, negate=True),
                 reads=[Sg], writes=[small])
            pmm = psL.next()
            S.op(PE, lambda e: e.transpose(out=pmm[0:2, 0:128], in_=small[:, 0:2], identity=ident_f[:, :]),
                 reads=[small, ident_f], writes=[pmm])
            S.op(DVE, lambda e: e.tensor_reduce(out=small[0:2, 2:3], in_=pmm[0:2, 0:128], axis=AX.X, op=ALU.min),
                 reads=[pmm], writes=[small])
            S.op(DVE, lambda e: e.tensor_scalar(out=small[0:2, 4:6], in0=ident_f[0:2, 0:2], scalar1=small[0:2, 2:3], scalar2=None,
                                                op0=ALU.mult), reads=[small, ident_f], writes=[small])
            pbc = psL.next()
            ones2 = sb("ones2", [2, 128], F32)
            S.op(DVE, lambda e: e.memset(ones2[:, :], 1.0), writes=[ones2])
            S.op(PE, lambda e: e.matmul(pbc[:, 0:2], lhsT=ones2[0:2, :], rhs=small[0:2, 4:6], start=True, stop=True),
                 reads=[ones2, small], writes=[pbc])
            S.op(DVE, lambda e: e.tensor_scalar(out=thr[:, 0:1], in0=pbc[:, 1:2], scalar1=-1.0, scalar2=None, op0=ALU.mult),
                 reads=[pbc], writes=["thr0"])
            S.op(DVE, lambda e: e.tensor_copy(out=thr[:, 1:2], in_=pbc[:, 0:1]), reads=[pbc], writes=["thr1"])

            def count_fn():
                S.op(DVE, lambda e: e.tensor_scalar(out=junkg[:, 0:129], in0=Sg[:, 0:129], scalar1=thr[:, 5:6], scalar2=0.0,
                                                    op0=ALU.is_ge, op1=ALU.add, accum_out=smallb[:, 2:3]),
                     reads=[Sg, "thr5"], writes=[junkg, smallb])
                pc = psL.next()
                S.op(PE, lambda e: e.matmul(pc[:, 0:1], lhsT=ones_b[:, :], rhs=smallb[:, 2:3], start=True, stop=True),
                     reads=[ones_b, smallb], writes=[pc])
                S.op(DVE, lambda e: e.tensor_copy(out=thr[:, 6:7], in_=pc[:, 0:1]), reads=[pc], writes=["thr6"])
            bisect(Sg, 128, count_fn, cfg.TOPK)
            S.op(DVE, lambda e: e.tensor_scalar(out=mk[:, 0:128], in0=Sg[:, 0:128], scalar1=thr[:, 3:4], scalar2=0.0,
                                                op0=ALU.is_ge, op1=ALU.add, accum_out=smallb[:, 4:5]),
                 reads=[Sg, "thr3"], writes=[mk, smallb])
            S.op(DVE, lambda e: e.tensor_tensor_scan(out=rsc[:, 0:128], data0=ones_f[:, :], data1=mk[:, 0:128], initial=0.0,
                                                     op0=ALU.mult, op1=ALU.add), reads=[mk, ones_f], writes=[rsc])
            poff = psL.next()

            def em_off(e):
                e.matmul(poff[:, 0:1], lhsT=lstr[:, :], rhs=smallb[:, 4:5], start=True, stop=True)
                return e.matmul(poff[:, 1:2], lhsT=ones_b[:, :], rhs=smallb[:, 4:5], start=True, stop=True)
            S.op(PE, em_off, reads=[lstr, ones_b, smallb], writes=[poff])
            S.op(DVE, lambda e: e.tensor_copy(out=small[:, 16:18], in_=poff[:, 0:2]), reads=[poff], writes=[small])
            S.op(DVE, lambda e: e.scalar_tensor_tensor(out=dest[:, 0:128], in0=rsc[:, 0:128], scalar=small[:, 16:17], in1=mk[:, 0:128],
                                                       op0=ALU.add, op1=ALU.mult), reads=[rsc, small, mk], writes=[dest])
            S.op(DVE, lambda e: e.tensor_scalar(out=dest[:, 0:128], in0=dest[:, 0:128], scalar1=-1.0, scalar2=None, op0=ALU.add),
                 reads=[dest], writes=[dest])
            S.op(DVE, lambda e: e.tensor_copy(out=rhs3[:, :, 0], in_=pts[:, 0:1].to_broadcast([128, 128])), reads=[pts], writes=[rhs3])
            S.op(DVE, lambda e: e.tensor_copy(out=rhs3[:, :, 1], in_=jcol[:, :]), reads=[jcol], writes=[rhs3])
            pslot = [psM[1], psL.next()]
            for j in range(128):
                E = Er.next()
                S.op(DVE, lambda e, E=E, j=j: e.tensor_scalar(out=E[:, 0:256], in0=iota_d[:, :], scalar1=dest[:, j:j + 1], scalar2=None,
                                                              op0=ALU.is_equal), reads=[iota_d, dest], writes=[E])

                def em3(e, E=E, j=j):
                    e.matmul(pslot[0][:, 0:2], lhsT=E[:, 0:128], rhs=rhs3[:, j, :], start=(j == 0), stop=(j == 127))
                    return e.matmul(pslot[1][:, 0:2], lhsT=E[:, 128:256], rhs=rhs3[:, j, :], start=(j == 0), stop=(j == 127))
                S.op(PE, em3, reads=[E, rhs3], writes=[pslot[0], pslot[1]])
            for tl in range(2):
                sl = small[:, 20 + 4 * tl:24 + 4 * tl]
                S.op(DVE, lambda e, sl=sl, tl=tl: e.tensor_copy(out=sl[:, 0:2], in_=pslot[tl][:, 0:2]), reads=[pslot[tl]], writes=[small])
                S.op(DVE, lambda e, sl=sl: e.scalar_tensor_tensor(out=sl[:, 3:4], in0=sl[:, 0:1], scalar=128.0, in1=sl[:, 1:2],
                                                                 op0=ALU.mult, op1=ALU.add), reads=[small], writes=[small])
                S.op(DVE, lambda e, sl=sl, tl=tl: e.tensor_copy(out=rowi[:, tl:tl + 1], in_=sl[:, 3:4]), reads=[small], writes=[rowi])
            S.op(DVE, lambda e: e.tensor_scalar(out=small[:, 30:32], in0=dslot[:, 0:2], scalar1=small[:, 17:18], scalar2=NEG,
                                                op0=ALU.is_ge, op1=ALU.mult), reads=[dslot, small], writes=[small])
            S.op(DVE, lambda e: e.tensor_scalar(out=small[0:1, 32:33], in0=Sg[0:1, 128:129], scalar1=thr[0:1, 3:4], scalar2=NEG,
                                                op0=ALU.is_lt, op1=ALU.mult), reads=[Sg, "thr3"], writes=[small])
            S.op(DVE, lambda e: e.tensor_copy(out=rowib[:, :], in_=rowi[:, :]), reads=[rowi], writes=[rowib])
            S.op(DVE, lambda e: e.memset(kg[:, :], 0.0), writes=[kg])
            S.op(DVE, lambda e: e.memset(vg[:, :], 0.0), writes=[vg])
            for tl in range(2):
                for (dstt, srcc) in ((kg, cache_k), (vg, cache_v)):
                    dv = dstt[:, tl * 512:(tl + 1) * 512]
                    S.dma(POOL, lambda e, dv=dv, srcc=srcc, tl=tl: e.indirect_dma_start(
                        out=dv, out_offset=None, in_=srcc[:, :],
                        in_offset=bass.IndirectOffsetOnAxis(ap=rowib[:, tl:tl + 1], axis=0),
                        bounds_check=cfg.NPHYS * 128 - 1, oob_is_err=False), reads=[rowib], writes=[dv])
            S.op(ACT, lambda e: e.copy(out=kgb[:, :], in_=kg[:, :]), reads=[kg], writes=[kgb])
            S.op(ACT, lambda e: e.copy(out=vgb[:, :], in_=vg[:, :]), reads=[vg], writes=[vgb])
            for tl in range(2):
                transpose_to(kgb[:, tl * 512:(tl + 1) * 512], 128, 512,
                             lambda j0, nj, tl=tl: kgT[:, tl * 512:(tl + 1) * 512].rearrange("p (g k) -> p g k", g=4)[:, j0:j0 + nj, :])
            pS = psL.next()
            for tl in range(2):
                def em4(e, tl=tl):
                    ins = None
                    for g in range(4):
                        ins = e.matmul(pS[:, tl * 16 + 4 * g:tl * 16 + 4 * g + 4],
                                       lhsT=kgT[:, tl * 512 + g * 128:tl * 512 + (g + 1) * 128],
                                       rhs=L.aqT[:, 4 * g:4 * g + 4, 0], start=True, stop=True)
                    return ins
                S.op(PE, em4, reads=[kgT, L.aqT], writes=[pS])
            pTd = smallb[:, 8:40]
            for tl in range(2):
                S.op(ACT, lambda e, tl=tl: e.activation(out=smallb[:, 8 + 16 * tl:24 + 16 * tl], in_=pS[:, 16 * tl:16 * tl + 16],
                                                        func=AF.Exp, bias=small[:, 30 + tl:31 + tl], scale=1.0),
                     reads=[pS, small], writes=[smallb])
            pss_ = psL.next()

            def em5(e):
                ins = None
                for g in range(4):
                    ins = e.matmul(pss_[0:1, 4 * g:4 * g + 4], lhsT=L.akTs[:, g:g + 1], rhs=L.aqT[:, 4 * g:4 * g + 4, 0],
                                   start=True, stop=True)
                return ins
            S.op(PE, em5, reads=[L.akTs, L.aqT], writes=[pss_])
            S.op(ACT, lambda e: e.activation(out=smallb[0:1, 40:56], in_=pss_[0:1, 0:16], func=AF.Exp,
                                             bias=small[0:1, 32:33], scale=1.0), reads=[pss_, small], writes=[smallb])
            po, pd = psM[0], psM[1]

            def em6(e):
                ins = None
                for g in range(4):
                    for tl in range(2):
                        e.matmul(po[:, 4 * g:4 * g + 4], lhsT=vgb[:, tl * 512 + g * 128:tl * 512 + (g + 1) * 128],
                                 rhs=smallb[:, 8 + 16 * tl + 4 * g:8 + 16 * tl + 4 * g + 4], start=(tl == 0), stop=False)
                    ins = e.matmul(po[:, 4 * g:4 * g + 4], lhsT=L.v_sb[0:1, g * 128:(g + 1) * 128],
                                   rhs=smallb[0:1, 40 + 4 * g:44 + 4 * g], start=False, stop=True)
                for tl in range(2):
                    e.matmul(pd[:, 0:16], lhsT=ones_b[:, :], rhs=smallb[:, 8 + 16 * tl:24 + 16 * tl], start=(tl == 0), stop=False)
                ins = e.matmul(pd[:, 0:16], lhsT=ones_b[0:1, :], rhs=smallb[0:1, 40:56], start=False, stop=True)
                return ins
            S.op(PE, em6, reads=[vgb, smallb, L.v_sb, ones_b], writes=[po, pd])
            S.op(DVE, lambda e: e.reciprocal(out=small[:, 40:56], in_=pd[:, 0:16]), reads=[pd], writes=[small])
            S.op(DVE, lambda e: e.tensor_tensor(out=L.attT[:, :, 0], in0=po[:, 0:16], in1=small[:, 40:56], op=ALU.mult),
                 reads=[po, small], writes=[L.attT[:, :, 0]])

        ones_f = sb("ones_f", [128, 128], F32)
        S.op(DVE, lambda e: e.memset(ones_f[:, :], 1.0), writes=[ones_f])

        def ones_f_ap():
            return ones_f[:, :]

        def run_block(src, dst, tok0, row0, T, is_sample, prefix=False):
            L = layout(T)
            tp, nt = L.tp, L.nt
            S.new_epoch()
            if is_sample:
                S.dma(SP, lambda e: e.dma_start(out=rs_p.rearrange("h d e -> d h e"), in_=state_f[:, :, :]), reads=SFK)
                S.dma(SP, lambda e: e.dma_start(out=state_f[:, :, :], in_=st_s.rearrange("h d e -> d h e")),
                      writes=SFK)
                for h in range(cfg.RH):
                    S.op(ACT, lambda e, h=h: e.copy(out=state_b[:, h, :], in_=state_f[:, h, :]),
                         reads=["state_f%d" % h], writes=["state_b%d" % h])
            for t in range(nt):
                xt = L.x_tm[:tp, t, :]
                S.dma(SP, lambda e, t=t, xt=xt: e.dma_start(out=xt, in_=src[row0 + t * 128:row0 + t * 128 + tp, :]), writes=[xt])
            ffn(L, g_f1, w_f1a, w_f1b, "f1")
            mixer(L, tok0, row0, is_sample, prefix)
            if prefix:
                return
            ffn(L, g_f2, w_f2a, w_f2b, "f2")
            for t in range(nt):
                xt = L.x_tm[:tp, t, :]
                S.dma(SP, lambda e, t=t, xt=xt: e.dma_start(out=dst[row0 + t * 128:row0 + t * 128 + tp, :], in_=xt), reads=[xt])

        for pb in range(NPRE // TB):
            run_block(xpre, None, pb * TB, pb * TB, TB, False, prefix=True)
        sfv = state_f[:, :, :].rearrange("p h e -> p (h e)")
        S.op(DVE, lambda e: e.tensor_scalar(out=sfv, in0=sfv, scalar1=flag_sb[:, 0:1], scalar2=None, op0=ALU.mult),
             reads=SFK + [flag_sb], writes=SFK)
        for h in range(cfg.RH):
            S.op(ACT, lambda e, h=h: e.copy(out=state_b[:, h, :], in_=state_f[:, h, :]),
                 reads=["state_f%d" % h], writes=["state_b%d" % h])
        for b in range(NOWN // TB):
            run_block(xo, y_p, NPRE + b * TB, b * TB, TB, False)
        if cfg.WITH_SAMPLE:
            run_block(xs, y_s, 0, 0, 1, True)
        else:
            S.dma(SP, lambda e: e.dma_start(out=rs_p.rearrange("h d e -> d h e"), in_=state_f[:, :, :]), reads=SFK)
        S.finish()
        S.emit(nc)
    return nc


def _consts(cfg, hf):
    SEQ = cfg.SEQ
    half = 64
    inv = (10000.0 ** (-np.arange(half, dtype=np.float32) / half)).astype(np.float32)
    pos = np.concatenate([np.arange(SEQ // 2), hf * (SEQ // 2) + np.arange(SEQ // 2), [cfg.NPAGES * 128]]).astype(np.float32)
    ang = pos[:, None] * inv[None, :]
    cos, sin = np.cos(ang).astype(np.float32), np.sin(ang).astype(np.float32)
    log_g = np.log1p(-np.exp2(-5.0 - np.arange(cfg.RH, dtype=np.float32))).astype(np.float32)
    i_in = np.concatenate([np.arange(SEQ) % 128, [0]]).astype(np.float32)
    dq = np.exp(log_g[None, :] * (i_in[:, None] + 1.0)).astype(np.float32)
    dk = (np.exp(-log_g[None, :] * (i_in[:, None] + 1.0)) * (128 ** -0.5)).astype(np.float32)
    rope = np.stack([cos[:, None, :] * dq[:, :, None], sin[:, None, :] * dq[:, :, None],
                     cos[:, None, :] * dk[:, :, None], sin[:, None, :] * dk[:, :, None]], axis=1)
    j = np.arange(128)
    return {
        "c_rope": np.ascontiguousarray(rope.astype(np.float32)),
        "c_ident": np.eye(128, dtype=np.float32),
        "c_caus": (j[:, None] <= j[None, :]).astype(np.float32),
        "c_negu": np.where(j[None, :] > j[:, None], -1e30, 0.0).astype(np.float32),
        "c_iota": np.broadcast_to(np.arange(256, dtype=np.float32)[None, :], (128, 256)).copy(),
        "c_lstr": (j[:, None] < j[None, :]).astype(np.float32),
        "c_jcol": np.broadcast_to(j.astype(np.float32)[None, :], (128, 128)).copy(),
        "c_dslot": np.stack([j, j + 128], axis=1).astype(np.float32),
        "c_pow2": np.broadcast_to((2.0 ** -np.arange(32, dtype=np.float32))[None, :], (128, 32)).copy(),
    }


def kernel(x_prompt, x_sample, state_ret, cache_k, cache_v, cache_idx_k, page_table,
           ffn1_norm, ffn1_w1, ffn1_w2, mix_norm, w_in, q_norm, k_norm, ret_norm,
           w_ret_out, w_att_out, w_o, ffn2_norm, ffn2_w1, ffn2_w2, _cfg=None):
    cfg = _cfg or Cfg
    f = lambda a: np.ascontiguousarray(np.asarray(a))
    nc = build_program(cfg)
    consts = [_consts(cfg, 0), _consts(cfg, 1)]
    HS = cfg.SEQ // 2
    B = x_prompt.shape[0]
    NCO = cfg.NCORES
    shared = {
        "ffn1_w1": f(ffn1_w1[0]), "ffn1_w2": f(ffn1_w2[0]), "ffn2_w1": f(ffn2_w1[0]), "ffn2_w2": f(ffn2_w2[0]),
        "w_in": f(w_in[0]), "w_ret_out": f(w_ret_out[0]), "w_att_out": f(w_att_out[0]), "w_o": f(w_o[0]),
        "ffn1_norm": f(ffn1_norm), "mix_norm": f(mix_norm), "ffn2_norm": f(ffn2_norm),
        "q_norm": f(q_norm), "k_norm": f(k_norm), "ret_norm": f(ret_norm),
    }
    if cfg.WITH_SAMPLE:
        shared["cache_k"] = f(cache_k[0]).reshape(cfg.NPHYS * 128, 512)
        shared["cache_v"] = f(cache_v[0]).reshape(cfg.NPHYS * 128, 512)
        shared["cache_ik"] = f(cache_idx_k[0]).reshape(cfg.NPHYS * 4, 2048)
    in_maps = []
    for c in range(NCO):
        m = dict(shared)
        b_, hf = (c // 2) % B, c % 2
        m.update(consts[hf])
        m["xo"] = f(x_prompt[b_, hf * HS:(hf + 1) * HS])
        m["xpre"] = f(x_prompt[b_, 0:HS])
        m["flag"] = np.full((128, 1), float(hf), np.float32)
        m["negflag"] = np.full((128, 1), 0.0 if hf else -1e30, np.float32)
        m["xs"] = f(x_sample[c % 8])
        m["st_s"] = f(state_ret[0, c % 8])
        if cfg.WITH_SAMPLE:
            m["ptab"] = f(page_table[c % 8]).reshape(128, 1).astype(np.int32)
        in_maps.append(m)
    res = run_bass_kernel_spmd(nc, in_maps, core_ids=list(range(NCO)))
    r = res.results
    cs = lambda c: r[c % NCO]
    cat = lambda b, k: np.concatenate([cs(2 * b)[k], cs(2 * b + 1)[k]], axis=0)
    y_prompt = np.stack([cat(b, "y_p") for b in range(B)])
    y_sample = np.stack([cs(c)["y_s"] for c in range(8)])
    rsp = np.stack([cs(2 * b + 1)["rs_p"] for b in range(B)])[None]
    kp = np.stack([cat(b, "k_p") for b in range(B)]).reshape(1, B, cfg.SEQ, 4, 128)
    vp = np.stack([cat(b, "v_p") for b in range(B)]).reshape(1, B, cfg.SEQ, 4, 128)
    ikp = np.stack([cat(b, "ik_p") for b in range(B)])[None]
    rss = np.stack([cs(c)["rs_s"] for c in range(8)])[None]
    ks = np.stack([cs(c)["k_s"] for c in range(8)]).reshape(1, 8, 1, 4, 128)
    vs = np.stack([cs(c)["v_s"] for c in range(8)]).reshape(1, 8, 1, 4, 128)
    iks = np.stack([cs(c)["ik_s"] for c in range(8)]).reshape(1, 8, 1, 64)
    return (y_prompt, y_sample, rsp, kp, vp, ikp, rss, ks, vs, iks)
```

```python
import contextlib
import numpy as np
import concourse.bass as bass
import concourse.mybir as mybir
from concourse.bass_utils import run_bass_kernel_spmd

F32 = mybir.dt.float32
BF16 = mybir.dt.bfloat16
I32 = mybir.dt.int32
AF = mybir.ActivationFunctionType
ALU = mybir.AluOpType
AX = mybir.AxisListType

PE, ACT, DVE, POOL, SP = "pe", "act", "dve", "pool", "sp"


class Cfg:
    D = 2048
    DFF = 5632
    SEQ = 2048
    TB = 512
    NPAGES = 128
    NPHYS = 1280
    RH = 8
    EPS = 1e-6
    TOPK = 256
    NIT = 20
    WITH_SAMPLE = True
    NCORES = 8
    STAGE = 99


O_RQ, O_RK, O_RV, O_RG = 0, 1024, 2048, 4096
O_AQ, O_AK, O_AV = 6144, 8192, 8704
O_IQ, O_IK, O_IW = 9216, 10240, 10304
O_GR, O_GA = 10320, 12368
IN_W = 14416
IDX_W_SCALE = (16 ** -0.5) * (64 ** -0.5)
NEG = -30000.0
ARENA = 47104
GRAN = 512


class Sched:
    NDMA = 24

    def __init__(self):
        self.ops = {e: [] for e in (PE, ACT, DVE, POOL, SP)}
        self.epoch = 0
        self.count = {}
        self.waited = {e: {} for e in (PE, ACT, DVE, POOL, SP)}
        self.last_w = {}
        self.readers = {}
        self.dma_i = 0
        self.dma_cnt = [0] * self.NDMA
        self.semkeys = []
        self.keyfn = None

    def new_epoch(self):
        self.epoch += 1

    def _key(self, eng):
        k = (eng, self.epoch)
        if k not in self.count:
            self.count[k] = 0
            self.semkeys.append(k)
        return k

    def _norm(self, items):
        out = []
        for it in items:
            if isinstance(it, (str, tuple)):
                out.append(it)
            elif isinstance(it, list):
                out.extend(self._norm(it))
            else:
                out.extend(self.keyfn(it))
        return out

    def _deps(self, reads, writes):
        deps = {}
        for r in reads:
            if r in self.last_w:
                sk, v = self.last_w[r]
                deps[sk] = max(deps.get(sk, 0), v)
        for w in writes:
            if w in self.last_w:
                sk, v = self.last_w[w]
                deps[sk] = max(deps.get(sk, 0), v)
            for sk, v in self.readers.get(w, ()):
                deps[sk] = max(deps.get(sk, 0), v)
        return deps

    def _waits(self, eng, deps, skip=None):
        waits = []
        for sk, v in deps.items():
            if skip is not None and sk == skip:
                continue
            if self.waited[eng].get(sk, 0) >= v:
                continue
            self.waited[eng][sk] = v
            waits.append((sk, v))
        return waits

    def _commit(self, me, reads, writes):
        for r in reads:
            lst = self.readers.setdefault(r, [])
            lst[:] = [x for x in lst if x[0] != me[0]] + [me]
        for w in writes:
            self.last_w[w] = me
            self.readers[w] = []

    def op(self, eng, emit, reads=(), writes=()):
        reads, writes = self._norm(reads), self._norm(writes)
        sk = self._key(eng)
        deps = self._deps(reads, writes)
        waits = self._waits(eng, deps, skip=sk if eng == PE else None)
        self.count[sk] += 1
        me = (sk, self.count[sk])
        self.ops[eng].append((waits, emit, (sk, 1), self.count[sk]))
        self._commit(me, reads, writes)
        return me

    def dma(self, eng, emit, reads=(), writes=()):
        reads, writes = self._norm(reads), self._norm(writes)
        i = self.dma_i % self.NDMA
        self.dma_i += 1
        sk = ("dma", i)
        if sk not in self.semkeys:
            self.semkeys.append(sk)
        deps = self._deps(reads, writes)
        if self.dma_cnt[i] > 0:
            deps[sk] = max(deps.get(sk, 0), 16 * self.dma_cnt[i])
        waits = self._waits(eng, deps)
        self.dma_cnt[i] += 1
        me = (sk, 16 * self.dma_cnt[i])
        self.ops[eng].append((waits, emit, (sk, 16), None))
        self._commit(me, reads, writes)
        return me

    def finish(self, eng=SP):
        deps = {("dma", i): 16 * c for i, c in enumerate(self.dma_cnt) if c > 0}
        waits = self._waits(eng, deps)
        self.ops[eng].append((waits, None, None, None))

    def emit(self, nc):
        sems = {}
        needed = {}
        for eng_ops in self.ops.values():
            for waits, _, _, _ in eng_ops:
                for sk, v in waits:
                    if sk[0] != "dma":
                        needed.setdefault(sk, set()).add(v)
        rank = {sk: {v: i + 1 for i, v in enumerate(sorted(vs))} for sk, vs in needed.items()}
        with contextlib.ExitStack() as st:
            for i, sk in enumerate(self.semkeys):
                sems[sk] = st.enter_context(nc.semaphore("s%d" % i))
            block = st.enter_context(nc.Block())

            def run(eng_name):
                def f(e):
                    for waits, emit, inc, idx in self.ops[eng_name]:
                        for sk, v in waits:
                            e.wait_ge(sems[sk], v if sk[0] == "dma" else rank[sk][v])
                        if emit is None:
                            continue
                        ins = emit(e)
                        if idx is None:
                            ins.then_inc(sems[inc[0]], inc[1])
                        elif idx in needed.get(inc[0], ()):
                            ins.then_inc(sems[inc[0]], 1)
                return f

            block.tensor(run(PE))
            block.scalar(run(ACT))
            block.vector(run(DVE))
            block.gpsimd(run(POOL))
            block.sync(run(SP))


class Ring:
    def __init__(self, items):
        self.items = items
        self.i = 0

    def next(self):
        it = self.items[self.i % len(self.items)]
        self.i += 1
        return it


def build_program(cfg):
    D, DFF, SEQ, TB = cfg.D, cfg.DFF, cfg.SEQ, cfg.TB
    KT = D // 128
    KTF = DFF // 128
    NTB = TB // 128
    NKEY = SEQ
    NKB = NKEY // 128
    nc = bass.Bass("TRN2", target_bir_lowering=False)
    S = Sched()

    def din(name, shape, dt=F32):
        return nc.dram_tensor(name, list(shape), dt, kind="ExternalInput").ap()

    def dout(name, shape, dt=F32):
        return nc.dram_tensor(name, list(shape), dt, kind="ExternalOutput").ap()

    NPRE = SEQ // 2
    NOWN = SEQ // 2
    xo = din("xo", [NOWN, D])
    xpre = din("xpre", [NPRE, D])
    flag_d = din("flag", [128, 1])
    negflag_d = din("negflag", [128, 1])
    xs = din("xs", [1, D])
    st_s = din("st_s", [cfg.RH, 128, 256])
    if cfg.WITH_SAMPLE:
        cache_k = din("cache_k", [cfg.NPHYS * 128, 512])
        cache_v = din("cache_v", [cfg.NPHYS * 128, 512])
        cache_ik = din("cache_ik", [cfg.NPHYS * 4, 2048])
        ptab = din("ptab", [128, 1], I32)
    w_f1a = din("ffn1_w1", [D, 2 * DFF])
    w_f1b = din("ffn1_w2", [DFF, D])
    w_f2a = din("ffn2_w1", [D, 2 * DFF])
    w_f2b = din("ffn2_w2", [DFF, D])
    w_in = din("w_in", [D, IN_W])
    w_ro = din("w_ret_out", [D, D])
    w_ao = din("w_att_out", [D, D])
    w_o = din("w_o", [D, D])
    g_f1 = din("ffn1_norm", [1, D])
    g_mx = din("mix_norm", [1, D])
    g_f2 = din("ffn2_norm", [1, D])
    g_q = din("q_norm", [1, 128])
    g_k = din("k_norm", [1, 128])
    g_r = din("ret_norm", [1, 256])
    c_rope = din("c_rope", [SEQ + 1, 4, cfg.RH, 64])
    c_ident = din("c_ident", [128, 128])
    c_caus = din("c_caus", [128, 128])
    c_negu = din("c_negu", [128, 128])
    c_iota = din("c_iota", [128, 256])
    c_lstr = din("c_lstr", [128, 128])
    c_jcol = din("c_jcol", [128, 128])
    c_dslot = din("c_dslot", [128, 2])
    c_pow2 = din("c_pow2", [128, 32])

    y_p = dout("y_p", [NOWN, D])
    y_s = dout("y_s", [1, D])
    rs_p = dout("rs_p", [cfg.RH, 128, 256])
    k_p = dout("k_p", [NOWN, 512])
    v_p = dout("v_p", [NOWN, 512])
    ik_p = dout("ik_p", [NOWN, 64])
    rs_s = dout("rs_s", [cfg.RH, 128, 256])
    k_s = dout("k_s", [1, 512])
    v_s = dout("v_s", [1, 512])
    ik_s = dout("ik_s", [1, 64])
    sc_x = nc.dram_tensor("sc_x", [TB, D], F32, kind="Internal").ap()
    sc_gr = nc.dram_tensor("sc_gr", [TB, D], BF16, kind="Internal").ap()
    sc_ga = nc.dram_tensor("sc_ga", [TB, D], BF16, kind="Internal").ap()
    NSLOT = 128
    wcache = nc.dram_tensor("wcache", [NSLOT, 128, 8192], BF16, kind="Internal").ap()
    wslots = {}

    with contextlib.ExitStack() as ctx:
        def sb(name, shape, dt):
            return ctx.enter_context(nc.sbuf_tensor(name, list(shape), dt))

        def pt(name, shape, dt):
            return ctx.enter_context(nc.psum_tensor(name, list(shape), dt))

        arena = sb("arena", [128, ARENA], BF16)

        def keyfn(ap):
            t_ = getattr(ap, "tensor", None)
            name = t_.name if t_ is not None else ap.name
            if name != "arena":
                return [name]
            esz = 2 if ap.dtype == BF16 else 4
            rowlen = (ARENA * 2) // esz
            off = int(ap.offset) % rowlen
            ext = sum((n - 1) * abs(s) for s, n in list(ap.ap)[1:]) + 1
            lo = (off * esz) // (2 * GRAN)
            hi = ((off + ext) * esz - 1) // (2 * GRAN)
            return [("A", g) for g in range(lo, hi + 1)]

        S.keyfn = keyfn

        def AV(off, n, dt=BF16):
            assert off % 2 == 0 and off + n <= ARENA, (off, n)
            v = arena[:, off:off + n]
            return v if dt == BF16 else v.bitcast(dt)

        ident_f = sb("ident_f", [128, 128], F32)
        ident_b = sb("ident_b", [128, 128], BF16)
        ones_b = sb("ones_b", [128, 128], BF16)
        caus = sb("caus", [128, 128], F32)
        negu = sb("negu", [128, 128], F32)
        gq_bc = sb("gq_bc", [128, 128], F32)
        gk_bc = sb("gk_bc", [128, 128], F32)
        gr_bc = sb("gr_bc", [128, 256], F32)
        kT = sb("kT", [128, 4, NKEY], BF16)
        Vc = sb("Vc", [128, NKB, 512], BF16)
        ikT2 = sb("ikT2", [128, NKEY], BF16)
        state_f = sb("state_f", [128, cfg.RH, 256], F32)
        state_b = sb("state_b", [128, cfg.RH, 256], BF16)
        wsc = sb("wsc", [128, NTB, 16], F32)
        thr = sb("thr", [128, 8], F32)
        pow2 = sb("pow2", [128, 32], F32)
        w_all = sb("w_all", [128, 32], F32)
        wring = Ring([sb("wb%d" % i, [128, 8192], BF16) for i in range(3)])
        f32s_ring = Ring([sb("fs%d" % i, [128, 512], F32) for i in range(3)])
        bfs_ring = Ring([sb("bs%d" % i, [128, 512], BF16) for i in range(3)])
        stat = sb("stat", [128, 64], F32)
        stat_i = [0]

        psL = Ring([pt("psL%d" % i, [128, 512], F32) for i in range(4)])
        psT = Ring([pt("psT%d" % i, [128, 1024], BF16) for i in range(2)])
        psM = [pt("psM%d" % i, [128, 512], F32) for i in range(2)]

        def stat_cols(n):
            if stat_i[0] + n > 64:
                stat_i[0] = 0
            a = stat_i[0]
            stat_i[0] += n
            return stat[:, a:a + n], ["stat%d" % i for i in range(a // 8, (a + n - 1) // 8 + 1)]

        deferred = []

        def flush_deferred(keep=0):
            while len(deferred) > keep:
                deferred.pop(0)()

        def cload(dst, src, eng=SP):
            S.dma(eng, lambda e: e.dma_start(out=dst, in_=src), writes=[dst])

        flagt = sb("flagt", [128, 16], F32)
        flag_sb = flagt[:, 0:1]
        negflag_sb = flagt[:, 8:9]
        cload(flag_sb, flag_d)
        cload(negflag_sb, negflag_d)
        cload(ident_f[:, :], c_ident)
        cload(pow2[:, :], c_pow2)
        cload(caus[:, :], c_caus)
        cload(negu[:, :], c_negu)
        cload(gk_bc[:, :], g_k.partition_broadcast(128))
        cload(gq_bc[:, :], g_q.partition_broadcast(128))
        cload(gr_bc[:, :], g_r.partition_broadcast(128))
        S.op(DVE, lambda e: e.tensor_copy(out=ident_b[:, :], in_=ident_f[:, :]), reads=[ident_f], writes=[ident_b])
        S.op(DVE, lambda e: e.memset(ones_b[:, :], 1.0), writes=[ones_b])
        S.op(DVE, lambda e: e.tensor_scalar(out=gq_bc[:, :], in0=gq_bc[:, :], scalar1=128 ** -0.5, scalar2=None,
                                            op0=ALU.mult), reads=[gq_bc], writes=[gq_bc])
        SFK = ["state_f%d" % h for h in range(cfg.RH)]
        SBK = ["state_b%d" % h for h in range(cfg.RH)]
        S.op(DVE, lambda e: e.memset(state_f[:, :, :], 0.0), writes=SFK)
        S.op(DVE, lambda e: e.memset(state_b[:, :, :], 0.0), writes=SBK)

        def rstd_from_ss(ss_ap, ss_keys, n, tp, inv_n):
            ms, msk = stat_cols(n)
            sd, sdk = stat_cols(n)
            rs, rsk = stat_cols(n)
            S.op(DVE, lambda e: e.tensor_scalar(out=ms[:tp], in0=ss_ap, scalar1=inv_n, scalar2=cfg.EPS,
                                                op0=ALU.mult, op1=ALU.add), reads=ss_keys, writes=msk)
            S.op(ACT, lambda e: e.activation(out=sd[:tp], in_=ms[:tp], func=AF.Sqrt), reads=msk, writes=sdk)
            S.op(DVE, lambda e: e.reciprocal(out=rs[:tp], in_=sd[:tp]), reads=sdk, writes=rsk)
            return rs, rsk

        def transpose_to(src_ap, tp, ncols, dst_fn):
            nj_all = ncols // 128
            j0 = 0
            while j0 < nj_all:
                nj = min(8, nj_all - j0)
                ps = psT.next()

                def emit_t(e, ps=ps, j0=j0, nj=nj):
                    ins = None
                    for j in range(nj):
                        ins = e.transpose(out=ps[:, j * 128:j * 128 + tp],
                                          in_=src_ap[:tp, (j0 + j) * 128:(j0 + j + 1) * 128],
                                          identity=ident_b[:tp, :tp])
                    return ins
                S.op(PE, emit_t, reads=[src_ap[:tp, :], ident_b], writes=[ps])
                dst = dst_fn(j0, nj)
                src_ps = ps[:, 0:nj * 128].rearrange("p (j t) -> p j t", j=nj)[:, :, 0:tp]
                S.op(ACT, lambda e, dst=dst, src_ps=src_ps: e.copy(out=dst, in_=src_ps), reads=[ps], writes=[dst])
                j0 += nj

        class Lay:
            pass

        def layout(T):
            L = Lay()
            tp = min(T, 128)
            nt = (T + 127) // 128
            L.tp, L.nt, L.T = tp, nt, T
            if T > 1:
                X0, A0, H0 = 0, 16384, 24576
                L.x_tm = AV(X0, 16384, F32).rearrange("p (t d) -> p t d", t=NTB)
                L.actT = AV(A0, 8192).rearrange("p (k t) -> p k t", k=KT)
                L.hT = AV(H0, KTF * TB).rearrange("p (k t) -> p k t", k=KTF)
                L.gain_bc = AV(H0, 2 * D, F32)
                L.xn = AV(H0 + 2 * D, D)
                L.aqT = AV(0, 8192).rearrange("p (k t) -> p k t", k=16)
                L.iqT = AV(8192, 4096).rearrange("p (k t) -> p k t", k=8)
                L.diagW = AV(12288, 2048).rearrange("p (h t) -> p h t", h=16)
                L.mb = AV(14336, 2048)
                L.attT = AV(H0, 8192).rearrange("p (k t) -> p k t", k=16)
                L.S_sb = AV(32768, 4096, F32)
                L.junk = AV(36864, 2048)
                L.Rring = Ring([AV(38912 + 512 * i, 512) for i in range(4)])
                L.pTring = Ring([AV(40960 + 512 * i, 512) for i in range(3)])
                L.rden = AV(42496, 1024, F32)
                L.mb2 = AV(43520, 2048)
                L.ogT = AV(32768, 8192).rearrange("p (k t) -> p k t", k=16)
                L.qrT = AV(0, 2048).rearrange("p (h t) -> p h t", h=4)
                L.krT = AV(2048, 2048).rearrange("p (h t) -> p h t", h=4)
                L.ktm = AV(4096, 2048).rearrange("p (t c) -> p t c", t=NTB)
                L.vtm = AV(6144, 4096).rearrange("p (t c) -> p t c", t=NTB)
                L.sgtm = AV(10240, 4096).rearrange("p (t c) -> p t c", t=NTB)
                L.rope = Ring([AV(14336 + 1024 * i, 1024, F32).rearrange("p (a h f) -> p a h f", a=2, h=4) for i in range(2)])
                L.o_raw = AV(40960, 2048, F32).rearrange("p (h e) -> p h e", h=4)
                L.G = AV(43008, 1024)
                L.og_tm = AV(44032, 1024)
                L.sTm = Ring([AV(45056 + 128 * i, 128) for i in range(4)])
                L.m_tm = AV(0, 8192).rearrange("p (t d) -> p t d", t=NTB)
                L.gtr = Ring([AV(8192 + 512 * i, 512) for i in range(3)])
            else:
                b = [20480]

                def al(n):
                    o = b[0]
                    b[0] += (n + 63) // 64 * 64
                    return o
                L.x_tm = AV(al(4096), 4096, F32).rearrange("p (t d) -> p t d", t=1)
                L.gain_bc = AV(0, 2 * D, F32)
                L.xn = AV(2 * D, D)
                L.actT = AV(al(16), 16).rearrange("p (k t) -> p k t", k=KT)
                L.hT = AV(al(KTF), KTF).rearrange("p (k t) -> p k t", k=KTF)
                L.aqT = AV(al(16), 16).rearrange("p (k t) -> p k t", k=16)
                L.attT = AV(al(16), 16).rearrange("p (k t) -> p k t", k=16)
                L.ogT = AV(al(16), 16).rearrange("p (k t) -> p k t", k=16)
                L.qrT = AV(al(4), 4).rearrange("p (h t) -> p h t", h=4)
                L.krT = AV(al(4), 4).rearrange("p (h t) -> p h t", h=4)
                L.ktm = AV(al(512), 512).rearrange("p (t c) -> p t c", t=1)
                L.vtm = AV(al(1024), 1024).rearrange("p (t c) -> p t c", t=1)
                L.sgtm = AV(al(1024), 1024).rearrange("p (t c) -> p t c", t=1)
                L.rope = Ring([AV(al(1024), 1024, F32).rearrange("p (a h f) -> p a h f", a=2, h=4) for i in range(2)])
                L.o_raw = AV(al(2048), 2048, F32).rearrange("p (h e) -> p h e", h=4)
                L.G = AV(al(1024), 1024)
                L.og_tm = AV(al(1024), 1024)
                L.sTm = Ring([AV(al(128), 128) for i in range(4)])
                L.m_tm = AV(al(2048), 2048).rearrange("p (t d) -> p t d", t=1)
                L.gtr = Ring([AV(al(512), 512) for i in range(3)])
                L.iqTs = AV(al(16), 16)
                L.ikTs = AV(al(2), 2)
                L.wcol = AV(al(2), 2)
                L.akTs = AV(al(4), 4)
                L.kn_s = AV(al(512), 512)
                L.v_sb = AV(al(512), 512)
                L.ik_sb = AV(al(64), 64)
                L.iw_s = AV(al(32), 32, F32)
                assert b[0] <= ARENA
            return L

        def rmsnorm_transpose(L, gain_dram):
            tp, nt = L.tp, L.nt
            gain_bc = L.gain_bc
            S.dma(SP, lambda e: e.dma_start(out=gain_bc[:, :], in_=gain_dram.partition_broadcast(128)),
                  writes=[gain_bc])
            for t in range(nt):
                xn = L.xn
                ss, ssk = stat_cols(1)
                xt = L.x_tm[:tp, t, :]
                S.op(DVE, lambda e, xt=xt, xn=xn, ss=ss: e.scalar_tensor_tensor(
                    out=xn[:tp, :], in0=xt, scalar=1.0, in1=xt, op0=ALU.mult, op1=ALU.mult, accum_out=ss[:tp]),
                    reads=[xt], writes=[xn] + ssk)
                rs, rsk = rstd_from_ss(ss[:tp], ssk, 1, tp, 1.0 / D)
                S.op(DVE, lambda e, xt=xt, xn=xn, rs=rs: e.scalar_tensor_tensor(
                    out=xn[:tp, :], in0=xt, scalar=rs[:tp], in1=gain_bc[:tp, :], op0=ALU.mult, op1=ALU.mult),
                    reads=[xt, gain_bc] + rsk, writes=[xn])
                transpose_to(xn, tp, D, lambda j0, nj, t=t: L.actT[:, j0:j0 + nj, t * 128:t * 128 + tp])

        def linear(L, act_ap, kt, w_dram, chunks, consumer, wkey):
            tp, nt = L.tp, L.nt
            wv = w_dram.rearrange("(k p) n -> p k n", p=128)
            for ci, blocks in enumerate(chunks):
                ncols = sum(n for _, n in blocks)
                kk_max = 8192 // ncols
                ksplits = [(k0, min(kk_max, kt - k0)) for k0 in range(0, kt, kk_max)]
                pss = [psL.next() for _ in range(nt)] if len(ksplits) > 1 else None
                for si, (k0, kk) in enumerate(ksplits):
                    wb = getattr(L, "wring", wring).next()
                    wbv = wb[:, 0:kk * ncols].rearrange("p (k n) -> p k n", k=kk)
                    skey = (wkey, ci, si)
                    nel = kk * ncols
                    if skey not in wslots:
                        slot = len(wslots)
                        assert slot < NSLOT
                        wslots[skey] = slot
                        off = 0
                        for (c0, n) in blocks:
                            S.dma(POOL, lambda e, wbv=wbv, off=off, n=n, c0=c0, k0=k0, kk=kk: e.dma_start(
                                out=wbv[:, :, off:off + n], in_=wv[:, k0:k0 + kk, c0:c0 + n]), writes=[wb])
                            off += n
                        S.dma(SP, lambda e, wb=wb, slot=slot, nel=nel: e.dma_start(
                            out=wcache[slot, :, 0:nel], in_=wb[:, 0:nel]), reads=[wb], writes=["wc%d" % slot])
                    else:
                        slot = wslots[skey]
                        S.dma(POOL, lambda e, wb=wb, slot=slot, nel=nel: e.dma_start(
                            out=wb[:, 0:nel], in_=wcache[slot, :, 0:nel]), reads=["wc%d" % slot], writes=[wb])
                    for t in range(nt):
                        flush_deferred(keep=2)
                        ps = psL.next() if pss is None else pss[t]

                        def emit_mm(e, ps=ps, t=t, k0=k0, kk=kk, wbv=wbv, ncols=ncols, si=si):
                            ins = None
                            for k in range(kk):
                                ins = e.matmul(ps[:tp, 0:ncols],
                                               lhsT=act_ap[:, k0 + k, t * 128:t * 128 + tp],
                                               rhs=wbv[:, k, :],
                                               start=(si == 0 and k == 0),
                                               stop=(si == len(ksplits) - 1 and k == kk - 1))
                            return ins
                        S.op(PE, emit_mm, reads=[wb, act_ap[:, :, t * 128:t * 128 + tp]], writes=[ps])
                        if si == len(ksplits) - 1:
                            consumer(ci, t, ps[:tp, 0:ncols], ps)
            flush_deferred(0)

        def ffn(L, gain_dram, w1, w2, wk):
            tp = L.tp
            rmsnorm_transpose(L, gain_dram)
            nch = DFF // 256

            def cons_h(ci, t, ps, pst):
                sg = f32s_ring.next()
                hb = bfs_ring.next()
                S.op(ACT, lambda e: e.activation(out=sg[:tp, 0:256], in_=ps[:, 0:256], func=AF.Silu),
                     reads=[pst], writes=[sg])
                S.op(DVE, lambda e: e.tensor_tensor(out=hb[:tp, 0:256], in0=ps[:, 256:512], in1=sg[:tp, 0:256],
                                                    op=ALU.mult), reads=[pst, sg], writes=[hb])
                deferred.append(lambda: transpose_to(
                    hb, tp, 256, lambda j0, nj: L.hT[:, 2 * ci + j0:2 * ci + j0 + nj, t * 128:t * 128 + tp]))

            linear(L, L.actT, KT, w1, [[(s * 256, 256), (DFF + s * 256, 256)] for s in range(nch)], cons_h, wk + "a")

            def cons_y(ci, t, ps, pst):
                xs_ = L.x_tm[:tp, t, ci * 256:(ci + 1) * 256]
                S.op(DVE, lambda e: e.scalar_tensor_tensor(out=xs_, in0=ps, scalar=0.5, in1=xs_,
                                                           op0=ALU.mult, op1=ALU.add),
                     reads=[pst, xs_], writes=[xs_])

            linear(L, L.hT, KTF, w2, [[(n * 256, 256)] for n in range(D // 256)], cons_y, wk + "b")

        def headnorm(tp, ps, pst, gbc):
            raw = f32s_ring.next()
            sq = f32s_ring.next()
            S.op(ACT, lambda e: e.copy(out=raw[:tp, :], in_=ps), reads=[pst], writes=[raw])
            S.op(DVE, lambda e: e.tensor_tensor(out=sq[:tp, :], in0=raw[:tp, :], in1=raw[:tp, :], op=ALU.mult),
                 reads=[raw], writes=[sq])
            ss, ssk = stat_cols(4)
            S.op(DVE, lambda e: e.tensor_reduce(out=ss[:tp], in_=sq[:tp, :].rearrange("p (h d) -> p h d", h=4),
                                                axis=AX.X, op=ALU.add), reads=[sq], writes=ssk)
            rs, rsk = rstd_from_ss(ss[:tp], ssk, 4, tp, 1.0 / 128)
            S.op(DVE, lambda e: e.tensor_tensor(
                out=sq[:tp, :].rearrange("p (h d) -> p h d", h=4),
                in0=raw[:tp, :].rearrange("p (h d) -> p h d", h=4),
                in1=rs[:tp].unsqueeze(2).to_broadcast([tp, 4, 128]), op=ALU.mult),
                reads=[raw] + rsk, writes=[sq])
            S.op(DVE, lambda e: e.tensor_tensor(
                out=raw[:tp, :].rearrange("p (h d) -> p h d", h=4),
                in0=sq[:tp, :].rearrange("p (h d) -> p h d", h=4),
                in1=gbc[:tp, :].unsqueeze(1).to_broadcast([tp, 4, 128]), op=ALU.mult),
                reads=[sq, gbc], writes=[raw])
            return raw

        def mixer(L, tok0, row0, is_sample, prefix=False):
            tp, nt, T = L.tp, L.nt, L.T
            rmsnorm_transpose(L, g_mx)
            for t in range(nt if not prefix else 0):
                xt = L.x_tm[:tp, t, :]
                S.dma(SP, lambda e, t=t, xt=xt: e.dma_start(out=sc_x[t * 128:t * 128 + tp, :], in_=xt),
                      reads=[xt], writes=["sc_x%d" % t])
            k_out, v_out, ik_out = (k_s, v_s, ik_s) if is_sample else (k_p, v_p, ik_p)

            def rows(t):
                return slice(row0 + t * 128, row0 + t * 128 + tp)

            def cons_gate(dst):
                def f(ci, t, ps, pst):
                    gb = bfs_ring.next()
                    S.op(ACT, lambda e: e.activation(out=gb[:tp, :], in_=ps, func=AF.Sigmoid), reads=[pst], writes=[gb])
                    S.dma(SP, lambda e: e.dma_start(out=dst[t * 128:t * 128 + tp, ci * 512:(ci + 1) * 512], in_=gb[:tp, :]),
                          reads=[gb], writes=["%s_%d_%d" % (dst.tensor.name, t, ci)])
                return f
            if not prefix:
                linear(L, L.actT, KT, w_in, [[(O_GR + c * 512, 512)] for c in range(4)], cons_gate(sc_gr), "gr")
                linear(L, L.actT, KT, w_in, [[(O_GA + c * 512, 512)] for c in range(4)], cons_gate(sc_ga), "ga")

            def cons_ak(ci, t, ps, pst):
                kn = headnorm(tp, ps, pst, gk_bc)
                if not prefix:
                    S.dma(SP, lambda e: e.dma_start(out=k_out[rows(t), :], in_=kn[:tp, :]), reads=[kn])
                kb = bfs_ring.next()
                S.op(ACT, lambda e: e.copy(out=kb[:tp, :], in_=kn[:tp, :]), reads=[kn], writes=[kb])
                if is_sample:
                    S.op(DVE, lambda e: e.tensor_copy(out=L.kn_s[:1, :], in_=kn[:1, :]), reads=[kn], writes=[L.kn_s])
                    deferred.append(lambda: transpose_to(kb, tp, 512, lambda j0, nj: L.akTs[:, j0:j0 + nj].unsqueeze(2)))
                else:
                    deferred.append(lambda: transpose_to(
                        kb, tp, 512, lambda j0, nj: kT[:, j0:j0 + nj, tok0 + t * 128:tok0 + t * 128 + tp]))

            def cons_av(ci, t, ps, pst):
                raw = f32s_ring.next()
                S.op(ACT, lambda e: e.copy(out=raw[:tp, :], in_=ps), reads=[pst], writes=[raw])
                if not prefix:
                    S.dma(SP, lambda e: e.dma_start(out=v_out[rows(t), :], in_=raw[:tp, :]), reads=[raw])
                dstv = L.v_sb[:1, :] if is_sample else Vc[:tp, tok0 // 128 + t, :]
                S.op(DVE, lambda e: e.tensor_copy(out=dstv, in_=raw[:tp, :]), reads=[raw], writes=[dstv])

            def cons_ik(ci, t, ps, pst):
                raw = f32s_ring.next()
                S.op(ACT, lambda e: e.copy(out=raw[:tp, 0:80], in_=ps), reads=[pst], writes=[raw])
                if not prefix:
                    S.dma(SP, lambda e: e.dma_start(out=ik_out[rows(t), :], in_=raw[:tp, 0:64]), reads=[raw])
                if is_sample:
                    S.op(DVE, lambda e: e.tensor_copy(out=L.ik_sb[:1, :], in_=raw[:1, 0:64]), reads=[raw], writes=[L.ik_sb])
                    S.op(DVE, lambda e: e.tensor_scalar(out=L.iw_s[:1, 0:16], in0=raw[:1, 64:80], scalar1=IDX_W_SCALE,
                                                        scalar2=None, op0=ALU.mult), reads=[raw], writes=[L.iw_s])
                else:
                    ib = bfs_ring.next()
                    S.op(DVE, lambda e: e.tensor_copy(out=ib[:tp, 0:64], in_=raw[:tp, 0:64]), reads=[raw], writes=[ib])
                    S.op(DVE, lambda e: e.tensor_copy(out=ib[:tp, 64:128], in_=raw[:tp, 0:64]), reads=[raw], writes=[ib])
                    S.op(DVE, lambda e: e.tensor_scalar(out=wsc[:tp, t, :], in0=raw[:tp, 64:80], scalar1=IDX_W_SCALE,
                                                        scalar2=None, op0=ALU.mult), reads=[raw], writes=[wsc])
                    deferred.append(lambda: transpose_to(
                        ib, tp, 128, lambda j0, nj: ikT2[:, tok0 + t * 128:tok0 + t * 128 + tp].unsqueeze(1)))

            linear(L, L.actT, KT, w_in, [[(O_AK, 512)]], cons_ak, "ak")
            linear(L, L.actT, KT, w_in, [[(O_AV, 512)]], cons_av, "av")
            linear(L, L.actT, KT, w_in, [[(O_IK, 80)]], cons_ik, "ik")

            def cons_aq(ci, t, ps, pst):
                qn = headnorm(tp, ps, pst, gq_bc)
                qb = bfs_ring.next()
                S.op(ACT, lambda e: e.copy(out=qb[:tp, :], in_=qn[:tp, :]), reads=[qn], writes=[qb])
                deferred.append(lambda: transpose_to(
                    qb, tp, 512, lambda j0, nj: L.aqT[:, 4 * ci + j0:4 * ci + j0 + nj, t * 128:t * 128 + tp]))
            if not prefix:
                linear(L, L.actT, KT, w_in, [[(O_AQ + c * 512, 512)] for c in range(4)], cons_aq, "aq")

            def cons_iq(ci, t, ps, pst):
                qb = bfs_ring.next()
                S.op(ACT, lambda e: e.copy(out=qb[:tp, :], in_=ps), reads=[pst], writes=[qb])
                if is_sample:
                    def tr():
                        psx = psT.next()

                        def em(e):
                            ins = None
                            for j in range(8):
                                ins = e.transpose(out=psx[0:64, 2 * j:2 * j + 1], in_=qb[:1, j * 64:(j + 1) * 64],
                                                  identity=ident_b[:1, :1])
                            return ins
                        S.op(PE, em, reads=[qb, ident_b], writes=[psx])
                        dst = L.iqTs[0:64, 8 * ci:8 * ci + 8]
                        S.op(ACT, lambda e: e.copy(out=dst, in_=psx[0:64, 0:16].rearrange("p (j two) -> p j two", two=2)[:, :, 0]),
                             reads=[psx], writes=[dst])
                    deferred.append(tr)
                else:
                    deferred.append(lambda: transpose_to(
                        qb, tp, 512, lambda j0, nj: L.iqT[:, 4 * ci + j0:4 * ci + j0 + nj, t * 128:t * 128 + tp]))
            if not prefix:
                linear(L, L.actT, KT, w_in, [[(O_IQ + c * 512, 512)] for c in range(2)], cons_iq, "iq")

            if prefix:
                pass
            elif is_sample:
                decode_dsa(L)
            else:
                prompt_dsa_all(L, tok0)

            for hg in range(2):
                retention_group(L, tok0, hg, is_sample, prefix)
            if prefix:
                return
            if is_sample:
                S.dma(SP, lambda e: e.dma_start(out=rs_s.rearrange("h d e -> d h e"), in_=state_f[:, :, :]),
                      reads=SFK)

            def cons_ao(ci, t, ps, pst):
                gt = L.gtr.next()
                S.dma(SP, lambda e: e.dma_start(out=gt[:tp, :], in_=sc_ga[t * 128:t * 128 + tp, ci * 512:(ci + 1) * 512]),
                      reads=["sc_ga_%d_%d" % (t, ci)], writes=[gt])
                dst = L.m_tm[:tp, t, ci * 512:(ci + 1) * 512]
                S.op(DVE, lambda e: e.tensor_tensor(out=dst, in0=ps, in1=gt[:tp, :], op=ALU.mult),
                     reads=[pst, gt], writes=[dst])
            linear(L, L.attT, KT, w_ao, [[(c * 512, 512)] for c in range(4)], cons_ao, "ao")

            def cons_ro(ci, t, ps, pst):
                gt = L.gtr.next()
                S.dma(SP, lambda e: e.dma_start(out=gt[:tp, :], in_=sc_gr[t * 128:t * 128 + tp, ci * 512:(ci + 1) * 512]),
                      reads=["sc_gr_%d_%d" % (t, ci)], writes=[gt])
                tmp = f32s_ring.next()
                dst = L.m_tm[:tp, t, ci * 512:(ci + 1) * 512]
                S.op(DVE, lambda e: e.tensor_tensor(out=tmp[:tp, :], in0=ps, in1=gt[:tp, :], op=ALU.mult),
                     reads=[pst, gt], writes=[tmp])
                S.op(DVE, lambda e: e.tensor_tensor(out=dst, in0=tmp[:tp, :], in1=dst, op=ALU.add),
                     reads=[tmp, dst], writes=[dst])
            linear(L, L.ogT, KT, w_ro, [[(c * 512, 512)] for c in range(4)], cons_ro, "ro")
            for t in range(nt):
                transpose_to(L.m_tm[:, t, :], tp, D, lambda j0, nj, t=t: L.actT[:, j0:j0 + nj, t * 128:t * 128 + tp])
            for t in range(nt):
                xt = L.x_tm[:tp, t, :]
                S.dma(SP, lambda e, t=t, xt=xt: e.dma_start(out=xt, in_=sc_x[t * 128:t * 128 + tp, :]),
                      reads=["sc_x%d" % t], writes=[xt])

            def cons_o(ci, t, ps, pst):
                xs_ = L.x_tm[:tp, t, ci * 512:(ci + 1) * 512]
                S.op(DVE, lambda e: e.tensor_tensor(out=xs_, in0=ps, in1=xs_, op=ALU.add), reads=[pst, xs_], writes=[xs_])
            linear(L, L.actT, KT, w_o, [[(c * 512, 512)] for c in range(4)], cons_o, "wo")

        def retention_group(L, tok0, hg, is_sample, prefix=False):
            tp, nt = L.tp, L.nt
            pos0 = SEQ if is_sample else tok0

            def cons_rot(which):
                def f(ci, t, ps, pst):
                    raw = f32s_ring.next()
                    tmp = f32s_ring.next()
                    ob = bfs_ring.next()
                    rp = L.rope.next()
                    S.dma(SP, lambda e: e.dma_start(
                        out=rp[:tp, :, :, :],
                        in_=c_rope[pos0 + t * 128:pos0 + t * 128 + tp, 2 * which:2 * which + 2, 4 * hg:4 * hg + 4, :]),
                        writes=[rp[:tp, :, :, :]])
                    S.op(ACT, lambda e: e.copy(out=raw[:tp, :], in_=ps), reads=[pst], writes=[raw])
                    x = raw[:tp, :].rearrange("p (h two f) -> p h two f", h=4, two=2)
                    x1, x2 = x[:, :, 0, :], x[:, :, 1, :]
                    C = rp[:tp, 0, :, :]
                    Sn = rp[:tp, 1, :, :]
                    tv = tmp[:tp, :].rearrange("p (a h f) -> p a h f", a=2, h=4)
                    o = ob[:tp, :].rearrange("p (h two f) -> p h two f", h=4, two=2)
                    rk = [raw, rp[:tp, :, :, :]]
                    S.op(DVE, lambda e: e.tensor_tensor(out=tv[:, 0], in0=x1, in1=C, op=ALU.mult), reads=rk, writes=[tmp])
                    S.op(DVE, lambda e: e.tensor_tensor(out=tv[:, 1], in0=x2, in1=Sn, op=ALU.mult), reads=rk, writes=[tmp])
                    S.op(DVE, lambda e: e.tensor_tensor(out=o[:, :, 0, :], in0=tv[:, 0], in1=tv[:, 1], op=ALU.subtract),
                         reads=[tmp], writes=[ob])
                    S.op(DVE, lambda e: e.tensor_tensor(out=tv[:, 0], in0=x1, in1=Sn, op=ALU.mult), reads=rk + [ob], writes=[tmp])
                    S.op(DVE, lambda e: e.tensor_tensor(out=tv[:, 1], in0=x2, in1=C, op=ALU.mult), reads=rk, writes=[tmp])
                    S.op(DVE, lambda e: e.tensor_tensor(out=o[:, :, 1, :], in0=tv[:, 0], in1=tv[:, 1], op=ALU.add),
                         reads=[tmp], writes=[ob])
                    if which == 1:
                        kd = L.ktm[:tp, t, :]
                        S.op(ACT, lambda e: e.copy(out=kd, in_=ob[:tp, :]), reads=[ob], writes=[kd])
                    dstT = L.qrT if which == 0 else L.krT
                    if not prefix:
                        deferred.append(lambda: transpose_to(
                            ob, tp, 512, lambda j0, nj: dstT[:, j0:j0 + nj, t * 128:t * 128 + tp]))
                return f
            if not prefix:
                linear(L, L.actT, KT, w_in, [[(O_RQ + hg * 512, 512)]], cons_rot(0), "rq%d" % hg)
            linear(L, L.actT, KT, w_in, [[(O_RK + hg * 512, 512)]], cons_rot(1), "rk%d" % hg)

            def cons_rv(ci, t, ps, pst):
                dst = L.vtm[:tp, t, ci * 512:(ci + 1) * 512]
                S.op(ACT, lambda e: e.copy(out=dst, in_=ps), reads=[pst], writes=[dst])
            linear(L, L.actT, KT, w_in, [[(O_RV + hg * 1024 + c * 512, 512)] for c in range(2)], cons_rv, "rv%d" % hg)

            def cons_rg(ci, t, ps, pst):
                dst = L.sgtm[:tp, t, ci * 512:(ci + 1) * 512]
                S.op(ACT, lambda e: e.activation(out=dst, in_=ps, func=AF.Silu), reads=[pst], writes=[dst])
            if not prefix:
                linear(L, L.actT, KT, w_in, [[(O_RG + hg * 1024 + c * 512, 512)] for c in range(2)], cons_rg, "rg%d" % hg)

            log_g = [float(np.log1p(-np.exp2(-5.0 - h))) for h in range(cfg.RH)]
            def ret_tile(t):
                ts = slice(t * 128, t * 128 + tp)
                sms = []
                for hl in range(4 if not prefix else 0):
                    ps_s = psL.next()
                    S.op(PE, lambda e, ps_s=ps_s, hl=hl, ts=ts: e.matmul(ps_s[:tp, 0:tp], lhsT=L.krT[:, hl, ts], rhs=L.qrT[:, hl, ts],
                                                               start=True, stop=True),
                         reads=[L.krT[:, hl, ts], L.qrT[:, hl, ts]], writes=[ps_s])
                    sm = L.sTm.next()
                    S.op(DVE, lambda e, ps_s=ps_s, sm=sm: e.tensor_tensor(out=sm[:tp, 0:tp], in0=ps_s[:tp, 0:tp],
                                                                           in1=caus[:tp, 0:tp], op=ALU.mult),
                         reads=[ps_s, caus], writes=[sm])
                    sms.append(sm)
                for hl in range(4):
                    h = 4 * hg + hl
                    ps_kv = psM[hl % 2]
                    vs_ = L.vtm[:tp, t, hl * 256:(hl + 1) * 256]
                    if not prefix:
                        sm = sms[hl]
                        ps_o = psL.next()

                    if not prefix:
                        def em_o(e, ps_o=ps_o, sm=sm, vs_=vs_, hl=hl, h=h, ts=ts):
                            e.matmul(ps_o[:tp, 0:256], lhsT=sm[:tp, 0:tp], rhs=vs_, start=True, stop=False)
                            return e.matmul(ps_o[:tp, 0:256], lhsT=L.qrT[:, hl, ts], rhs=state_b[:, h, :], start=False, stop=True)
                        S.op(PE, em_o, reads=[sm, vs_, L.qrT[:, hl, ts], "state_b%d" % h], writes=[ps_o])
                    kslice = L.ktm[:tp, t, hl * 128:(hl + 1) * 128]
                    S.op(PE, lambda e, ps_kv=ps_kv, kslice=kslice, vs_=vs_: e.matmul(
                        ps_kv[:, 0:256], lhsT=kslice, rhs=vs_, start=True, stop=True),
                        reads=[kslice, vs_], writes=[ps_kv])
                    if not prefix:
                        orw = L.o_raw[:tp, hl, :]
                        S.op(ACT, lambda e, orw=orw, ps_o=ps_o: e.copy(out=orw, in_=ps_o[:tp, 0:256]), reads=[ps_o], writes=[orw])
                    sf = state_f[:, h, :]
                    gC = float(np.exp(log_g[h] * tp))
                    S.op(DVE, lambda e, sf=sf, ps_kv=ps_kv: e.tensor_tensor(out=sf, in0=sf, in1=ps_kv[:, 0:256], op=ALU.add),
                         reads=[ps_kv, "state_f%d" % h], writes=["state_f%d" % h])
                    S.op(DVE, lambda e, sf=sf, gC=gC: e.tensor_scalar(out=sf, in0=sf, scalar1=gC, scalar2=None, op0=ALU.mult),
                         reads=["state_f%d" % h], writes=["state_f%d" % h])
                    S.op(ACT, lambda e, sf=sf, h=h: e.copy(out=state_b[:, h, :], in_=sf),
                         reads=["state_f%d" % h], writes=["state_b%d" % h])
                if prefix:
                    return
                ss, ssk = stat_cols(4)
                for hl in range(4):
                    orw = L.o_raw[:tp, hl, :]
                    S.op(DVE, lambda e, orw=orw, hl=hl: e.scalar_tensor_tensor(
                        out=L.og_tm[:tp, hl * 256:(hl + 1) * 256], in0=orw, scalar=1.0, in1=orw,
                        op0=ALU.mult, op1=ALU.mult, accum_out=ss[:tp, hl:hl + 1]),
                        reads=[orw], writes=[L.og_tm[:tp, :]] + ssk)
                rs, rsk = rstd_from_ss(ss[:tp], ssk, 4, tp, 1.0 / 256)
                Gv = L.G[:tp, :].rearrange("p (h e) -> p h e", h=4)
                S.op(DVE, lambda e: e.tensor_tensor(
                    out=Gv, in0=L.sgtm[:tp, t, :].rearrange("p (h e) -> p h e", h=4),
                    in1=gr_bc[:tp, :].unsqueeze(1).to_broadcast([tp, 4, 256]), op=ALU.mult),
                    reads=[L.sgtm[:tp, t, :], gr_bc], writes=[L.G[:tp, :]])
                orall = L.o_raw[:tp, :, :]
                S.op(DVE, lambda e: e.tensor_tensor(out=orall, in0=orall,
                                                    in1=rs[:tp].unsqueeze(2).to_broadcast([tp, 4, 256]), op=ALU.mult),
                     reads=[orall] + rsk, writes=[orall])
                S.op(DVE, lambda e: e.tensor_tensor(out=L.og_tm[:tp, :].rearrange("p (h e) -> p h e", h=4),
                                                    in0=orall, in1=Gv, op=ALU.mult),
                     reads=[orall, L.G[:tp, :]], writes=[L.og_tm[:tp, :]])
                transpose_to(L.og_tm, tp, 1024, lambda j0, nj, t=t: L.ogT[:, 8 * hg + j0:8 * hg + j0 + nj, t * 128:t * 128 + tp])

            for t in range(nt):
                ret_tile(t)

        def bisect_init(tp):
            T_ = lambda i: thr[:tp, i:i + 1]
            k_ = lambda i: "thr%d" % i
            S.op(DVE, lambda e: e.tensor_tensor(out=T_(2), in0=T_(0), in1=T_(1), op=ALU.subtract),
                 reads=[k_(0), k_(1)], writes=[k_(2)])
            S.op(DVE, lambda e: e.tensor_scalar(out=T_(2), in0=T_(2), scalar1=1.0001, scalar2=1e-20,
                                                op0=ALU.mult, op1=ALU.add), reads=[k_(2)], writes=[k_(2)])
            S.op(DVE, lambda e: e.tensor_scalar(out=w_all[:tp, :], in0=pow2[:tp, :], scalar1=T_(2), scalar2=None, op0=ALU.mult),
                 reads=[k_(2), pow2], writes=[w_all])
            S.op(DVE, lambda e: e.tensor_tensor(out=T_(5), in0=T_(1), in1=w_all[:tp, 1:2], op=ALU.add),
                 reads=[k_(1), w_all], writes=[k_(5)])

        def bisect_iters(tp, count_fn, K, i0, i1):
            T_ = lambda i: thr[:tp, i:i + 1]
            k_ = lambda i: "thr%d" % i
            for i in range(i0, i1):
                count_fn()
                S.op(DVE, lambda e: e.tensor_scalar(out=T_(7), in0=T_(6), scalar1=K - 0.5, scalar2=0.5,
                                                    op0=ALU.is_ge, op1=ALU.subtract),
                     reads=[k_(6)], writes=[k_(7)])
                S.op(DVE, lambda e, i=i: e.scalar_tensor_tensor(out=T_(5), in0=T_(7), scalar=w_all[:tp, i + 1:i + 2], in1=T_(5),
                                                                op0=ALU.mult, op1=ALU.add),
                     reads=[k_(7), k_(5), w_all], writes=[k_(5)])

        def bisect_finish(tp):
            n = cfg.NIT
            S.op(DVE, lambda e: e.tensor_tensor(out=thr[:tp, 3:4], in0=thr[:tp, 5:6], in1=w_all[:tp, n + 1:n + 2], op=ALU.subtract),
                 reads=["thr5", w_all], writes=["thr3"])

        def bisect(S_ap, tp, count_fn, K):
            bisect_init(tp)
            bisect_iters(tp, count_fn, K, 0, cfg.NIT)
            bisect_finish(tp)

        def dsa_index(L, tok0, t):
            tp = L.tp
            ts = slice(t * 128, t * 128 + tp)
            nk = tok0 + (t + 1) * 128
            for h in range(16):
                S.op(DVE, lambda e, h=h: e.tensor_scalar(out=L.diagW[:tp, h, 0:tp], in0=ident_b[:tp, 0:tp],
                                                         scalar1=wsc[:tp, t, h:h + 1], scalar2=None, op0=ALU.mult),
                     reads=[ident_b, wsc], writes=[L.diagW[:tp, h, 0:tp]])
            def idx_chunk(kc, c0):
                cw = min(512, nk - c0)
                S_ps = psM[kc % 2]
                pend = []
                for h in range(16):
                    r0 = (h % 2) * 64
                    Pps = psL.next()
                    lq = L.iqT[r0:r0 + 64, h // 2, ts]
                    rk_ = ikT2[r0:r0 + 64, c0:c0 + cw]
                    S.op(PE, lambda e, Pps=Pps, lq=lq, rk_=rk_: e.matmul(Pps[:tp, 0:cw], lhsT=lq, rhs=rk_, start=True, stop=True),
                         reads=[lq, ikT2], writes=[Pps])
                    Rb = L.Rring.next()
                    S.op(ACT, lambda e, Pps=Pps, Rb=Rb: e.activation(out=Rb[:tp, 0:cw], in_=Pps[:tp, 0:cw], func=AF.Relu),
                         reads=[Pps], writes=[Rb])

                    def dg(h=h, Rb=Rb, S_ps=S_ps, cw=cw):
                        S.op(PE, lambda e: e.matmul(S_ps[:tp, 0:cw], lhsT=L.diagW[:tp, h, 0:tp], rhs=Rb[:tp, 0:cw],
                                                    start=(h == 0), stop=(h == 15)),
                             reads=[L.diagW[:tp, h, 0:tp], Rb], writes=[S_ps])
                    pend.append(dg)
                    if len(pend) > 2:
                        pend.pop(0)()
                while pend:
                    pend.pop(0)()
                dstS = L.S_sb[:tp, c0:c0 + cw]
                S.op(ACT, lambda e, dstS=dstS, S_ps=S_ps: e.copy(out=dstS, in_=S_ps[:tp, 0:cw]), reads=[S_ps], writes=[dstS])

            for kc, c0 in enumerate(range(0, nk, 512)):
                idx_chunk(kc, c0)
            Sall = L.S_sb[:tp, 0:nk]
            S.op(DVE, lambda e: e.tensor_reduce(out=thr[:tp, 0:1], in_=Sall, axis=AX.X, op=ALU.max), reads=[Sall], writes=["thr0"])
            S.op(DVE, lambda e: e.tensor_reduce(out=thr[:tp, 1:2], in_=Sall, axis=AX.X, op=ALU.min), reads=[Sall], writes=["thr1"])
            pre_ = L.S_sb[:tp, 0:NPRE]
            S.op(DVE, lambda e: e.tensor_scalar(out=pre_, in0=pre_, scalar1=negflag_sb[:tp, 0:1], scalar2=None, op0=ALU.add),
                 reads=[pre_, negflag_sb], writes=[pre_])
            dg_ = L.S_sb[:tp, nk - 128:nk]
            S.op(DVE, lambda e: e.tensor_tensor(out=dg_, in0=dg_, in1=negu[:tp, :], op=ALU.add), reads=[dg_, negu], writes=[dg_])

            def count_fn():
                S.op(DVE, lambda e: e.tensor_scalar(out=L.junk[:tp, 0:nk], in0=Sall, scalar1=thr[:tp, 5:6], scalar2=0.0,
                                                    op0=ALU.is_ge, op1=ALU.add, accum_out=thr[:tp, 6:7]),
                     reads=[Sall, "thr5"], writes=[L.junk[:tp, 0:nk], "thr6"])
            bisect_init(tp)
            return count_fn, Sall

        def dsa_mask(L, tok0, t, Sall, mbuf):
            tp = L.tp
            nk = tok0 + (t + 1) * 128
            bisect_finish(tp)
            mbv = mbuf[:tp, 0:nk]
            S.op(DVE, lambda e: e.tensor_scalar(out=mbv, in0=Sall, scalar1=thr[:tp, 3:4], scalar2=NEG,
                                                op0=ALU.is_lt, op1=ALU.mult), reads=[Sall, "thr3"], writes=[mbv])

        def dsa_attend(L, tok0, t, g, mbuf):
            tp = L.tp
            ts = slice(t * 128, t * 128 + tp)
            nk = tok0 + (t + 1) * 128
            nkb = nk // 128
            identrep = ident_b[:tp, 0:tp].unsqueeze(1).to_broadcast([tp, 4, tp])
            def att_group(g):
                oT, den = psM[0], psM[1]
                pend = []
                qv = L.aqT[:, 4 * g:4 * g + 4, ts]
                for kb in range(nkb):
                    sps = psL.next()
                    spv = sps[:, 0:4 * tp].rearrange("p (h t) -> p h t", h=4)
                    kslice = kT[:, g, kb * 128:(kb + 1) * 128]
                    mslice = mbuf[:tp, kb * 128:(kb + 1) * 128]

                    def em_s(e, spv=spv, kslice=kslice, mslice=mslice):
                        e.matmul(spv, lhsT=kslice, rhs=qv, start=True, stop=False)
                        return e.matmul(spv, lhsT=mslice, rhs=identrep, start=False, stop=True)
                    S.op(PE, em_s, reads=[kT, qv, mslice, ident_b], writes=[sps])
                    pT_ = L.pTring.next()
                    S.op(ACT, lambda e, sps=sps, pT_=pT_: e.activation(out=pT_[:, 0:4 * tp], in_=sps[:, 0:4 * tp], func=AF.Exp),
                         reads=[sps], writes=[pT_])

                    def pv(kb=kb, pT_=pT_):
                        vsl = Vc[:, kb, g * 128:(g + 1) * 128]

                        def em(e):
                            e.matmul(oT[:, 0:4 * tp], lhsT=vsl, rhs=pT_[:, 0:4 * tp], start=(kb == 0), stop=(kb == nkb - 1))
                            return e.matmul(den[:, 0:4 * tp], lhsT=ones_b[:, :], rhs=pT_[:, 0:4 * tp],
                                            start=(kb == 0), stop=(kb == nkb - 1))
                        S.op(PE, em, reads=[Vc, pT_, ones_b], writes=[oT, den])
                    pend.append(pv)
                    if len(pend) > 1:
                        pend.pop(0)()
                while pend:
                    pend.pop(0)()
                S.op(DVE, lambda e: e.reciprocal(out=L.rden[:, 0:4 * tp], in_=den[:, 0:4 * tp]), reads=[den], writes=[L.rden])
                dsta = L.attT[:, 4 * g:4 * g + 4, ts]
                S.op(DVE, lambda e, dsta=dsta: e.tensor_tensor(
                    out=dsta, in0=oT[:, 0:4 * tp].rearrange("p (h t) -> p h t", h=4),
                    in1=L.rden[:, 0:4 * tp].rearrange("p (h t) -> p h t", h=4), op=ALU.mult),
                    reads=[oT, L.rden], writes=[dsta])

            att_group(g)

        def prompt_dsa_all(L, tok0):
            tp, nt = L.tp, L.nt
            mbufs = [L.mb, L.mb2]
            nsl = 4
            per = (cfg.NIT + nsl - 1) // nsl
            cur = dsa_index(L, tok0, 0)
            for t in range(nt):
                count_fn, Sall = cur
                for sl in range(nsl):
                    bisect_iters(tp, count_fn, cfg.TOPK, sl * per, min(cfg.NIT, (sl + 1) * per))
                    if t > 0:
                        dsa_attend(L, tok0, t - 1, sl, mbufs[(t - 1) % 2])
                dsa_mask(L, tok0, t, Sall, mbufs[t % 2])
                if t + 1 < nt:
                    cur = dsa_index(L, tok0, t + 1)
            for g in range(4):
                dsa_attend(L, tok0, nt - 1, g, mbufs[(nt - 1) % 2])

        def decode_dsa(L):
            D0 = 0
            kidx_g = AV(D0, 4096, F32)
            kidxTq = AV(4096, 4096)
            Rr = Ring([AV(8192 + 512 * i, 512) for i in range(4)])
            b = [10240]

            def al(n, dt=BF16):
                o = b[0]
                b[0] += (n * (2 if dt != BF16 else 1) + 63) // 64 * 64
                assert b[0] <= 20480
                return AV(o, n * (2 if dt != BF16 else 1), dt)
            Sg = al(130, F32)
            junkg = al(130)
            rsc = al(128, F32)
            dest = al(128, F32)
            mk = al(128, F32)
            Er = Ring([al(256, F32) for _ in range(3)])
            kg = al(1024, F32)
            vg = al(1024, F32)
            kgb = al(1024)
            vgb = al(1024)
            kgT = al(1024)
            pts = al(8, F32)
            idx4 = sb("idx4", [128, 1], I32)
            ptsb = sb("ptsb", [128, 1], I32)
            rowi = sb("rowi", [128, 2], I32)
            small = sb("dsm", [128, 64], F32)
            smallb = sb("dsmb", [128, 64], BF16)
            iota_d = sb("iota_d", [128, 256], F32)
            lstr = sb("lstr", [128, 128], BF16)
            jcol = sb("jcol", [128, 128], F32)
            dslot = sb("dslot", [128, 2], F32)
            rhs3 = sb("rhs3", [128, 128, 2], F32)
            cload(iota_d[:, :], c_iota)
            cload(jcol[:, :], c_jcol)
            cload(dslot[:, :], c_dslot)
            cload(mk[:, 0:128], c_lstr)
            S.op(DVE, lambda e: e.tensor_copy(out=lstr[:, :], in_=mk[:, 0:128]), reads=[mk], writes=[lstr])
            idx4b = sb("idx4b", [128, 1], I32)
            rowib = sb("rowib", [128, 2], I32)
            for tt_ in (idx4, idx4b, rowi, rowib):
                S.op(DVE, lambda e, tt_=tt_: e.memset(tt_[:, :], 0), writes=[tt_])
            cload(ptsb[:, :], ptab)
            S.op(DVE, lambda e: e.tensor_copy(out=pts[:, 0:1], in_=ptsb[:, :]), reads=[ptsb], writes=[pts])
            pw = psL.next()
            S.op(PE, lambda e: e.transpose(out=pw[0:16, 0:1], in_=L.iw_s[:1, 0:16], identity=ident_f[:1, :1]),
                 reads=[L.iw_s, ident_f], writes=[pw])
            S.op(ACT, lambda e: e.copy(out=L.wcol[0:16, 0:1], in_=pw[0:16, 0:1]), reads=[pw], writes=[L.wcol])
            px = psT.next()
            S.op(PE, lambda e: e.transpose(out=px[0:64, 0:1], in_=L.ik_sb[:1, 0:64], identity=ident_b[:1, :1]),
                 reads=[L.ik_sb, ident_b], writes=[px])
            S.op(ACT, lambda e: e.copy(out=L.ikTs[0:64, 0:1], in_=px[0:64, 0:1]), reads=[px], writes=[L.ikTs])
            psG = psM[0]
            for q4 in range(4):
                S.op(DVE, lambda e, q4=q4: e.tensor_scalar(out=idx4[:, :], in0=pts[:, 0:1], scalar1=4.0, scalar2=float(q4),
                                                           op0=ALU.mult, op1=ALU.add), reads=[pts], writes=[idx4])
                S.op(DVE, lambda e: e.tensor_copy(out=idx4b[:, :], in_=idx4[:, :]), reads=[idx4], writes=[idx4b])
                S.dma(POOL, lambda e: e.indirect_dma_start(
                    out=kidx_g[:, :], out_offset=None, in_=cache_ik[:, :],
                    in_offset=bass.IndirectOffsetOnAxis(ap=idx4b[:, :], axis=0),
                    bounds_check=cfg.NPHYS * 4 - 1, oob_is_err=False), reads=[idx4b], writes=[kidx_g])
                for jb in range(8):
                    pk = psL.next()

                    def em(e, pk=pk, jb=jb):
                        ins = None
                        for jj in range(4):
                            j = jb * 4 + jj
                            ins = e.transpose(out=pk[0:64, jj * 128:(jj + 1) * 128], in_=kidx_g[:, j * 64:(j + 1) * 64],
                                              identity=ident_f[:, :])
                        return ins
                    S.op(PE, em, reads=[kidx_g, ident_f], writes=[pk])
                    dstk = kidxTq[0:64, jb * 512:(jb + 1) * 512]
                    S.op(ACT, lambda e, pk=pk, dstk=dstk: e.copy(out=dstk, in_=pk[0:64, :]), reads=[pk], writes=[dstk])
                for c in range(8):
                    Pp = psL.next()
                    rk_ = kidxTq[0:64, c * 512:(c + 1) * 512]
                    S.op(PE, lambda e, Pp=Pp, rk_=rk_: e.matmul(Pp[0:16, :], lhsT=L.iqTs[0:64, 0:16], rhs=rk_, start=True, stop=True),
                         reads=[L.iqTs, rk_], writes=[Pp])
                    Rb = Rr.next()
                    S.op(ACT, lambda e, Pp=Pp, Rb=Rb: e.activation(out=Rb[0:16, :], in_=Pp[0:16, :], func=AF.Relu),
                         reads=[Pp], writes=[Rb])

                    def em2(e, Rb=Rb, c=c, q4=q4):
                        ins = None
                        for jj in range(4):
                            j = q4 * 32 + c * 4 + jj
                            ins = e.matmul(psG[:, j:j + 1], lhsT=Rb[0:16, jj * 128:(jj + 1) * 128], rhs=L.wcol[0:16, 0:1],
                                           start=True, stop=True)
                        return ins
                    S.op(PE, em2, reads=[Rb, L.wcol], writes=[psG])
            S.op(ACT, lambda e: e.copy(out=Sg[:, 0:128], in_=psG[:, 0:128]), reads=[psG], writes=[Sg])
            S.op(DVE, lambda e: e.memset(Sg[:, 128:130], -1e30), writes=[Sg])
            pself = psL.next()
            S.op(PE, lambda e: e.matmul(pself[0:16, 0:1], lhsT=L.iqTs[0:64, 0:16], rhs=L.ikTs[0:64, 0:1], start=True, stop=True),
                 reads=[L.iqTs, L.ikTs], writes=[pself])
            S.op(ACT, lambda e: e.activation(out=smallb[0:16, 0:1], in_=pself[0:16, 0:1], func=AF.Relu), reads=[pself], writes=[smallb])
            pself2 = psL.next()
            S.op(PE, lambda e: e.matmul(pself2[0:1, 0:1], lhsT=smallb[0:16, 0:1], rhs=L.wcol[0:16, 0:1], start=True, stop=True),
                 reads=[smallb, L.wcol], writes=[pself2])
            S.op(ACT, lambda e: e.copy(out=Sg[0:1, 128:129], in_=pself2[0:1, 0:1]), reads=[pself2], writes=[Sg])
            S.op(DVE, lambda e: e.tensor_reduce(out=small[:, 0:1], in_=Sg[:, 0:128], axis=AX.X, op=ALU.min), reads=[Sg], writes=[small])
            S.op(DVE, lambda e: e.tensor_reduce(out=small[:, 1:2], in_=Sg[:, 0:128], axis=AX.X, op=ALU.max, negate=True),
                 reads=[Sg], writes=[small])
            pmm = psL.next()
            S.op(PE, lambda e: e.transpose(out=pmm[0:2, 0:128], in_=small[:, 0:2], identity=ident_f[:, :]),
                 reads=[small, ident_f], writes=[pmm])
            S.op(DVE, lambda e: e.tensor_reduce(out=small[0:2, 2:3], in_=pmm[0:2, 0:128], axis=AX.X, op=ALU.min),
                 reads=[pmm], writes=[small])
            S.op(DVE, lambda e: e.tensor_scalar(out=small[0:2, 4:6], in0=ident_f[0:2, 0:2], scalar1=small[0:2, 2:3], scalar2=None,
                                                op0=ALU.mult), reads=[small, ident_f], writes=[small])
            pbc = psL.next()
            ones2 = sb("ones2", [2, 128], F32)
            S.op(DVE, lambda e: e.memset(ones2[:, :], 1.0), writes=[ones2])
            S.op(PE, lambda e: e.matmul(pbc[:, 0:2], lhsT=ones2[0:2, :], rhs=small[0:2, 4:6], start=True, stop=True),
                 reads=[ones2, small], writes=[pbc])
            S.op(DVE, lambda e: e.tensor_scalar(out=thr[:, 0:1], in0=pbc[:, 1:2], scalar1=-1.0, scalar2=None, op0=ALU.mult),
                 reads=[pbc], writes=["thr0"])
            S.op(DVE, lambda e: e.tensor_copy(out=thr[:, 1:2], in_=pbc[:, 0:1]), reads=[pbc], writes=["thr1"])

            def count_fn():
                S.op(DVE, lambda e: e.tensor_scalar(out=junkg[:, 0:129], in0=Sg[:, 0:129], scalar1=thr[:, 5:6], scalar2=0.0,
                                                    op0=ALU.is_ge, op1=ALU.add, accum_out=smallb[:, 2:3]),
                     reads=[Sg, "thr5"], writes=[junkg, smallb])
                pc = psL.next()
                S.op(PE, lambda e: e.matmul(pc[:, 0:1], lhsT=ones_b[:, :], rhs=smallb[:, 2:3], start=True, stop=True),
                     reads=[ones_b, smallb], writes=[pc])
                S.op(DVE, lambda e: e.tensor_copy(out=thr[:, 6:7], in_=pc[:, 0:1]), reads=[pc], writes=["thr6"])
            bisect(Sg, 128, count_fn, cfg.TOPK)
            S.op(DVE, lambda e: e.tensor_scalar(out=mk[:, 0:128], in0=Sg[:, 0:128], scalar1=thr[:, 3:4], scalar2=0.0,
                                                op0=ALU.is_ge, op1=ALU.add, accum_out=smallb[:, 4:5]),
                 reads=[Sg, "thr3"], writes=[mk, smallb])
            S.op(DVE, lambda e: e.tensor_tensor_scan(out=rsc[:, 0:128], data0=ones_f[:, :], data1=mk[:, 0:128], initial=0.0,
                                                     op0=ALU.mult, op1=ALU.add), reads=[mk, ones_f], writes=[rsc])
            poff = psL.next()

            def em_off(e):
                e.matmul(poff[:, 0:1], lhsT=lstr[:, :], rhs=smallb[:, 4:5], start=True, stop=True)
                return e.matmul(poff[:, 1:2], lhsT=ones_b[:, :], rhs=smallb[:, 4:5], start=True, stop=True)
            S.op(PE, em_off, reads=[lstr, ones_b, smallb], writes=[poff])
            S.op(DVE, lambda e: e.tensor_copy(out=small[:, 16:18], in_=poff[:, 0:2]), reads=[poff], writes=[small])
            S.op(DVE, lambda e: e.scalar_tensor_tensor(out=dest[:, 0:128], in0=rsc[:, 0:128], scalar=small[:, 16:17], in1=mk[:, 0:128],
                                                       op0=ALU.add, op1=ALU.mult), reads=[rsc, small, mk], writes=[dest])
            S.op(DVE, lambda e: e.tensor_scalar(out=dest[:, 0:128], in0=dest[:, 0:128], scalar1=-1.0, scalar2=None, op0=ALU.add),
                 reads=[dest], writes=[dest])
            S.op(DVE, lambda e: e.tensor_copy(out=rhs3[:, :, 0], in_=pts[:, 0:1].to_broadcast([128, 128])), reads=[pts], writes=[rhs3])
            S.op(DVE, lambda e: e.tensor_copy(out=rhs3[:, :, 1], in_=jcol[:, :]), reads=[jcol], writes=[rhs3])
            pslot = [psM[1], psL.next()]
            for j in range(128):
                E = Er.next()
                S.op(DVE, lambda e, E=E, j=j: e.tensor_scalar(out=E[:, 0:256], in0=iota_d[:, :], scalar1=dest[:, j:j + 1], scalar2=None,
                                                              op0=ALU.is_equal), reads=[iota_d, dest], writes=[E])

                def em3(e, E=E, j=j):
                    e.matmul(pslot[0][:, 0:2], lhsT=E[:, 0:128], rhs=rhs3[:, j, :], start=(j == 0), stop=(j == 127))
                    return e.matmul(pslot[1][:, 0:2], lhsT=E[:, 128:256], rhs=rhs3[:, j, :], start=(j == 0), stop=(j == 127))
                S.op(PE, em3, reads=[E, rhs3], writes=[pslot[0], pslot[1]])
            for tl in range(2):
                sl = small[:, 20 + 4 * tl:24 + 4 * tl]
                S.op(DVE, lambda e, sl=sl, tl=tl: e.tensor_copy(out=sl[:, 0:2], in_=pslot[tl][:, 0:2]), reads=[pslot[tl]], writes=[small])
                S.op(DVE, lambda e, sl=sl: e.scalar_tensor_tensor(out=sl[:, 3:4], in0=sl[:, 0:1], scalar=128.0, in1=sl[:, 1:2],
                                                                 op0=ALU.mult, op1=ALU.add), reads=[small], writes=[small])
                S.op(DVE, lambda e, sl=sl, tl=tl: e.tensor_copy(out=rowi[:, tl:tl + 1], in_=sl[:, 3:4]), reads=[small], writes=[rowi])
            S.op(DVE, lambda e: e.tensor_scalar(out=small[:, 30:32], in0=dslot[:, 0:2], scalar1=small[:, 17:18], scalar2=NEG,
                                                op0=ALU.is_ge, op1=ALU.mult), reads=[dslot, small], writes=[small])
            S.op(DVE, lambda e: e.tensor_scalar(out=small[0:1, 32:33], in0=Sg[0:1, 128:129], scalar1=thr[0:1, 3:4], scalar2=NEG,
                                                op0=ALU.is_lt, op1=ALU.mult), reads=[Sg, "thr3"], writes=[small])
            S.op(DVE, lambda e: e.tensor_copy(out=rowib[:, :], in_=rowi[:, :]), reads=[rowi], writes=[rowib])
            S.op(DVE, lambda e: e.memset(kg[:, :], 0.0), writes=[kg])
            S.op(DVE, lambda e: e.memset(vg[:, :], 0.0), writes=[vg])
            for tl in range(2):
                for (dstt, srcc) in ((kg, cache_k), (vg, cache_v)):
                    dv = dstt[:, tl * 512:(tl + 1) * 512]
                    S.dma(POOL, lambda e, dv=dv, srcc=srcc, tl=tl: e.indirect_dma_start(
                        out=dv, out_offset=None, in_=srcc[:, :],
                        in_offset=bass.IndirectOffsetOnAxis(ap=rowib[:, tl:tl + 1], axis=0),
                        bounds_check=cfg.NPHYS * 128 - 1, oob_is_err=False), reads=[rowib], writes=[dv])
            S.op(ACT, lambda e: e.copy(out=kgb[:, :], in_=kg[:, :]), reads=[kg], writes=[kgb])
            S.op(ACT, lambda e: e.copy(out=vgb[:, :], in_=vg[:, :]), reads=[vg], writes=[vgb])
            for tl in range(2):
                transpose_to(kgb[:, tl * 512:(tl + 1) * 512], 128, 512,
                             lambda j0, nj, tl=tl: kgT[:, tl * 512:(tl + 1) * 512].rearrange("p (g k) -> p g k", g=4)[:, j0:j0 + nj, :])
            pS = psL.next()
            for tl in range(2):
                def em4(e, tl=tl):
                    ins = None
                    for g in range(4):
                        ins = e.matmul(pS[:, tl * 16 + 4 * g:tl * 16 + 4 * g + 4],
                                       lhsT=kgT[:, tl * 512 + g * 128:tl * 512 + (g + 1) * 128],
                                       rhs=L.aqT[:, 4 * g:4 * g + 4, 0], start=True, stop=True)
                    return ins
                S.op(PE, em4, reads=[kgT, L.aqT], writes=[pS])
            pTd = smallb[:, 8:40]
            for tl in range(2):
                S.op(ACT, lambda e, tl=tl: e.activation(out=smallb[:, 8 + 16 * tl:24 + 16 * tl], in_=pS[:, 16 * tl:16 * tl + 16],
                                                        func=AF.Exp, bias=small[:, 30 + tl:31 + tl], scale=1.0),
                     reads=[pS, small], writes=[smallb])
            pss_ = psL.next()

            def em5(e):
                ins = None
                for g in range(4):
                    ins = e.matmul(pss_[0:1, 4 * g:4 * g + 4], lhsT=L.akTs[:, g:g + 1], rhs=L.aqT[:, 4 * g:4 * g + 4, 0],
                                   start=True, stop=True)
                return ins
            S.op(PE, em5, reads=[L.akTs, L.aqT], writes=[pss_])
            S.op(ACT, lambda e: e.activation(out=smallb[0:1, 40:56], in_=pss_[0:1, 0:16], func=AF.Exp,
                                             bias=small[0:1, 32:33], scale=1.0), reads=[pss_, small], writes=[smallb])
            po, pd = psM[0], psM[1]

            def em6(e):
                ins = None
                for g in range(4):
                    for tl in range(2):
                        e.matmul(po[:, 4 * g:4 * g + 4], lhsT=vgb[:, tl * 512 + g * 128:tl * 512 + (g + 1) * 128],
                                 rhs=smallb[:, 8 + 16 * tl + 4 * g:8 + 16 * tl + 4 * g + 4], start=(tl == 0), stop=False)
                    ins = e.matmul(po[:, 4 * g:4 * g + 4], lhsT=L.v_sb[0:1, g * 128:(g + 1) * 128],
                                   rhs=smallb[0:1, 40 + 4 * g:44 + 4 * g], start=False, stop=True)
                for tl in range(2):
                    e.matmul(pd[:, 0:16], lhsT=ones_b[:, :], rhs=smallb[:, 8 + 16 * tl:24 + 16 * tl], start=(tl == 0), stop=False)
                ins = e.matmul(pd[:, 0:16], lhsT=ones_b[0:1, :], rhs=smallb[0:1, 40:56], start=False, stop=True)
                return ins
            S.op(PE, em6, reads=[vgb, smallb, L.v_sb, ones_b], writes=[po, pd])
            S.op(DVE, lambda e: e.reciprocal(out=small[:, 40:56], in_=pd[:, 0:16]), reads=[pd], writes=[small])
            S.op(DVE, lambda e: e.tensor_tensor(out=L.attT[:, :, 0], in0=po[:, 0:16], in1=small[:, 40:56], op=ALU.mult),
                 reads=[po, small], writes=[L.attT[:, :, 0]])

        ones_f = sb("ones_f", [128, 128], F32)
        S.op(DVE, lambda e: e.memset(ones_f[:, :], 1.0), writes=[ones_f])

        def ones_f_ap():
            return ones_f[:, :]

        def run_block(src, dst, tok0, row0, T, is_sample, prefix=False):
            L = layout(T)
            tp, nt = L.tp, L.nt
            S.new_epoch()
            if is_sample:
                S.dma(SP, lambda e: e.dma_start(out=rs_p.rearrange("h d e -> d h e"), in_=state_f[:, :, :]), reads=SFK)
                S.dma(SP, lambda e: e.dma_start(out=state_f[:, :, :], in_=st_s.rearrange("h d e -> d h e")),
                      writes=SFK)
                for h in range(cfg.RH):
                    S.op(ACT, lambda e, h=h: e.copy(out=state_b[:, h, :], in_=state_f[:, h, :]),
                         reads=["state_f%d" % h], writes=["state_b%d" % h])
            for t in range(nt):
                xt = L.x_tm[:tp, t, :]
                S.dma(SP, lambda e, t=t, xt=xt: e.dma_start(out=xt, in_=src[row0 + t * 128:row0 + t * 128 + tp, :]), writes=[xt])
            ffn(L, g_f1, w_f1a, w_f1b, "f1")
            mixer(L, tok0, row0, is_sample, prefix)
            if prefix:
                return
            ffn(L, g_f2, w_f2a, w_f2b, "f2")
            for t in range(nt):
                xt = L.x_tm[:tp, t, :]
                S.dma(SP, lambda e, t=t, xt=xt: e.dma_start(out=dst[row0 + t * 128:row0 + t * 128 + tp, :], in_=xt), reads=[xt])

        for pb in range(NPRE // TB):
            run_block(xpre, None, pb * TB, pb * TB, TB, False, prefix=True)
        sfv = state_f[:, :, :].rearrange("p h e -> p (h e)")
        S.op(DVE, lambda e: e.tensor_scalar(out=sfv, in0=sfv, scalar1=flag_sb[:, 0:1], scalar2=None, op0=ALU.mult),
             reads=SFK + [flag_sb], writes=SFK)
        for h in range(cfg.RH):
            S.op(ACT, lambda e, h=h: e.copy(out=state_b[:, h, :], in_=state_f[:, h, :]),
                 reads=["state_f%d" % h], writes=["state_b%d" % h])
        for b in range(NOWN // TB):
            run_block(xo, y_p, NPRE + b * TB, b * TB, TB, False)
        if cfg.WITH_SAMPLE:
            run_block(xs, y_s, 0, 0, 1, True)
        else:
            S.dma(SP, lambda e: e.dma_start(out=rs_p.rearrange("h d e -> d h e"), in_=state_f[:, :, :]), reads=SFK)
        S.finish()
        S.emit(nc)
    return nc


def _consts(cfg, hf):
    SEQ = cfg.SEQ
    half = 64
    inv = (10000.0 ** (-np.arange(half, dtype=np.float32) / half)).astype(np.float32)
    pos = np.concatenate([np.arange(SEQ // 2), hf * (SEQ // 2) + np.arange(SEQ // 2), [cfg.NPAGES * 128]]).astype(np.float32)
    ang = pos[:, None] * inv[None, :]
    cos, sin = np.cos(ang).astype(np.float32), np.sin(ang).astype(np.float32)
    log_g = np.log1p(-np.exp2(-5.0 - np.arange(cfg.RH, dtype=np.float32))).astype(np.float32)
    i_in = np.concatenate([np.arange(SEQ) % 128, [0]]).astype(np.float32)
    dq = np.exp(log_g[None, :] * (i_in[:, None] + 1.0)).astype(np.float32)
    dk = (np.exp(-log_g[None, :] * (i_in[:, None] + 1.0)) * (128 ** -0.5)).astype(np.float32)
    rope = np.stack([cos[:, None, :] * dq[:, :, None], sin[:, None, :] * dq[:, :, None],
                     cos[:, None, :] * dk[:, :, None], sin[:, None, :] * dk[:, :, None]], axis=1)
    j = np.arange(128)
    return {
        "c_rope": np.ascontiguousarray(rope.astype(np.float32)),
        "c_ident": np.eye(128, dtype=np.float32),
        "c_caus": (j[:, None] <= j[None, :]).astype(np.float32),
        "c_negu": np.where(j[None, :] > j[:, None], -1e30, 0.0).astype(np.float32),
        "c_iota": np.broadcast_to(np.arange(256, dtype=np.float32)[None, :], (128, 256)).copy(),
        "c_lstr": (j[:, None] < j[None, :]).astype(np.float32),
        "c_jcol": np.broadcast_to(j.astype(np.float32)[None, :], (128, 128)).copy(),
        "c_dslot": np.stack([j, j + 128], axis=1).astype(np.float32),
        "c_pow2": np.broadcast_to((2.0 ** -np.arange(32, dtype=np.float32))[None, :], (128, 32)).copy(),
    }


def kernel(x_prompt, x_sample, state_ret, cache_k, cache_v, cache_idx_k, page_table,
           ffn1_norm, ffn1_w1, ffn1_w2, mix_norm, w_in, q_norm, k_norm, ret_norm,
           w_ret_out, w_att_out, w_o, ffn2_norm, ffn2_w1, ffn2_w2, _cfg=None):
    cfg = _cfg or Cfg
    f = lambda a: np.ascontiguousarray(np.asarray(a))
    nc = build_program(cfg)
    consts = [_consts(cfg, 0), _consts(cfg, 1)]
    HS = cfg.SEQ // 2
    B = x_prompt.shape[0]
    NCO = cfg.NCORES
    shared = {
        "ffn1_w1": f(ffn1_w1[0]), "ffn1_w2": f(ffn1_w2[0]), "ffn2_w1": f(ffn2_w1[0]), "ffn2_w2": f(ffn2_w2[0]),
        "w_in": f(w_in[0]), "w_ret_out": f(w_ret_out[0]), "w_att_out": f(w_att_out[0]), "w_o": f(w_o[0]),
        "ffn1_norm": f(ffn1_norm), "mix_norm": f(mix_norm), "ffn2_norm": f(ffn2_norm),
        "q_norm": f(q_norm), "k_norm": f(k_norm), "ret_norm": f(ret_norm),
    }
    if cfg.WITH_SAMPLE:
        shared["cache_k"] = f(cache_k[0]).reshape(cfg.NPHYS * 128, 512)
        shared["cache_v"] = f(cache_v[0]).reshape(cfg.NPHYS * 128, 512)
        shared["cache_ik"] = f(cache_idx_k[0]).reshape(cfg.NPHYS * 4, 2048)
    in_maps = []
    for c in range(NCO):
        m = dict(shared)
        b_, hf = (c // 2) % B, c % 2
        m.update(consts[hf])
        m["xo"] = f(x_prompt[b_, hf * HS:(hf + 1) * HS])
        m["xpre"] = f(x_prompt[b_, 0:HS])
        m["flag"] = np.full((128, 1), float(hf), np.float32)
        m["negflag"] = np.full((128, 1), 0.0 if hf else -1e30, np.float32)
        m["xs"] = f(x_sample[c % 8])
        m["st_s"] = f(state_ret[0, c % 8])
        if cfg.WITH_SAMPLE:
            m["ptab"] = f(page_table[c % 8]).reshape(128, 1).astype(np.int32)
        in_maps.append(m)
    res = run_bass_kernel_spmd(nc, in_maps, core_ids=list(range(NCO)))
    r = res.results
    cs = lambda c: r[c % NCO]
    cat = lambda b, k: np.concatenate([cs(2 * b)[k], cs(2 * b + 1)[k]], axis=0)
    y_prompt = np.stack([cat(b, "y_p") for b in range(B)])
    y_sample = np.stack([cs(c)["y_s"] for c in range(8)])
    rsp = np.stack([cs(2 * b + 1)["rs_p"] for b in range(B)])[None]
    kp = np.stack([cat(b, "k_p") for b in range(B)]).reshape(1, B, cfg.SEQ, 4, 128)
    vp = np.stack([cat(b, "v_p") for b in range(B)]).reshape(1, B, cfg.SEQ, 4, 128)
    ikp = np.stack([cat(b, "ik_p") for b in range(B)])[None]
    rss = np.stack([cs(c)["rs_s"] for c in range(8)])[None]
    ks = np.stack([cs(c)["k_s"] for c in range(8)]).reshape(1, 8, 1, 4, 128)
    vs = np.stack([cs(c)["v_s"] for c in range(8)]).reshape(1, 8, 1, 4, 128)
    iks = np.stack([cs(c)["ik_s"] for c in range(8)]).reshape(1, 8, 1, 64)
    return (y_prompt, y_sample, rsp, kp, vp, ikp, rss, ks, vs, iks)
```

```python
import contextlib
import numpy as np
import concourse.bass as bass
import concourse.mybir as mybir
from concourse.bass_utils import run_bass_kernel_spmd

F32 = mybir.dt.float32
BF16 = mybir.dt.bfloat16
I32 = mybir.dt.int32
AF = mybir.ActivationFunctionType
ALU = mybir.AluOpType
AX = mybir.AxisListType

PE, ACT, DVE, POOL, SP = "pe", "act", "dve", "pool", "sp"


class Cfg:
    D = 2048
    DFF = 5632
    SEQ = 2048
    TB = 512
    NPAGES = 128
    NPHYS = 1280
    RH = 8
    EPS = 1e-6
    TOPK = 256
    NIT = 20
    WITH_SAMPLE = True
    NCORES = 8
    STAGE = 99


O_RQ, O_RK, O_RV, O_RG = 0, 1024, 2048, 4096
O_AQ, O_AK, O_AV = 6144, 8192, 8704
O_IQ, O_IK, O_IW = 9216, 10240, 10304
O_GR, O_GA = 10320, 12368
IN_W = 14416
IDX_W_SCALE = (16 ** -0.5) * (64 ** -0.5)
NEG = -30000.0
ARENA = 47104
GRAN = 512


class Sched:
    NDMA = 24

    def __init__(self):
        self.ops = {e: [] for e in (PE, ACT, DVE, POOL, SP)}
        self.epoch = 0
        self.count = {}
        self.waited = {e: {} for e in (PE, ACT, DVE, POOL, SP)}
        self.last_w = {}
        self.readers = {}
        self.dma_i = 0
        self.dma_cnt = [0] * self.NDMA
        self.semkeys = []
        self.keyfn = None

    def new_epoch(self):
        self.epoch += 1

    def _key(self, eng):
        k = (eng, self.epoch)
        if k not in self.count:
            self.count[k] = 0
            self.semkeys.append(k)
        return k

    def _norm(self, items):
        out = []
        for it in items:
            if isinstance(it, (str, tuple)):
                out.append(it)
            elif isinstance(it, list):
                out.extend(self._norm(it))
            else:
                out.extend(self.keyfn(it))
        return out

    def _deps(self, reads, writes):
        deps = {}
        for r in reads:
            if r in self.last_w:
                sk, v = self.last_w[r]
                deps[sk] = max(deps.get(sk, 0), v)
        for w in writes:
            if w in self.last_w:
                sk, v = self.last_w[w]
                deps[sk] = max(deps.get(sk, 0), v)
            for sk, v in self.readers.get(w, ()):
                deps[sk] = max(deps.get(sk, 0), v)
        return deps

    def _waits(self, eng, deps, skip=None):
        waits = []
        for sk, v in deps.items():
            if skip is not None and sk == skip:
                continue
            if self.waited[eng].get(sk, 0) >= v:
                continue
            self.waited[eng][sk] = v
            waits.append((sk, v))
        return waits

    def _commit(self, me, reads, writes):
        for r in reads:
            lst = self.readers.setdefault(r, [])
            lst[:] = [x for x in lst if x[0] != me[0]] + [me]
        for w in writes:
            self.last_w[w] = me
            self.readers[w] = []

    def op(self, eng, emit, reads=(), writes=()):
        reads, writes = self._norm(reads), self._norm(writes)
        sk = self._key(eng)
        deps = self._deps(reads, writes)
        waits = self._waits(eng, deps, skip=sk if eng == PE else None)
        self.count[sk] += 1
        me = (sk, self.count[sk])
        self.ops[eng].append((waits, emit, (sk, 1), self.count[sk]))
        self._commit(me, reads, writes)
        return me

    def dma(self, eng, emit, reads=(), writes=()):
        reads, writes = self._norm(reads), self._norm(writes)
        i = self.dma_i % self.NDMA
        self.dma_i += 1
        sk = ("dma", i)
        if sk not in self.semkeys:
            self.semkeys.append(sk)
        deps = self._deps(reads, writes)
        if self.dma_cnt[i] > 0:
            deps[sk] = max(deps.get(sk, 0), 16 * self.dma_cnt[i])
        waits = self._waits(eng, deps)
        self.dma_cnt[i] += 1
        me = (sk, 16 * self.dma_cnt[i])
        self.ops[eng].append((waits, emit, (sk, 16), None))
        self._commit(me, reads, writes)
        return me

    def finish(self, eng=SP):
        deps = {("dma", i): 16 * c for i, c in enumerate(self.dma_cnt) if c > 0}
        waits = self._waits(eng, deps)
        self.ops[eng].append((waits, None, None, None))

    def emit(self, nc):
        sems = {}
        needed = {}
        for eng_ops in self.ops.values():
            for waits, _, _, _ in eng_ops:
                for sk, v in waits:
                    if sk[0] != "dma":
                        needed.setdefault(sk, set()).add(v)
        rank = {sk: {v: i + 1 for i, v in enumerate(sorted(vs))} for sk, vs in needed.items()}
        with contextlib.ExitStack() as st:
            for i, sk in enumerate(self.semkeys):
                sems[sk] = st.enter_context(nc.semaphore("s%d" % i))
            block = st.enter_context(nc.Block())

            def run(eng_name):
                def f(e):
                    for waits, emit, inc, idx in self.ops[eng_name]:
                        for sk, v in waits:
                            e.wait_ge(sems[sk], v if sk[0] == "dma" else rank[sk][v])
                        if emit is None:
                            continue
                        ins = emit(e)
                        if idx is None:
                            ins.then_inc(sems[inc[0]], inc[1])
                        elif idx in needed.get(inc[0], ()):
                            ins.then_inc(sems[inc[0]], 1)
                return f

            block.tensor(run(PE))
            block.scalar(run(ACT))
            block.vector(run(DVE))
            block.gpsimd(run(POOL))
            block.sync(run(SP))


class Ring:
    def __init__(self, items):
        self.items = items
        self.i = 0

    def next(self):
        it = self.items[self.i % len(self.items)]
        self.i += 1
        return it


def build_program(cfg):
    D, DFF, SEQ, TB = cfg.D, cfg.DFF, cfg.SEQ, cfg.TB
    KT = D // 128
    KTF = DFF // 128
    NTB = TB // 128
    NKEY = SEQ
    NKB = NKEY // 128
    nc = bass.Bass("TRN2", target_bir_lowering=False)
    S = Sched()

    def din(name, shape, dt=F32):
        return nc.dram_tensor(name, list(shape), dt, kind="ExternalInput").ap()

    def dout(name, shape, dt=F32):
        return nc.dram_tensor(name, list(shape), dt, kind="ExternalOutput").ap()

    NPRE = SEQ // 2
    NOWN = SEQ // 2
    xo = din("xo", [NOWN, D])
    xpre = din("xpre", [NPRE, D])
    flag_d = din("flag", [128, 1])
    negflag_d = din("negflag", [128, 1])
    xs = din("xs", [1, D])
    st_s = din("st_s", [cfg.RH, 128, 256])
    if cfg.WITH_SAMPLE:
        cache_k = din("cache_k", [cfg.NPHYS * 128, 512])
        cache_v = din("cache_v", [cfg.NPHYS * 128, 512])
        cache_ik = din("cache_ik", [cfg.NPHYS * 4, 2048])
        ptab = din("ptab", [128, 1], I32)
    w_f1a = din("ffn1_w1", [D, 2 * DFF])
    w_f1b = din("ffn1_w2", [DFF, D])
    w_f2a = din("ffn2_w1", [D, 2 * DFF])
    w_f2b = din("ffn2_w2", [DFF, D])
    w_in = din("w_in", [D, IN_W])
    w_ro = din("w_ret_out", [D, D])
    w_ao = din("w_att_out", [D, D])
    w_o = din("w_o", [D, D])
    g_f1 = din("ffn1_norm", [1, D])
    g_mx = din("mix_norm", [1, D])
    g_f2 = din("ffn2_norm", [1, D])
    g_q = din("q_norm", [1, 128])
    g_k = din("k_norm", [1, 128])
    g_r = din("ret_norm", [1, 256])
    c_rope = din("c_rope", [SEQ + 1, 4, cfg.RH, 64])
    c_ident = din("c_ident", [128, 128])
    c_caus = din("c_caus", [128, 128])
    c_negu = din("c_negu", [128, 128])
    c_iota = din("c_iota", [128, 256])
    c_lstr = din("c_lstr", [128, 128])
    c_jcol = din("c_jcol", [128, 128])
    c_dslot = din("c_dslot", [128, 2])
    c_pow2 = din("c_pow2", [128, 32])

    y_p = dout("y_p", [NOWN, D])
    y_s = dout("y_s", [1, D])
    rs_p = dout("rs_p", [cfg.RH, 128, 256])
    k_p = dout("k_p", [NOWN, 512])
    v_p = dout("v_p", [NOWN, 512])
    ik_p = dout("ik_p", [NOWN, 64])
    rs_s = dout("rs_s", [cfg.RH, 128, 256])
    k_s = dout("k_s", [1, 512])
    v_s = dout("v_s", [1, 512])
    ik_s = dout("ik_s", [1, 64])
    sc_x = nc.dram_tensor("sc_x", [TB, D], F32, kind="Internal").ap()
    sc_gr = nc.dram_tensor("sc_gr", [TB, D], BF16, kind="Internal").ap()
    sc_ga = nc.dram_tensor("sc_ga", [TB, D], BF16, kind="Internal").ap()
    NSLOT = 128
    wcache = nc.dram_tensor("wcache", [NSLOT, 128, 8192], BF16, kind="Internal").ap()
    wslots = {}

    with contextlib.ExitStack() as ctx:
        def sb(name, shape, dt):
            return ctx.enter_context(nc.sbuf_tensor(name, list(shape), dt))

        def pt(name, shape, dt):
            return ctx.enter_context(nc.psum_tensor(name, list(shape), dt))

        arena = sb("arena", [128, ARENA], BF16)

        def keyfn(ap):
            t_ = getattr(ap, "tensor", None)
            name = t_.name if t_ is not None else ap.name
            if name != "arena":
                return [name]
            esz = 2 if ap.dtype == BF16 else 4
            rowlen = (ARENA * 2) // esz
            off = int(ap.offset) % rowlen
            ext = sum((n - 1) * abs(s) for s, n in list(ap.ap)[1:]) + 1
            lo = (off * esz) // (2 * GRAN)
            hi = ((off + ext) * esz - 1) // (2 * GRAN)
            return [("A", g) for g in range(lo, hi + 1)]

        S.keyfn = keyfn

        def AV(off, n, dt=BF16):
            assert off % 2 == 0 and off + n <= ARENA, (off, n)
            v = arena[:, off:off + n]
            return v if dt == BF16 else v.bitcast(dt)

        ident_f = sb("ident_f", [128, 128], F32)
        ident_b = sb("ident_b", [128, 128], BF16)
        ones_b = sb("ones_b", [128, 128], BF16)
        caus = sb("caus", [128, 128], F32)
        negu = sb("negu", [128, 128], F32)
        gq_bc = sb("gq_bc", [128, 128], F32)
        gk_bc = sb("gk_bc", [128, 128], F32)
        gr_bc = sb("gr_bc", [128, 256], F32)
        kT = sb("kT", [128, 4, NKEY], BF16)
        Vc = sb("Vc", [128, NKB, 512], BF16)
        ikT2 = sb("ikT2", [128, NKEY], BF16)
        state_f = sb("state_f", [128, cfg.RH, 256], F32)
        state_b = sb("state_b", [128, cfg.RH, 256], BF16)
        wsc = sb("wsc", [128, NTB, 16], F32)
        thr = sb("thr", [128, 8], F32)
        pow2 = sb("pow2", [128, 32], F32)
        w_all = sb("w_all", [128, 32], F32)
        wring = Ring([sb("wb%d" % i, [128, 8192], BF16) for i in range(3)])
        f32s_ring = Ring([sb("fs%d" % i, [128, 512], F32) for i in range(3)])
        bfs_ring = Ring([sb("bs%d" % i, [128, 512], BF16) for i in range(3)])
        stat = sb("stat", [128, 64], F32)
        stat_i = [0]

        psL = Ring([pt("psL%d" % i, [128, 512], F32) for i in range(4)])
        psT = Ring([pt("psT%d" % i, [128, 1024], BF16) for i in range(2)])
        psM = [pt("psM%d" % i, [128, 512], F32) for i in range(2)]

        def stat_cols(n):
            if stat_i[0] + n > 64:
                stat_i[0] = 0
            a = stat_i[0]
            stat_i[0] += n
            return stat[:, a:a + n], ["stat%d" % i for i in range(a // 8, (a + n - 1) // 8 + 1)]

        deferred = []

        def flush_deferred(keep=0):
            while len(deferred) > keep:
                deferred.pop(0)()

        def cload(dst, src, eng=SP):
            S.dma(eng, lambda e: e.dma_start(out=dst, in_=src), writes=[dst])

        flagt = sb("flagt", [128, 16], F32)
        flag_sb = flagt[:, 0:1]
        negflag_sb = flagt[:, 8:9]
        cload(flag_sb, flag_d)
        cload(negflag_sb, negflag_d)
        cload(ident_f[:, :], c_ident)
        cload(pow2[:, :], c_pow2)
        cload(caus[:, :], c_caus)
        cload(negu[:, :], c_negu)
        cload(gk_bc[:, :], g_k.partition_broadcast(128))
        cload(gq_bc[:, :], g_q.partition_broadcast(128))
        cload(gr_bc[:, :], g_r.partition_broadcast(128))
        S.op(DVE, lambda e: e.tensor_copy(out=ident_b[:, :], in_=ident_f[:, :]), reads=[ident_f], writes=[ident_b])
        S.op(DVE, lambda e: e.memset(ones_b[:, :], 1.0), writes=[ones_b])
        S.op(DVE, lambda e: e.tensor_scalar(out=gq_bc[:, :], in0=gq_bc[:, :], scalar1=128 ** -0.5, scalar2=None,
                                            op0=ALU.mult), reads=[gq_bc], writes=[gq_bc])
        SFK = ["state_f%d" % h for h in range(cfg.RH)]
        SBK = ["state_b%d" % h for h in range(cfg.RH)]
        S.op(DVE, lambda e: e.memset(state_f[:, :, :], 0.0), writes=SFK)
        S.op(DVE, lambda e: e.memset(state_b[:, :, :], 0.0), writes=SBK)

        def rstd_from_ss(ss_ap, ss_keys, n, tp, inv_n):
            ms, msk = stat_cols(n)
            sd, sdk = stat_cols(n)
            rs, rsk = stat_cols(n)
            S.op(DVE, lambda e: e.tensor_scalar(out=ms[:tp], in0=ss_ap, scalar1=inv_n, scalar2=cfg.EPS,
                                                op0=ALU.mult, op1=ALU.add), reads=ss_keys, writes=msk)
            S.op(ACT, lambda e: e.activation(out=sd[:tp], in_=ms[:tp], func=AF.Sqrt), reads=msk, writes=sdk)
            S.op(DVE, lambda e: e.reciprocal(out=rs[:tp], in_=sd[:tp]), reads=sdk, writes=rsk)
            return rs, rsk

        def transpose_to(src_ap, tp, ncols, dst_fn):
            nj_all = ncols // 128
            j0 = 0
            while j0 < nj_all:
                nj = min(8, nj_all - j0)
                ps = psT.next()

                def emit_t(e, ps=ps, j0=j0, nj=nj):
                    ins = None
                    for j in range(nj):
                        ins = e.transpose(out=ps[:, j * 128:j * 128 + tp],
                                          in_=src_ap[:tp, (j0 + j) * 128:(j0 + j + 1) * 128],
                                          identity=ident_b[:tp, :tp])
                    return ins
                S.op(PE, emit_t, reads=[src_ap[:tp, :], ident_b], writes=[ps])
                dst = dst_fn(j0, nj)
                src_ps = ps[:, 0:nj * 128].rearrange("p (j t) -> p j t", j=nj)[:, :, 0:tp]
                S.op(ACT, lambda e, dst=dst, src_ps=src_ps: e.copy(out=dst, in_=src_ps), reads=[ps], writes=[dst])
                j0 += nj

        class Lay:
            pass

        def layout(T):
            L = Lay()
            tp = min(T, 128)
            nt = (T + 127) // 128
            L.tp, L.nt, L.T = tp, nt, T
            if T > 1:
                X0, A0, H0 = 0, 16384, 24576
                L.x_tm = AV(X0, 16384, F32).rearrange("p (t d) -> p t d", t=NTB)
                L.actT = AV(A0, 8192).rearrange("p (k t) -> p k t", k=KT)
                L.hT = AV(H0, KTF * TB).rearrange("p (k t) -> p k t", k=KTF)
                L.gain_bc = AV(H0, 2 * D, F32)
                L.xn = AV(H0 + 2 * D, D)
                L.xn2 = AV(H0 + 3 * D, D)
                L.aqT = AV(0, 8192).rearrange("p (k t) -> p k t", k=16)
                L.iqT = AV(8192, 4096).rearrange("p (k t) -> p k t", k=8)
                L.diagW = AV(12288, 2048).rearrange("p (h t) -> p h t", h=16)
                L.mb = AV(14336, 2048)
                L.attT = AV(H0, 8192).rearrange("p (k t) -> p k t", k=16)
                L.S_sb = AV(32768, 4096, F32)
                L.junk = AV(36864, 2048)
                L.Rring = Ring([AV(38912 + 512 * i, 512) for i in range(4)])
                L.pTring = Ring([AV(40960 + 512 * i, 512) for i in range(3)])
                L.rden = AV(42496, 1024, F32)
                L.mb2 = AV(43520, 2048)
                L.ogT = AV(32768, 8192).rearrange("p (k t) -> p k t", k=16)
                L.qrT = AV(0, 2048).rearrange("p (h t) -> p h t", h=4)
                L.krT = AV(2048, 2048).rearrange("p (h t) -> p h t", h=4)
                L.ktm = AV(4096, 2048).rearrange("p (t c) -> p t c", t=NTB)
                L.vtm = AV(6144, 4096).rearrange("p (t c) -> p t c", t=NTB)
                L.sgtm = AV(10240, 4096).rearrange("p (t c) -> p t c", t=NTB)
                L.rope = Ring([AV(14336 + 1024 * i, 1024, F32).rearrange("p (a h f) -> p a h f", a=2, h=4) for i in range(2)])
                L.o_raw = AV(40960, 2048, F32).rearrange("p (h e) -> p h e", h=4)
                L.G = AV(43008, 1024)
                L.og_tm = AV(44032, 1024)
                L.sTm = Ring([AV(45056 + 128 * i, 128) for i in range(4)])
                L.m_tm = AV(0, 8192).rearrange("p (t d) -> p t d", t=NTB)
                L.gtr = Ring([AV(8192 + 512 * i, 512) for i in range(3)])
            else:
                b = [20480]

                def al(n):
                    o = b[0]
                    b[0] += (n + 63) // 64 * 64
                    return o
                L.x_tm = AV(al(4096), 4096, F32).rearrange("p (t d) -> p t d", t=1)
                L.gain_bc = AV(0, 2 * D, F32)
                L.xn = AV(2 * D, D)
                L.xn2 = AV(3 * D, D)
                L.actT = AV(al(16), 16).rearrange("p (k t) -> p k t", k=KT)
                L.hT = AV(al(KTF), KTF).rearrange("p (k t) -> p k t", k=KTF)
                L.aqT = AV(al(16), 16).rearrange("p (k t) -> p k t", k=16)
                L.attT = AV(al(16), 16).rearrange("p (k t) -> p k t", k=16)
                L.ogT = AV(al(16), 16).rearrange("p (k t) -> p k t", k=16)
                L.qrT = AV(al(4), 4).rearrange("p (h t) -> p h t", h=4)
                L.krT = AV(al(4), 4).rearrange("p (h t) -> p h t", h=4)
                L.ktm = AV(al(512), 512).rearrange("p (t c) -> p t c", t=1)
                L.vtm = AV(al(1024), 1024).rearrange("p (t c) -> p t c", t=1)
                L.sgtm = AV(al(1024), 1024).rearrange("p (t c) -> p t c", t=1)
                L.rope = Ring([AV(al(1024), 1024, F32).rearrange("p (a h f) -> p a h f", a=2, h=4) for i in range(2)])
                L.o_raw = AV(al(2048), 2048, F32).rearrange("p (h e) -> p h e", h=4)
                L.G = AV(al(1024), 1024)
                L.og_tm = AV(al(1024), 1024)
                L.sTm = Ring([AV(al(128), 128) for i in range(4)])
                L.m_tm = AV(al(2048), 2048).rearrange("p (t d) -> p t d", t=1)
                L.gtr = Ring([AV(al(512), 512) for i in range(3)])
                L.iqTs = AV(al(16), 16)
                L.ikTs = AV(al(2), 2)
                L.wcol = AV(al(2), 2)
                L.akTs = AV(al(4), 4)
                L.kn_s = AV(al(512), 512)
                L.v_sb = AV(al(512), 512)
                L.ik_sb = AV(al(64), 64)
                L.iw_s = AV(al(32), 32, F32)
                assert b[0] <= ARENA
            return L

        def rmsnorm_transpose(L, gain_dram):
            tp, nt = L.tp, L.nt
            gain_bc = L.gain_bc
            S.dma(SP, lambda e: e.dma_start(out=gain_bc[:, :], in_=gain_dram.partition_broadcast(128)),
                  writes=[gain_bc])
            xns = [L.xn, L.xn2]
            ss, ssk = stat_cols(nt)
            for t in range(nt):
                xn = xns[t % 2]
                xt = L.x_tm[:tp, t, :]
                S.op(DVE, lambda e, xt=xt, xn=xn, t=t: e.scalar_tensor_tensor(
                    out=xn[:tp, :], in0=xt, scalar=1.0, in1=xt, op0=ALU.mult, op1=ALU.mult, accum_out=ss[:tp, t:t + 1]),
                    reads=[xt], writes=[xn] + ssk)
            rs, rsk = rstd_from_ss(ss[:tp], ssk, nt, tp, 1.0 / D)
            for t in range(nt):
                xn = xns[t % 2]
                xt = L.x_tm[:tp, t, :]
                S.op(DVE, lambda e, xt=xt, xn=xn, t=t: e.scalar_tensor_tensor(
                    out=xn[:tp, :], in0=xt, scalar=rs[:tp, t:t + 1], in1=gain_bc[:tp, :], op0=ALU.mult, op1=ALU.mult),
                    reads=[xt, gain_bc] + rsk, writes=[xn])
                transpose_to(xn, tp, D, lambda j0, nj, t=t: L.actT[:, j0:j0 + nj, t * 128:t * 128 + tp])

        def linear(L, act_ap, kt, w_dram, chunks, consumer, wkey):
            tp, nt = L.tp, L.nt
            wv = w_dram.rearrange("(k p) n -> p k n", p=128)
            for ci, blocks in enumerate(chunks):
                ncols = sum(n for _, n in blocks)
                kk_max = 8192 // ncols
                ksplits = [(k0, min(kk_max, kt - k0)) for k0 in range(0, kt, kk_max)]
                pss = [psL.next() for _ in range(nt)] if len(ksplits) > 1 else None
                for si, (k0, kk) in enumerate(ksplits):
                    wb = getattr(L, "wring", wring).next()
                    wbv = wb[:, 0:kk * ncols].rearrange("p (k n) -> p k n", k=kk)
                    skey = (wkey, ci, si)
                    nel = kk * ncols
                    if skey not in wslots:
                        slot = len(wslots)
                        assert slot < NSLOT
                        wslots[skey] = slot
                        off = 0
                        for (c0, n) in blocks:
                            S.dma(POOL, lambda e, wbv=wbv, off=off, n=n, c0=c0, k0=k0, kk=kk: e.dma_start(
                                out=wbv[:, :, off:off + n], in_=wv[:, k0:k0 + kk, c0:c0 + n]), writes=[wb])
                            off += n
                        S.dma(SP, lambda e, wb=wb, slot=slot, nel=nel: e.dma_start(
                            out=wcache[slot, :, 0:nel], in_=wb[:, 0:nel]), reads=[wb], writes=["wc%d" % slot])
                    else:
                        slot = wslots[skey]
                        S.dma(POOL, lambda e, wb=wb, slot=slot, nel=nel: e.dma_start(
                            out=wb[:, 0:nel], in_=wcache[slot, :, 0:nel]), reads=["wc%d" % slot], writes=[wb])
                    for t in range(nt):
                        flush_deferred(keep=2)
                        ps = psL.next() if pss is None else pss[t]

                        def emit_mm(e, ps=ps, t=t, k0=k0, kk=kk, wbv=wbv, ncols=ncols, si=si):
                            ins = None
                            for k in range(kk):
                                ins = e.matmul(ps[:tp, 0:ncols],
                                               lhsT=act_ap[:, k0 + k, t * 128:t * 128 + tp],
                                               rhs=wbv[:, k, :],
                                               start=(si == 0 and k == 0),
                                               stop=(si == len(ksplits) - 1 and k == kk - 1))
                            return ins
                        S.op(PE, emit_mm, reads=[wb, act_ap[:, :, t * 128:t * 128 + tp]], writes=[ps])
                        if si == len(ksplits) - 1:
                            consumer(ci, t, ps[:tp, 0:ncols], ps)
            flush_deferred(0)

        def ffn(L, gain_dram, w1, w2, wk):
            tp = L.tp
            rmsnorm_transpose(L, gain_dram)
            nch = DFF // 256

            def cons_h(ci, t, ps, pst):
                sg = f32s_ring.next()
                hb = bfs_ring.next()
                S.op(ACT, lambda e: e.activation(out=sg[:tp, 0:256], in_=ps[:, 0:256], func=AF.Silu),
                     reads=[pst], writes=[sg])
                S.op(DVE, lambda e: e.tensor_tensor(out=hb[:tp, 0:256], in0=ps[:, 256:512], in1=sg[:tp, 0:256],
                                                    op=ALU.mult), reads=[pst, sg], writes=[hb])
                deferred.append(lambda: transpose_to(
                    hb, tp, 256, lambda j0, nj: L.hT[:, 2 * ci + j0:2 * ci + j0 + nj, t * 128:t * 128 + tp]))

            linear(L, L.actT, KT, w1, [[(s * 256, 256), (DFF + s * 256, 256)] for s in range(nch)], cons_h, wk + "a")

            def cons_y(ci, t, ps, pst):
                xs_ = L.x_tm[:tp, t, ci * 256:(ci + 1) * 256]
                S.op(DVE, lambda e: e.scalar_tensor_tensor(out=xs_, in0=ps, scalar=0.5, in1=xs_,
                                                           op0=ALU.mult, op1=ALU.add),
                     reads=[pst, xs_], writes=[xs_])

            linear(L, L.hT, KTF, w2, [[(n * 256, 256)] for n in range(D // 256)], cons_y, wk + "b")

        def headnorm(tp, ps, pst, gbc):
            raw = f32s_ring.next()
            sq = f32s_ring.next()
            S.op(ACT, lambda e: e.copy(out=raw[:tp, :], in_=ps), reads=[pst], writes=[raw])
            S.op(DVE, lambda e: e.tensor_tensor(out=sq[:tp, :], in0=raw[:tp, :], in1=raw[:tp, :], op=ALU.mult),
                 reads=[raw], writes=[sq])
            ss, ssk = stat_cols(4)
            S.op(DVE, lambda e: e.tensor_reduce(out=ss[:tp], in_=sq[:tp, :].rearrange("p (h d) -> p h d", h=4),
                                                axis=AX.X, op=ALU.add), reads=[sq], writes=ssk)
            rs, rsk = rstd_from_ss(ss[:tp], ssk, 4, tp, 1.0 / 128)
            S.op(DVE, lambda e: e.tensor_tensor(
                out=sq[:tp, :].rearrange("p (h d) -> p h d", h=4),
                in0=raw[:tp, :].rearrange("p (h d) -> p h d", h=4),
                in1=rs[:tp].unsqueeze(2).to_broadcast([tp, 4, 128]), op=ALU.mult),
                reads=[raw] + rsk, writes=[sq])
            S.op(DVE, lambda e: e.tensor_tensor(
                out=raw[:tp, :].rearrange("p (h d) -> p h d", h=4),
                in0=sq[:tp, :].rearrange("p (h d) -> p h d", h=4),
                in1=gbc[:tp, :].unsqueeze(1).to_broadcast([tp, 4, 128]), op=ALU.mult),
                reads=[sq, gbc], writes=[raw])
            return raw

        def mixer(L, tok0, row0, is_sample, prefix=False):
            tp, nt, T = L.tp, L.nt, L.T
            rmsnorm_transpose(L, g_mx)
            for t in range(nt if not prefix else 0):
                xt = L.x_tm[:tp, t, :]
                S.dma(SP, lambda e, t=t, xt=xt: e.dma_start(out=sc_x[t * 128:t * 128 + tp, :], in_=xt),
                      reads=[xt], writes=["sc_x%d" % t])
            k_out, v_out, ik_out = (k_s, v_s, ik_s) if is_sample else (k_p, v_p, ik_p)

            def rows(t):
                return slice(row0 + t * 128, row0 + t * 128 + tp)

            def cons_gate(dst):
                def f(ci, t, ps, pst):
                    gb = bfs_ring.next()
                    S.op(ACT, lambda e: e.activation(out=gb[:tp, :], in_=ps, func=AF.Sigmoid), reads=[pst], writes=[gb])
                    S.dma(SP, lambda e: e.dma_start(out=dst[t * 128:t * 128 + tp, ci * 512:(ci + 1) * 512], in_=gb[:tp, :]),
                          reads=[gb], writes=["%s_%d_%d" % (dst.tensor.name, t, ci)])
                return f
            if not prefix:
                linear(L, L.actT, KT, w_in, [[(O_GR + c * 512, 512)] for c in range(4)], cons_gate(sc_gr), "gr")
                linear(L, L.actT, KT, w_in, [[(O_GA + c * 512, 512)] for c in range(4)], cons_gate(sc_ga), "ga")

            def cons_ak(ci, t, ps, pst):
                kn = headnorm(tp, ps, pst, gk_bc)
                if not prefix:
                    S.dma(SP, lambda e: e.dma_start(out=k_out[rows(t), :], in_=kn[:tp, :]), reads=[kn])
                kb = bfs_ring.next()
                S.op(ACT, lambda e: e.copy(out=kb[:tp, :], in_=kn[:tp, :]), reads=[kn], writes=[kb])
                if is_sample:
                    S.op(DVE, lambda e: e.tensor_copy(out=L.kn_s[:1, :], in_=kn[:1, :]), reads=[kn], writes=[L.kn_s])
                    deferred.append(lambda: transpose_to(kb, tp, 512, lambda j0, nj: L.akTs[:, j0:j0 + nj].unsqueeze(2)))
                else:
                    deferred.append(lambda: transpose_to(
                        kb, tp, 512, lambda j0, nj: kT[:, j0:j0 + nj, tok0 + t * 128:tok0 + t * 128 + tp]))

            def cons_av(ci, t, ps, pst):
                raw = f32s_ring.next()
                S.op(ACT, lambda e: e.copy(out=raw[:tp, :], in_=ps), reads=[pst], writes=[raw])
                if not prefix:
                    S.dma(SP, lambda e: e.dma_start(out=v_out[rows(t), :], in_=raw[:tp, :]), reads=[raw])
                dstv = L.v_sb[:1, :] if is_sample else Vc[:tp, tok0 // 128 + t, :]
                S.op(DVE, lambda e: e.tensor_copy(out=dstv, in_=raw[:tp, :]), reads=[raw], writes=[dstv])

            def cons_ik(ci, t, ps, pst):
                raw = f32s_ring.next()
                S.op(ACT, lambda e: e.copy(out=raw[:tp, 0:80], in_=ps), reads=[pst], writes=[raw])
                if not prefix:
                    S.dma(SP, lambda e: e.dma_start(out=ik_out[rows(t), :], in_=raw[:tp, 0:64]), reads=[raw])
                if is_sample:
                    S.op(DVE, lambda e: e.tensor_copy(out=L.ik_sb[:1, :], in_=raw[:1, 0:64]), reads=[raw], writes=[L.ik_sb])
                    S.op(DVE, lambda e: e.tensor_scalar(out=L.iw_s[:1, 0:16], in0=raw[:1, 64:80], scalar1=IDX_W_SCALE,
                                                        scalar2=None, op0=ALU.mult), reads=[raw], writes=[L.iw_s])
                else:
                    ib = bfs_ring.next()
                    S.op(DVE, lambda e: e.tensor_copy(out=ib[:tp, 0:64], in_=raw[:tp, 0:64]), reads=[raw], writes=[ib])
                    S.op(DVE, lambda e: e.tensor_copy(out=ib[:tp, 64:128], in_=raw[:tp, 0:64]), reads=[raw], writes=[ib])
                    S.op(DVE, lambda e: e.tensor_scalar(out=wsc[:tp, t, :], in0=raw[:tp, 64:80], scalar1=IDX_W_SCALE,
                                                        scalar2=None, op0=ALU.mult), reads=[raw], writes=[wsc])
                    deferred.append(lambda: transpose_to(
                        ib, tp, 128, lambda j0, nj: ikT2[:, tok0 + t * 128:tok0 + t * 128 + tp].unsqueeze(1)))

            linear(L, L.actT, KT, w_in, [[(O_AK, 512)]], cons_ak, "ak")
            linear(L, L.actT, KT, w_in, [[(O_AV, 512)]], cons_av, "av")
            linear(L, L.actT, KT, w_in, [[(O_IK, 80)]], cons_ik, "ik")

            def cons_aq(ci, t, ps, pst):
                qn = headnorm(tp, ps, pst, gq_bc)
                qb = bfs_ring.next()
                S.op(ACT, lambda e: e.copy(out=qb[:tp, :], in_=qn[:tp, :]), reads=[qn], writes=[qb])
                deferred.append(lambda: transpose_to(
                    qb, tp, 512, lambda j0, nj: L.aqT[:, 4 * ci + j0:4 * ci + j0 + nj, t * 128:t * 128 + tp]))
            if not prefix:
                linear(L, L.actT, KT, w_in, [[(O_AQ + c * 512, 512)] for c in range(4)], cons_aq, "aq")

            def cons_iq(ci, t, ps, pst):
                qb = bfs_ring.next()
                S.op(ACT, lambda e: e.copy(out=qb[:tp, :], in_=ps), reads=[pst], writes=[qb])
                if is_sample:
                    def tr():
                        psx = psT.next()

                        def em(e):
                            ins = None
                            for j in range(8):
                                ins = e.transpose(out=psx[0:64, 2 * j:2 * j + 1], in_=qb[:1, j * 64:(j + 1) * 64],
                                                  identity=ident_b[:1, :1])
                            return ins
                        S.op(PE, em, reads=[qb, ident_b], writes=[psx])
                        dst = L.iqTs[0:64, 8 * ci:8 * ci + 8]
                        S.op(ACT, lambda e: e.copy(out=dst, in_=psx[0:64, 0:16].rearrange("p (j two) -> p j two", two=2)[:, :, 0]),
                             reads=[psx], writes=[dst])
                    deferred.append(tr)
                else:
                    deferred.append(lambda: transpose_to(
                        qb, tp, 512, lambda j0, nj: L.iqT[:, 4 * ci + j0:4 * ci + j0 + nj, t * 128:t * 128 + tp]))
            if not prefix:
                linear(L, L.actT, KT, w_in, [[(O_IQ + c * 512, 512)] for c in range(2)], cons_iq, "iq")

            if prefix:
                pass
            elif is_sample:
                decode_dsa(L)
            else:
                prompt_dsa_all(L, tok0)

            for hg in range(2):
                retention_group(L, tok0, hg, is_sample, prefix)
            if prefix:
                return
            if is_sample:
                S.dma(SP, lambda e: e.dma_start(out=rs_s.rearrange("h d e -> d h e"), in_=state_f[:, :, :]),
                      reads=SFK)

            def cons_ao(ci, t, ps, pst):
                gt = L.gtr.next()
                S.dma(SP, lambda e: e.dma_start(out=gt[:tp, :], in_=sc_ga[t * 128:t * 128 + tp, ci * 512:(ci + 1) * 512]),
                      reads=["sc_ga_%d_%d" % (t, ci)], writes=[gt])
                dst = L.m_tm[:tp, t, ci * 512:(ci + 1) * 512]
                S.op(DVE, lambda e: e.tensor_tensor(out=dst, in0=ps, in1=gt[:tp, :], op=ALU.mult),
                     reads=[pst, gt], writes=[dst])
            linear(L, L.attT, KT, w_ao, [[(c * 512, 512)] for c in range(4)], cons_ao, "ao")

            def cons_ro(ci, t, ps, pst):
                gt = L.gtr.next()
                S.dma(SP, lambda e: e.dma_start(out=gt[:tp, :], in_=sc_gr[t * 128:t * 128 + tp, ci * 512:(ci + 1) * 512]),
                      reads=["sc_gr_%d_%d" % (t, ci)], writes=[gt])
                tmp = f32s_ring.next()
                dst = L.m_tm[:tp, t, ci * 512:(ci + 1) * 512]
                S.op(DVE, lambda e: e.tensor_tensor(out=tmp[:tp, :], in0=ps, in1=gt[:tp, :], op=ALU.mult),
                     reads=[pst, gt], writes=[tmp])
                S.op(DVE, lambda e: e.tensor_tensor(out=dst, in0=tmp[:tp, :], in1=dst, op=ALU.add),
                     reads=[tmp, dst], writes=[dst])
            linear(L, L.ogT, KT, w_ro, [[(c * 512, 512)] for c in range(4)], cons_ro, "ro")
            for t in range(nt):
                transpose_to(L.m_tm[:, t, :], tp, D, lambda j0, nj, t=t: L.actT[:, j0:j0 + nj, t * 128:t * 128 + tp])
            for t in range(nt):
                xt = L.x_tm[:tp, t, :]
                S.dma(SP, lambda e, t=t, xt=xt: e.dma_start(out=xt, in_=sc_x[t * 128:t * 128 + tp, :]),
                      reads=["sc_x%d" % t], writes=[xt])

            def cons_o(ci, t, ps, pst):
                xs_ = L.x_tm[:tp, t, ci * 512:(ci + 1) * 512]
                S.op(DVE, lambda e: e.tensor_tensor(out=xs_, in0=ps, in1=xs_, op=ALU.add), reads=[pst, xs_], writes=[xs_])
            linear(L, L.actT, KT, w_o, [[(c * 512, 512)] for c in range(4)], cons_o, "wo")

        def retention_group(L, tok0, hg, is_sample, prefix=False):
            tp, nt = L.tp, L.nt
            pos0 = SEQ if is_sample else tok0

            def cons_rot(which):
                def f(ci, t, ps, pst):
                    raw = f32s_ring.next()
                    tmp = f32s_ring.next()
                    ob = bfs_ring.next()
                    rp = L.rope.next()
                    S.dma(SP, lambda e: e.dma_start(
                        out=rp[:tp, :, :, :],
                        in_=c_rope[pos0 + t * 128:pos0 + t * 128 + tp, 2 * which:2 * which + 2, 4 * hg:4 * hg + 4, :]),
                        writes=[rp[:tp, :, :, :]])
                    S.op(ACT, lambda e: e.copy(out=raw[:tp, :], in_=ps), reads=[pst], writes=[raw])
                    x = raw[:tp, :].rearrange("p (h two f) -> p h two f", h=4, two=2)
                    x1, x2 = x[:, :, 0, :], x[:, :, 1, :]
                    C = rp[:tp, 0, :, :]
                    Sn = rp[:tp, 1, :, :]
                    tv = tmp[:tp, :].rearrange("p (a h f) -> p a h f", a=2, h=4)
                    o = ob[:tp, :].rearrange("p (h two f) -> p h two f", h=4, two=2)
                    rk = [raw, rp[:tp, :, :, :]]
                    S.op(DVE, lambda e: e.tensor_tensor(out=tv[:, 0], in0=x1, in1=C, op=ALU.mult), reads=rk, writes=[tmp])
                    S.op(DVE, lambda e: e.tensor_tensor(out=tv[:, 1], in0=x2, in1=Sn, op=ALU.mult), reads=rk, writes=[tmp])
                    S.op(DVE, lambda e: e.tensor_tensor(out=o[:, :, 0, :], in0=tv[:, 0], in1=tv[:, 1], op=ALU.subtract),
                         reads=[tmp], writes=[ob])
                    S.op(DVE, lambda e: e.tensor_tensor(out=tv[:, 0], in0=x1, in1=Sn, op=ALU.mult), reads=rk + [ob], writes=[tmp])
                    S.op(DVE, lambda e: e.tensor_tensor(out=tv[:, 1], in0=x2, in1=C, op=ALU.mult), reads=rk, writes=[tmp])
                    S.op(DVE, lambda e: e.tensor_tensor(out=o[:, :, 1, :], in0=tv[:, 0], in1=tv[:, 1], op=ALU.add),
                         reads=[tmp], writes=[ob])
                    if which == 1:
                        kd = L.ktm[:tp, t, :]
                        S.op(ACT, lambda e: e.copy(out=kd, in_=ob[:tp, :]), reads=[ob], writes=[kd])
                    dstT = L.qrT if which == 0 else L.krT
                    if not prefix:
                        deferred.append(lambda: transpose_to(
                            ob, tp, 512, lambda j0, nj: dstT[:, j0:j0 + nj, t * 128:t * 128 + tp]))
                return f
            if not prefix:
                linear(L, L.actT, KT, w_in, [[(O_RQ + hg * 512, 512)]], cons_rot(0), "rq%d" % hg)
            linear(L, L.actT, KT, w_in, [[(O_RK + hg * 512, 512)]], cons_rot(1), "rk%d" % hg)

            def cons_rv(ci, t, ps, pst):
                dst = L.vtm[:tp, t, ci * 512:(ci + 1) * 512]
                S.op(ACT, lambda e: e.copy(out=dst, in_=ps), reads=[pst], writes=[dst])
            linear(L, L.actT, KT, w_in, [[(O_RV + hg * 1024 + c * 512, 512)] for c in range(2)], cons_rv, "rv%d" % hg)

            def cons_rg(ci, t, ps, pst):
                dst = L.sgtm[:tp, t, ci * 512:(ci + 1) * 512]
                S.op(ACT, lambda e: e.activation(out=dst, in_=ps, func=AF.Silu), reads=[pst], writes=[dst])
            if not prefix:
                linear(L, L.actT, KT, w_in, [[(O_RG + hg * 1024 + c * 512, 512)] for c in range(2)], cons_rg, "rg%d" % hg)

            log_g = [float(np.log1p(-np.exp2(-5.0 - h))) for h in range(cfg.RH)]
            def ret_tile(t):
                ts = slice(t * 128, t * 128 + tp)
                sms = []
                for hl in range(4 if not prefix else 0):
                    ps_s = psL.next()
                    S.op(PE, lambda e, ps_s=ps_s, hl=hl, ts=ts: e.matmul(ps_s[:tp, 0:tp], lhsT=L.krT[:, hl, ts], rhs=L.qrT[:, hl, ts],
                                                               start=True, stop=True),
                         reads=[L.krT[:, hl, ts], L.qrT[:, hl, ts]], writes=[ps_s])
                    sm = L.sTm.next()
                    S.op(DVE, lambda e, ps_s=ps_s, sm=sm: e.tensor_tensor(out=sm[:tp, 0:tp], in0=ps_s[:tp, 0:tp],
                                                                           in1=caus[:tp, 0:tp], op=ALU.mult),
                         reads=[ps_s, caus], writes=[sm])
                    sms.append(sm)
                for hl in range(4):
                    h = 4 * hg + hl
                    ps_kv = psM[hl % 2]
                    vs_ = L.vtm[:tp, t, hl * 256:(hl + 1) * 256]
                    if not prefix:
                        sm = sms[hl]
                        ps_o = psL.next()

                    if not prefix:
                        def em_o(e, ps_o=ps_o, sm=sm, vs_=vs_, hl=hl, h=h, ts=ts):
                            e.matmul(ps_o[:tp, 0:256], lhsT=sm[:tp, 0:tp], rhs=vs_, start=True, stop=False)
                            return e.matmul(ps_o[:tp, 0:256], lhsT=L.qrT[:, hl, ts], rhs=state_b[:, h, :], start=False, stop=True)
                        S.op(PE, em_o, reads=[sm, vs_, L.qrT[:, hl, ts], "state_b%d" % h], writes=[ps_o])
                    kslice = L.ktm[:tp, t, hl * 128:(hl + 1) * 128]
                    S.op(PE, lambda e, ps_kv=ps_kv, kslice=kslice, vs_=vs_: e.matmul(
                        ps_kv[:, 0:256], lhsT=kslice, rhs=vs_, start=True, stop=True),
                        reads=[kslice, vs_], writes=[ps_kv])
                    if not prefix:
                        orw = L.o_raw[:tp, hl, :]
                        S.op(ACT, lambda e, orw=orw, ps_o=ps_o: e.copy(out=orw, in_=ps_o[:tp, 0:256]), reads=[ps_o], writes=[orw])
                    sf = state_f[:, h, :]
                    gC = float(np.exp(log_g[h] * tp))
                    S.op(DVE, lambda e, sf=sf, ps_kv=ps_kv: e.tensor_tensor(out=sf, in0=sf, in1=ps_kv[:, 0:256], op=ALU.add),
                         reads=[ps_kv, "state_f%d" % h], writes=["state_f%d" % h])
                    S.op(DVE, lambda e, sf=sf, gC=gC: e.tensor_scalar(out=sf, in0=sf, scalar1=gC, scalar2=None, op0=ALU.mult),
                         reads=["state_f%d" % h], writes=["state_f%d" % h])
                    S.op(ACT, lambda e, sf=sf, h=h: e.copy(out=state_b[:, h, :], in_=sf),
                         reads=["state_f%d" % h], writes=["state_b%d" % h])
                if prefix:
                    return
                ss, ssk = stat_cols(4)
                for hl in range(4):
                    orw = L.o_raw[:tp, hl, :]
                    S.op(DVE, lambda e, orw=orw, hl=hl: e.scalar_tensor_tensor(
                        out=L.og_tm[:tp, hl * 256:(hl + 1) * 256], in0=orw, scalar=1.0, in1=orw,
                        op0=ALU.mult, op1=ALU.mult, accum_out=ss[:tp, hl:hl + 1]),
                        reads=[orw], writes=[L.og_tm[:tp, :]] + ssk)
                rs, rsk = rstd_from_ss(ss[:tp], ssk, 4, tp, 1.0 / 256)
                Gv = L.G[:tp, :].rearrange("p (h e) -> p h e", h=4)
                S.op(DVE, lambda e: e.tensor_tensor(
                    out=Gv, in0=L.sgtm[:tp, t, :].rearrange("p (h e) -> p h e", h=4),
                    in1=gr_bc[:tp, :].unsqueeze(1).to_broadcast([tp, 4, 256]), op=ALU.mult),
                    reads=[L.sgtm[:tp, t, :], gr_bc], writes=[L.G[:tp, :]])
                orall = L.o_raw[:tp, :, :]
                S.op(DVE, lambda e: e.tensor_tensor(out=orall, in0=orall,
                                                    in1=rs[:tp].unsqueeze(2).to_broadcast([tp, 4, 256]), op=ALU.mult),
                     reads=[orall] + rsk, writes=[orall])
                S.op(DVE, lambda e: e.tensor_tensor(out=L.og_tm[:tp, :].rearrange("p (h e) -> p h e", h=4),
                                                    in0=orall, in1=Gv, op=ALU.mult),
                     reads=[orall, L.G[:tp, :]], writes=[L.og_tm[:tp, :]])
                transpose_to(L.og_tm, tp, 1024, lambda j0, nj, t=t: L.ogT[:, 8 * hg + j0:8 * hg + j0 + nj, t * 128:t * 128 + tp])

            for t in range(nt):
                ret_tile(t)

        def bisect_init(tp):
            T_ = lambda i: thr[:tp, i:i + 1]
            k_ = lambda i: "thr%d" % i
            S.op(DVE, lambda e: e.tensor_tensor(out=T_(2), in0=T_(0), in1=T_(1), op=ALU.subtract),
                 reads=[k_(0), k_(1)], writes=[k_(2)])
            S.op(DVE, lambda e: e.tensor_scalar(out=T_(2), in0=T_(2), scalar1=1.0001, scalar2=1e-20,
                                                op0=ALU.mult, op1=ALU.add), reads=[k_(2)], writes=[k_(2)])
            S.op(DVE, lambda e: e.tensor_scalar(out=w_all[:tp, :], in0=pow2[:tp, :], scalar1=T_(2), scalar2=None, op0=ALU.mult),
                 reads=[k_(2), pow2], writes=[w_all])
            S.op(DVE, lambda e: e.tensor_tensor(out=T_(5), in0=T_(1), in1=w_all[:tp, 1:2], op=ALU.add),
                 reads=[k_(1), w_all], writes=[k_(5)])

        def bisect_iters(tp, count_fn, K, i0, i1):
            T_ = lambda i: thr[:tp, i:i + 1]
            k_ = lambda i: "thr%d" % i
            for i in range(i0, i1):
                count_fn()
                S.op(DVE, lambda e: e.tensor_scalar(out=T_(7), in0=T_(6), scalar1=K - 0.5, scalar2=0.5,
                                                    op0=ALU.is_ge, op1=ALU.subtract),
                     reads=[k_(6)], writes=[k_(7)])
                S.op(DVE, lambda e, i=i: e.scalar_tensor_tensor(out=T_(5), in0=T_(7), scalar=w_all[:tp, i + 1:i + 2], in1=T_(5),
                                                                op0=ALU.mult, op1=ALU.add),
                     reads=[k_(7), k_(5), w_all], writes=[k_(5)])

        def bisect_finish(tp):
            n = cfg.NIT
            S.op(DVE, lambda e: e.tensor_tensor(out=thr[:tp, 3:4], in0=thr[:tp, 5:6], in1=w_all[:tp, n + 1:n + 2], op=ALU.subtract),
                 reads=["thr5", w_all], writes=["thr3"])

        def bisect(S_ap, tp, count_fn, K):
            bisect_init(tp)
            bisect_iters(tp, count_fn, K, 0, cfg.NIT)
            bisect_finish(tp)

        def dsa_index(L, tok0, t):
            tp = L.tp
            ts = slice(t * 128, t * 128 + tp)
            nk = tok0 + (t + 1) * 128
            for h in range(16):
                S.op(DVE, lambda e, h=h: e.tensor_scalar(out=L.diagW[:tp, h, 0:tp], in0=ident_b[:tp, 0:tp],
                                                         scalar1=wsc[:tp, t, h:h + 1], scalar2=None, op0=ALU.mult),
                     reads=[ident_b, wsc], writes=[L.diagW[:tp, h, 0:tp]])
            def idx_chunk(kc, c0):
                cw = min(512, nk - c0)
                S_ps = psM[kc % 2]
                pend = []
                for h in range(16):
                    r0 = (h % 2) * 64
                    Pps = psL.next()
                    lq = L.iqT[r0:r0 + 64, h // 2, ts]
                    rk_ = ikT2[r0:r0 + 64, c0:c0 + cw]
                    S.op(PE, lambda e, Pps=Pps, lq=lq, rk_=rk_: e.matmul(Pps[:tp, 0:cw], lhsT=lq, rhs=rk_, start=True, stop=True),
                         reads=[lq, ikT2], writes=[Pps])
                    Rb = L.Rring.next()
                    S.op(ACT, lambda e, Pps=Pps, Rb=Rb: e.activation(out=Rb[:tp, 0:cw], in_=Pps[:tp, 0:cw], func=AF.Relu),
                         reads=[Pps], writes=[Rb])

                    def dg(h=h, Rb=Rb, S_ps=S_ps, cw=cw):
                        S.op(PE, lambda e: e.matmul(S_ps[:tp, 0:cw], lhsT=L.diagW[:tp, h, 0:tp], rhs=Rb[:tp, 0:cw],
                                                    start=(h == 0), stop=(h == 15)),
                             reads=[L.diagW[:tp, h, 0:tp], Rb], writes=[S_ps])
                    pend.append(dg)
                    if len(pend) > 2:
                        pend.pop(0)()
                while pend:
                    pend.pop(0)()
                dstS = L.S_sb[:tp, c0:c0 + cw]
                S.op(ACT, lambda e, dstS=dstS, S_ps=S_ps: e.copy(out=dstS, in_=S_ps[:tp, 0:cw]), reads=[S_ps], writes=[dstS])

            for kc, c0 in enumerate(range(0, nk, 512)):
                idx_chunk(kc, c0)
            Sall = L.S_sb[:tp, 0:nk]
            S.op(DVE, lambda e: e.tensor_reduce(out=thr[:tp, 0:1], in_=Sall, axis=AX.X, op=ALU.max), reads=[Sall], writes=["thr0"])
            S.op(DVE, lambda e: e.tensor_reduce(out=thr[:tp, 1:2], in_=Sall, axis=AX.X, op=ALU.min), reads=[Sall], writes=["thr1"])
            pre_ = L.S_sb[:tp, 0:NPRE]
            S.op(DVE, lambda e: e.tensor_scalar(out=pre_, in0=pre_, scalar1=negflag_sb[:tp, 0:1], scalar2=None, op0=ALU.add),
                 reads=[pre_, negflag_sb], writes=[pre_])
            dg_ = L.S_sb[:tp, nk - 128:nk]
            S.op(DVE, lambda e: e.tensor_tensor(out=dg_, in0=dg_, in1=negu[:tp, :], op=ALU.add), reads=[dg_, negu], writes=[dg_])

            def count_fn():
                S.op(DVE, lambda e: e.tensor_scalar(out=L.junk[:tp, 0:nk], in0=Sall, scalar1=thr[:tp, 5:6], scalar2=0.0,
                                                    op0=ALU.is_ge, op1=ALU.add, accum_out=thr[:tp, 6:7]),
                     reads=[Sall, "thr5"], writes=[L.junk[:tp, 0:nk], "thr6"])
            bisect_init(tp)
            return count_fn, Sall

        def dsa_mask(L, tok0, t, Sall, mbuf):
            tp = L.tp
            nk = tok0 + (t + 1) * 128
            bisect_finish(tp)
            mbv = mbuf[:tp, 0:nk]
            S.op(DVE, lambda e: e.tensor_scalar(out=mbv, in0=Sall, scalar1=thr[:tp, 3:4], scalar2=NEG,
                                                op0=ALU.is_lt, op1=ALU.mult), reads=[Sall, "thr3"], writes=[mbv])

        def dsa_attend(L, tok0, t, g, mbuf):
            tp = L.tp
            ts = slice(t * 128, t * 128 + tp)
            nk = tok0 + (t + 1) * 128
            nkb = nk // 128
            identrep = ident_b[:tp, 0:tp].unsqueeze(1).to_broadcast([tp, 4, tp])
            def att_group(g):
                oT, den = psM[0], psM[1]
                pend = []
                qv = L.aqT[:, 4 * g:4 * g + 4, ts]
                for kb in range(nkb):
                    sps = psL.next()
                    spv = sps[:, 0:4 * tp].rearrange("p (h t) -> p h t", h=4)
                    kslice = kT[:, g, kb * 128:(kb + 1) * 128]
                    mslice = mbuf[:tp, kb * 128:(kb + 1) * 128]

                    def em_s(e, spv=spv, kslice=kslice, mslice=mslice):
                        e.matmul(spv, lhsT=kslice, rhs=qv, start=True, stop=False)
                        return e.matmul(spv, lhsT=mslice, rhs=identrep, start=False, stop=True)
                    S.op(PE, em_s, reads=[kT, qv, mslice, ident_b], writes=[sps])
                    pT_ = L.pTring.next()
                    S.op(ACT, lambda e, sps=sps, pT_=pT_: e.activation(out=pT_[:, 0:4 * tp], in_=sps[:, 0:4 * tp], func=AF.Exp),
                         reads=[sps], writes=[pT_])

                    def pv(kb=kb, pT_=pT_):
                        vsl = Vc[:, kb, g * 128:(g + 1) * 128]

                        def em(e):
                            e.matmul(oT[:, 0:4 * tp], lhsT=vsl, rhs=pT_[:, 0:4 * tp], start=(kb == 0), stop=(kb == nkb - 1))
                            return e.matmul(den[:, 0:4 * tp], lhsT=ones_b[:, :], rhs=pT_[:, 0:4 * tp],
                                            start=(kb == 0), stop=(kb == nkb - 1))
                        S.op(PE, em, reads=[Vc, pT_, ones_b], writes=[oT, den])
                    pend.append(pv)
                    if len(pend) > 1:
                        pend.pop(0)()
                while pend:
                    pend.pop(0)()
                S.op(DVE, lambda e: e.reciprocal(out=L.rden[:, 0:4 * tp], in_=den[:, 0:4 * tp]), reads=[den], writes=[L.rden])
                dsta = L.attT[:, 4 * g:4 * g + 4, ts]
                S.op(DVE, lambda e, dsta=dsta: e.tensor_tensor(
                    out=dsta, in0=oT[:, 0:4 * tp].rearrange("p (h t) -> p h t", h=4),
                    in1=L.rden[:, 0:4 * tp].rearrange("p (h t) -> p h t", h=4), op=ALU.mult),
                    reads=[oT, L.rden], writes=[dsta])

            att_group(g)

        def prompt_dsa_all(L, tok0):
            tp, nt = L.tp, L.nt
            mbufs = [L.mb, L.mb2]
            nsl = 4
            per = (cfg.NIT + nsl - 1) // nsl
            cur = dsa_index(L, tok0, 0)
            for t in range(nt):
                count_fn, Sall = cur
                for sl in range(nsl):
                    bisect_iters(tp, count_fn, cfg.TOPK, sl * per, min(cfg.NIT, (sl + 1) * per))
                    if t > 0:
                        dsa_attend(L, tok0, t - 1, sl, mbufs[(t - 1) % 2])
                dsa_mask(L, tok0, t, Sall, mbufs[t % 2])
                if t + 1 < nt:
                    cur = dsa_index(L, tok0, t + 1)
            for g in range(4):
                dsa_attend(L, tok0, nt - 1, g, mbufs[(nt - 1) % 2])

        def decode_dsa(L):
            D0 = 0
            kidx_g = AV(D0, 4096, F32)
            kidxTq = AV(4096, 4096)
            Rr = Ring([AV(8192 + 512 * i, 512) for i in range(4)])
            b = [10240]

            def al(n, dt=BF16):
                o = b[0]
                b[0] += (n * (2 if dt != BF16 else 1) + 63) // 64 * 64
                assert b[0] <= 20480
                return AV(o, n * (2 if dt != BF16 else 1), dt)
            Sg = al(130, F32)
            junkg = al(130)
            rsc = al(128, F32)
            dest = al(128, F32)
            mk = al(128, F32)
            Er = Ring([al(256, F32) for _ in range(3)])
            kg = al(1024, F32)
            vg = al(1024, F32)
            kgb = al(1024)
            vgb = al(1024)
            kgT = al(1024)
            pts = al(8, F32)
            idx4 = sb("idx4", [128, 1], I32)
            ptsb = sb("ptsb", [128, 1], I32)
            rowi = sb("rowi", [128, 2], I32)
            small = sb("dsm", [128, 64], F32)
            smallb = sb("dsmb", [128, 64], BF16)
            iota_d = sb("iota_d", [128, 256], F32)
            lstr = sb("lstr", [128, 128], BF16)
            jcol = sb("jcol", [128, 128], F32)
            dslot = sb("dslot", [128, 2], F32)
            rhs3 = sb("rhs3", [128, 128, 2], F32)
            cload(iota_d[:, :], c_iota)
            cload(jcol[:, :], c_jcol)
            cload(dslot[:, :], c_dslot)
            cload(mk[:, 0:128], c_lstr)
            S.op(DVE, lambda e: e.tensor_copy(out=lstr[:, :], in_=mk[:, 0:128]), reads=[mk], writes=[lstr])
            idx4b = sb("idx4b", [128, 1], I32)
            rowib = sb("rowib", [128, 2], I32)
            for tt_ in (idx4, idx4b, rowi, rowib):
                S.op(DVE, lambda e, tt_=tt_: e.memset(tt_[:, :], 0), writes=[tt_])
            cload(ptsb[:, :], ptab)
            S.op(DVE, lambda e: e.tensor_copy(out=pts[:, 0:1], in_=ptsb[:, :]), reads=[ptsb], writes=[pts])
            pw = psL.next()
            S.op(PE, lambda e: e.transpose(out=pw[0:16, 0:1], in_=L.iw_s[:1, 0:16], identity=ident_f[:1, :1]),
                 reads=[L.iw_s, ident_f], writes=[pw])
            S.op(ACT, lambda e: e.copy(out=L.wcol[0:16, 0:1], in_=pw[0:16, 0:1]), reads=[pw], writes=[L.wcol])
            px = psT.next()
            S.op(PE, lambda e: e.transpose(out=px[0:64, 0:1], in_=L.ik_sb[:1, 0:64], identity=ident_b[:1, :1]),
                 reads=[L.ik_sb, ident_b], writes=[px])
            S.op(ACT, lambda e: e.copy(out=L.ikTs[0:64, 0:1], in_=px[0:64, 0:1]), reads=[px], writes=[L.ikTs])
            psG = psM[0]
            for q4 in range(4):
                S.op(DVE, lambda e, q4=q4: e.tensor_scalar(out=idx4[:, :], in0=pts[:, 0:1], scalar1=4.0, scalar2=float(q4),
                                                           op0=ALU.mult, op1=ALU.add), reads=[pts], writes=[idx4])
                S.op(DVE, lambda e: e.tensor_copy(out=idx4b[:, :], in_=idx4[:, :]), reads=[idx4], writes=[idx4b])
                S.dma(POOL, lambda e: e.indirect_dma_start(
                    out=kidx_g[:, :], out_offset=None, in_=cache_ik[:, :],
                    in_offset=bass.IndirectOffsetOnAxis(ap=idx4b[:, :], axis=0),
                    bounds_check=cfg.NPHYS * 4 - 1, oob_is_err=False), reads=[idx4b], writes=[kidx_g])
                for jb in range(8):
                    pk = psL.next()

                    def em(e, pk=pk, jb=jb):
                        ins = None
                        for jj in range(4):
                            j = jb * 4 + jj
                            ins = e.transpose(out=pk[0:64, jj * 128:(jj + 1) * 128], in_=kidx_g[:, j * 64:(j + 1) * 64],
                                              identity=ident_f[:, :])
                        return ins
                    S.op(PE, em, reads=[kidx_g, ident_f], writes=[pk])
                    dstk = kidxTq[0:64, jb * 512:(jb + 1) * 512]
                    S.op(ACT, lambda e, pk=pk, dstk=dstk: e.copy(out=dstk, in_=pk[0:64, :]), reads=[pk], writes=[dstk])
                for c in range(8):
                    Pp = psL.next()
                    rk_ = kidxTq[0:64, c * 512:(c + 1) * 512]
                    S.op(PE, lambda e, Pp=Pp, rk_=rk_: e.matmul(Pp[0:16, :], lhsT=L.iqTs[0:64, 0:16], rhs=rk_, start=True, stop=True),
                         reads=[L.iqTs, rk_], writes=[Pp])
                    Rb = Rr.next()
                    S.op(ACT, lambda e, Pp=Pp, Rb=Rb: e.activation(out=Rb[0:16, :], in_=Pp[0:16, :], func=AF.Relu),
                         reads=[Pp], writes=[Rb])

                    def em2(e, Rb=Rb, c=c, q4=q4):
                        ins = None
                        for jj in range(4):
                            j = q4 * 32 + c * 4 + jj
                            ins = e.matmul(psG[:, j:j + 1], lhsT=Rb[0:16, jj * 128:(jj + 1) * 128], rhs=L.wcol[0:16, 0:1],
                                           start=True, stop=True)
                        return ins
                    S.op(PE, em2, reads=[Rb, L.wcol], writes=[psG])
            S.op(ACT, lambda e: e.copy(out=Sg[:, 0:128], in_=psG[:, 0:128]), reads=[psG], writes=[Sg])
            S.op(DVE, lambda e: e.memset(Sg[:, 128:130], -1e30), writes=[Sg])
            pself = psL.next()
            S.op(PE, lambda e: e.matmul(pself[0:16, 0:1], lhsT=L.iqTs[0:64, 0:16], rhs=L.ikTs[0:64, 0:1], start=True, stop=True),
                 reads=[L.iqTs, L.ikTs], writes=[pself])
            S.op(ACT, lambda e: e.activation(out=smallb[0:16, 0:1], in_=pself[0:16, 0:1], func=AF.Relu), reads=[pself], writes=[smallb])
            pself2 = psL.next()
            S.op(PE, lambda e: e.matmul(pself2[0:1, 0:1], lhsT=smallb[0:16, 0:1], rhs=L.wcol[0:16, 0:1], start=True, stop=True),
                 reads=[smallb, L.wcol], writes=[pself2])
            S.op(ACT, lambda e: e.copy(out=Sg[0:1, 128:129], in_=pself2[0:1, 0:1]), reads=[pself2], writes=[Sg])
            S.op(DVE, lambda e: e.tensor_reduce(out=small[:, 0:1], in_=Sg[:, 0:128], axis=AX.X, op=ALU.min), reads=[Sg], writes=[small])
            S.op(DVE, lambda e: e.tensor_reduce(out=small[:, 1:2], in_=Sg[:, 0:128], axis=AX.X, op=ALU.max, negate=True),
                 reads=[Sg], writes=[small])
            pmm = psL.next()
            S.op(PE, lambda e: e.transpose(out=pmm[0:2, 0:128], in_=small[:, 0:2], identity=ident_f[:, :]),
                 reads=[small, ident_f], writes=[pmm])
            S.op(DVE, lambda e: e.tensor_reduce(out=small[0:2, 2:3], in_=pmm[0:2, 0:128], axis=AX.X, op=ALU.min),
                 reads=[pmm], writes=[small])
            S.op(DVE, lambda e: e.tensor_scalar(out=small[0:2, 4:6], in0=ident_f[0:2, 0:2], scalar1=small[0:2, 2:3], scalar2=None,
                                                op0=ALU.mult), reads=[small, ident_f], writes=[small])
            pbc = psL.next()
            ones2 = sb("ones2", [2, 128], F32)
            S.op(DVE, lambda e: e.memset(ones2[:, :], 1.0), writes=[ones2])
            S.op(PE, lambda e: e.matmul(pbc[:, 0:2], lhsT=ones2[0:2, :], rhs=small[0:2, 4:6], start=True, stop=True),
                 reads=[ones2, small], writes=[pbc])
            S.op(DVE, lambda e: e.tensor_scalar(out=thr[:, 0:1], in0=pbc[:, 1:2], scalar1=-1.0, scalar2=None, op0=ALU.mult),
                 reads=[pbc], writes=["thr0"])
            S.op(DVE, lambda e: e.tensor_copy(out=thr[:, 1:2], in_=pbc[:, 0:1]), reads=[pbc], writes=["thr1"])

            def count_fn():
                S.op(DVE, lambda e: e.tensor_scalar(out=junkg[:, 0:129], in0=Sg[:, 0:129], scalar1=thr[:, 5:6], scalar2=0.0,
                                                    op0=ALU.is_ge, op1=ALU.add, accum_out=smallb[:, 2:3]),
                     reads=[Sg, "thr5"], writes=[junkg, smallb])
                pc = psL.next()
                S.op(PE, lambda e: e.matmul(pc[:, 0:1], lhsT=ones_b[:, :], rhs=smallb[:, 2:3], start=True, stop=True),
                     reads=[ones_b, smallb], writes=[pc])
                S.op(DVE, lambda e: e.tensor_copy(out=thr[:, 6:7], in_=pc[:, 0:1]), reads=[pc], writes=["thr6"])
            bisect(Sg, 128, count_fn, cfg.TOPK)
            S.op(DVE, lambda e: e.tensor_scalar(out=mk[:, 0:128], in0=Sg[:, 0:128], scalar1=thr[:, 3:4], scalar2=0.0,
                                                op0=ALU.is_ge, op1=ALU.add, accum_out=smallb[:, 4:5]),
                 reads=[Sg, "thr3"], writes=[mk, smallb])
            S.op(DVE, lambda e: e.tensor_tensor_scan(out=rsc[:, 0:128], data0=ones_f[:, :], data1=mk[:, 0:128], initial=0.0,
                                                     op0=ALU.mult, op1=ALU.add), reads=[mk, ones_f], writes=[rsc])
            poff = psL.next()

            def em_off(e):
                e.matmul(poff[:, 0:1], lhsT=lstr[:, :], rhs=smallb[:, 4:5], start=True, stop=True)
                return e.matmul(poff[:, 1:2], lhsT=ones_b[:, :], rhs=smallb[:, 4:5], start=True, stop=True)
            S.op(PE, em_off, reads=[lstr, ones_b, smallb], writes=[poff])
            S.op(DVE, lambda e: e.tensor_copy(out=small[:, 16:18], in_=poff[:, 0:2]), reads=[poff], writes=[small])
            S.op(DVE, lambda e: e.scalar_tensor_tensor(out=dest[:, 0:128], in0=rsc[:, 0:128], scalar=small[:, 16:17], in1=mk[:, 0:128],
                                                       op0=ALU.add, op1=ALU.mult), reads=[rsc, small, mk], writes=[dest])
            S.op(DVE, lambda e: e.tensor_scalar(out=dest[:, 0:128], in0=dest[:, 0:128], scalar1=-1.0, scalar2=None, op0=ALU.add),
                 reads=[dest], writes=[dest])
            S.op(DVE, lambda e: e.tensor_copy(out=rhs3[:, :, 0], in_=pts[:, 0:1].to_broadcast([128, 128])), reads=[pts], writes=[rhs3])
            S.op(DVE, lambda e: e.tensor_copy(out=rhs3[:, :, 1], in_=jcol[:, :]), reads=[jcol], writes=[rhs3])
            pslot = [psM[1], psL.next()]
            for j in range(128):
                E = Er.next()
                S.op(DVE, lambda e, E=E, j=j: e.tensor_scalar(out=E[:, 0:256], in0=iota_d[:, :], scalar1=dest[:, j:j + 1], scalar2=None,
                                                              op0=ALU.is_equal), reads=[iota_d, dest], writes=[E])

                def em3(e, E=E, j=j):
                    e.matmul(pslot[0][:, 0:2], lhsT=E[:, 0:128], rhs=rhs3[:, j, :], start=(j == 0), stop=(j == 127))
                    return e.matmul(pslot[1][:, 0:2], lhsT=E[:, 128:256], rhs=rhs3[:, j, :], start=(j == 0), stop=(j == 127))
                S.op(PE, em3, reads=[E, rhs3], writes=[pslot[0], pslot[1]])
            for tl in range(2):
                sl = small[:, 20 + 4 * tl:24 + 4 * tl]
                S.op(DVE, lambda e, sl=sl, tl=tl: e.tensor_copy(out=sl[:, 0:2], in_=pslot[tl][:, 0:2]), reads=[pslot[tl]], writes=[small])
                S.op(DVE, lambda e, sl=sl: e.scalar_tensor_tensor(out=sl[:, 3:4], in0=sl[:, 0:1], scalar=128.0, in1=sl[:, 1:2],
                                                                 op0=ALU.mult, op1=ALU.add), reads=[small], writes=[small])
                S.op(DVE, lambda e, sl=sl, tl=tl: e.tensor_copy(out=rowi[:, tl:tl + 1], in_=sl[:, 3:4]), reads=[small], writes=[rowi])
            S.op(DVE, lambda e: e.tensor_scalar(out=small[:, 30:32], in0=dslot[:, 0:2], scalar1=small[:, 17:18], scalar2=NEG,
                                                op0=ALU.is_ge, op1=ALU.mult), reads=[dslot, small], writes=[small])
            S.op(DVE, lambda e: e.tensor_scalar(out=small[0:1, 32:33], in0=Sg[0:1, 128:129], scalar1=thr[0:1, 3:4], scalar2=NEG,
                                                op0=ALU.is_lt, op1=ALU.mult), reads=[Sg, "thr3"], writes=[small])
            S.op(DVE, lambda e: e.tensor_copy(out=rowib[:, :], in_=rowi[:, :]), reads=[rowi], writes=[rowib])
            S.op(DVE, lambda e: e.memset(kg[:, :], 0.0), writes=[kg])
            S.op(DVE, lambda e: e.memset(vg[:, :], 0.0), writes=[vg])
            for tl in range(2):
                for (dstt, srcc) in ((kg, cache_k), (vg, cache_v)):
                    dv = dstt[:, tl * 512:(tl + 1) * 512]
                    S.dma(POOL, lambda e, dv=dv, srcc=srcc, tl=tl: e.indirect_dma_start(
                        out=dv, out_offset=None, in_=srcc[:, :],
                        in_offset=bass.IndirectOffsetOnAxis(ap=rowib[:, tl:tl + 1], axis=0),
                        bounds_check=cfg.NPHYS * 128 - 1, oob_is_err=False), reads=[rowib], writes=[dv])
            S.op(ACT, lambda e: e.copy(out=kgb[:, :], in_=kg[:, :]), reads=[kg], writes=[kgb])
            S.op(ACT, lambda e: e.copy(out=vgb[:, :], in_=vg[:, :]), reads=[vg], writes=[vgb])
            for tl in range(2):
                transpose_to(kgb[:, tl * 512:(tl + 1) * 512], 128, 512,
                             lambda j0, nj, tl=tl: kgT[:, tl * 512:(tl + 1) * 512].rearrange("p (g k) -> p g k", g=4)[:, j0:j0 + nj, :])
            pS = psL.next()
            for tl in range(2):
                def em4(e, tl=tl):
                    ins = None
                    for g in range(4):
                        ins = e.matmul(pS[:, tl * 16 + 4 * g:tl * 16 + 4 * g + 4],
                                       lhsT=kgT[:, tl * 512 + g * 128:tl * 512 + (g + 1) * 128],
                                       rhs=L.aqT[:, 4 * g:4 * g + 4, 0], start=True, stop=True)
                    return ins
                S.op(PE, em4, reads=[kgT, L.aqT], writes=[pS])
            pTd = smallb[:, 8:40]
            for tl in range(2):
                S.op(ACT, lambda e, tl=tl: e.activation(out=smallb[:, 8 + 16 * tl:24 + 16 * tl], in_=pS[:, 16 * tl:16 * tl + 16],
                                                        func=AF.Exp, bias=small[:, 30 + tl:31 + tl], scale=1.0),
                     reads=[pS, small], writes=[smallb])
            pss_ = psL.next()

            def em5(e):
                ins = None
                for g in range(4):
                    ins = e.matmul(pss_[0:1, 4 * g:4 * g + 4], lhsT=L.akTs[:, g:g + 1], rhs=L.aqT[:, 4 * g:4 * g + 4, 0],
                                   start=True, stop=True)
                return ins
            S.op(PE, em5, reads=[L.akTs, L.aqT], writes=[pss_])
            S.op(ACT, lambda e: e.activation(out=smallb[0:1, 40:56], in_=pss_[0:1, 0:16], func=AF.Exp,
                                             bias=small[0:1, 32:33], scale=1.0), reads=[pss_, small], writes=[smallb])
            po, pd = psM[0], psM[1]

            def em6(e):
                ins = None
                for g in range(4):
                    for tl in range(2):
                        e.matmul(po[:, 4 * g:4 * g + 4], lhsT=vgb[:, tl * 512 + g * 128:tl * 512 + (g + 1) * 128],
                                 rhs=smallb[:, 8 + 16 * tl + 4 * g:8 + 16 * tl + 4 * g + 4], start=(tl == 0), stop=False)
                    ins = e.matmul(po[:, 4 * g:4 * g + 4], lhsT=L.v_sb[0:1, g * 128:(g + 1) * 128],
                                   rhs=smallb[0:1, 40 + 4 * g:44 + 4 * g], start=False, stop=True)
                for tl in range(2):
                    e.matmul(pd[:, 0:16], lhsT=ones_b[:, :], rhs=smallb[:, 8 + 16 * tl:24 + 16 * tl], start=(tl == 0), stop=False)
                ins = e.matmul(pd[:, 0:16], lhsT=ones_b[0:1, :], rhs=smallb[0:1, 40:56], start=False, stop=True)
                return ins
            S.op(PE, em6, reads=[vgb, smallb, L.v_sb, ones_b], writes=[po, pd])
            S.op(DVE, lambda e: e.reciprocal(out=small[:, 40:56], in_=pd[:, 0:16]), reads=[pd], writes=[small])
            S.op(DVE, lambda e: e.tensor_tensor(out=L.attT[:, :, 0], in0=po[:, 0:16], in1=small[:, 40:56], op=ALU.mult),
                 reads=[po, small], writes=[L.attT[:, :, 0]])

        ones_f = sb("ones_f", [128, 128], F32)
        S.op(DVE, lambda e: e.memset(ones_f[:, :], 1.0), writes=[ones_f])

        def ones_f_ap():
            return ones_f[:, :]

        def run_block(src, dst, tok0, row0, T, is_sample, prefix=False):
            L = layout(T)
            tp, nt = L.tp, L.nt
            S.new_epoch()
            if is_sample:
                S.dma(SP, lambda e: e.dma_start(out=rs_p.rearrange("h d e -> d h e"), in_=state_f[:, :, :]), reads=SFK)
                S.dma(SP, lambda e: e.dma_start(out=state_f[:, :, :], in_=st_s.rearrange("h d e -> d h e")),
                      writes=SFK)
                for h in range(cfg.RH):
                    S.op(ACT, lambda e, h=h: e.copy(out=state_b[:, h, :], in_=state_f[:, h, :]),
                         reads=["state_f%d" % h], writes=["state_b%d" % h])
            for t in range(nt):
                xt = L.x_tm[:tp, t, :]
                S.dma(SP, lambda e, t=t, xt=xt: e.dma_start(out=xt, in_=src[row0 + t * 128:row0 + t * 128 + tp, :]), writes=[xt])
            ffn(L, g_f1, w_f1a, w_f1b, "f1")
            mixer(L, tok0, row0, is_sample, prefix)
            if prefix:
                return
            ffn(L, g_f2, w_f2a, w_f2b, "f2")
            for t in range(nt):
                xt = L.x_tm[:tp, t, :]
                S.dma(SP, lambda e, t=t, xt=xt: e.dma_start(out=dst[row0 + t * 128:row0 + t * 128 + tp, :], in_=xt), reads=[xt])

        for pb in range(NPRE // TB):
            run_block(xpre, None, pb * TB, pb * TB, TB, False, prefix=True)
        sfv = state_f[:, :, :].rearrange("p h e -> p (h e)")
        S.op(DVE, lambda e: e.tensor_scalar(out=sfv, in0=sfv, scalar1=flag_sb[:, 0:1], scalar2=None, op0=ALU.mult),
             reads=SFK + [flag_sb], writes=SFK)
        for h in range(cfg.RH):
            S.op(ACT, lambda e, h=h: e.copy(out=state_b[:, h, :], in_=state_f[:, h, :]),
                 reads=["state_f%d" % h], writes=["state_b%d" % h])
        for b in range(NOWN // TB):
            run_block(xo, y_p, NPRE + b * TB, b * TB, TB, False)
        if cfg.WITH_SAMPLE:
            run_block(xs, y_s, 0, 0, 1, True)
        else:
            S.dma(SP, lambda e: e.dma_start(out=rs_p.rearrange("h d e -> d h e"), in_=state_f[:, :, :]), reads=SFK)
        S.finish()
        S.emit(nc)
    return nc


def _consts(cfg, hf):
    SEQ = cfg.SEQ
    half = 64
    inv = (10000.0 ** (-np.arange(half, dtype=np.float32) / half)).astype(np.float32)
    pos = np.concatenate([np.arange(SEQ // 2), hf * (SEQ // 2) + np.arange(SEQ // 2), [cfg.NPAGES * 128]]).astype(np.float32)
    ang = pos[:, None] * inv[None, :]
    cos, sin = np.cos(ang).astype(np.float32), np.sin(ang).astype(np.float32)
    log_g = np.log1p(-np.exp2(-5.0 - np.arange(cfg.RH, dtype=np.float32))).astype(np.float32)
    i_in = np.concatenate([np.arange(SEQ) % 128, [0]]).astype(np.float32)
    dq = np.exp(log_g[None, :] * (i_in[:, None] + 1.0)).astype(np.float32)
    dk = (np.exp(-log_g[None, :] * (i_in[:, None] + 1.0)) * (128 ** -0.5)).astype(np.float32)
    rope = np.stack([cos[:, None, :] * dq[:, :, None], sin[:, None, :] * dq[:, :, None],
                     cos[:, None, :] * dk[:, :, None], sin[:, None, :] * dk[:, :, None]], axis=1)
    j = np.arange(128)
    return {
        "c_rope": np.ascontiguousarray(rope.astype(np.float32)),
        "c_ident": np.eye(128, dtype=np.float32),
        "c_caus": (j[:, None] <= j[None, :]).astype(np.float32),
        "c_negu": np.where(j[None, :] > j[:, None], -1e30, 0.0).astype(np.float32),
        "c_iota": np.broadcast_to(np.arange(256, dtype=np.float32)[None, :], (128, 256)).copy(),
        "c_lstr": (j[:, None] < j[None, :]).astype(np.float32),
        "c_jcol": np.broadcast_to(j.astype(np.float32)[None, :], (128, 128)).copy(),
        "c_dslot": np.stack([j, j + 128], axis=1).astype(np.float32),
        "c_pow2": np.broadcast_to((2.0 ** -np.arange(32, dtype=np.float32))[None, :], (128, 32)).copy(),
    }


def kernel(x_prompt, x_sample, state_ret, cache_k, cache_v, cache_idx_k, page_table,
           ffn1_norm, ffn1_w1, ffn1_w2, mix_norm, w_in, q_norm, k_norm, ret_norm,
           w_ret_out, w_att_out, w_o, ffn2_norm, ffn2_w1, ffn2_w2, _cfg=None):
    cfg = _cfg or Cfg
    f = lambda a: np.ascontiguousarray(np.asarray(a))
    nc = build_program(cfg)
    consts = [_consts(cfg, 0), _consts(cfg, 1)]
    HS = cfg.SEQ // 2
    B = x_prompt.shape[0]
    NCO = cfg.NCORES
    shared = {
        "ffn1_w1": f(ffn1_w1[0]), "ffn1_w2": f(ffn1_w2[0]), "ffn2_w1": f(ffn2_w1[0]), "ffn2_w2": f(ffn2_w2[0]),
        "w_in": f(w_in[0]), "w_ret_out": f(w_ret_out[0]), "w_att_out": f(w_att_out[0]), "w_o": f(w_o[0]),
        "ffn1_norm": f(ffn1_norm), "mix_norm": f(mix_norm), "ffn2_norm": f(ffn2_norm),
        "q_norm": f(q_norm), "k_norm": f(k_norm), "ret_norm": f(ret_norm),
    }
    if cfg.WITH_SAMPLE:
        shared["cache_k"] = f(cache_k[0]).reshape(cfg.NPHYS * 128, 512)
        shared["cache_v"] = f(cache_v[0]).reshape(cfg.NPHYS * 128, 512)
        shared["cache_ik"] = f(cache_idx_k[0]).reshape(cfg.NPHYS * 4, 2048)
    in_maps = []
    for c in range(NCO):
        m = dict(shared)
        b_, hf = (c // 2) % B, c % 2
        m.update(consts[hf])
        m["xo"] = f(x_prompt[b_, hf * HS:(hf + 1) * HS])
        m["xpre"] = f(x_prompt[b_, 0:HS])
        m["flag"] = np.full((128, 1), float(hf), np.float32)
        m["negflag"] = np.full((128, 1), 0.0 if hf else -1e30, np.float32)
        m["xs"] = f(x_sample[c % 8])
        m["st_s"] = f(state_ret[0, c % 8])
        if cfg.WITH_SAMPLE:
            m["ptab"] = f(page_table[c % 8]).reshape(128, 1).astype(np.int32)
        in_maps.append(m)
    res = run_bass_kernel_spmd(nc, in_maps, core_ids=list(range(NCO)))
    r = res.results
    cs = lambda c: r[c % NCO]
    cat = lambda b, k: np.concatenate([cs(2 * b)[k], cs(2 * b + 1)[k]], axis=0)
    y_prompt = np.stack([cat(b, "y_p") for b in range(B)])
    y_sample = np.stack([cs(c)["y_s"] for c in range(8)])
    rsp = np.stack([cs(2 * b + 1)["rs_p"] for b in range(B)])[None]
    kp = np.stack([cat(b, "k_p") for b in range(B)]).reshape(1, B, cfg.SEQ, 4, 128)
    vp = np.stack([cat(b, "v_p") for b in range(B)]).reshape(1, B, cfg.SEQ, 4, 128)
    ikp = np.stack([cat(b, "ik_p") for b in range(B)])[None]
    rss = np.stack([cs(c)["rs_s"] for c in range(8)])[None]
    ks = np.stack([cs(c)["k_s"] for c in range(8)]).reshape(1, 8, 1, 4, 128)
    vs = np.stack([cs(c)["v_s"] for c in range(8)]).reshape(1, 8, 1, 4, 128)
    iks = np.stack([cs(c)["ik_s"] for c in range(8)]).reshape(1, 8, 1, 64)
    return (y_prompt, y_sample, rsp, kp, vp, ikp, rss, ks, vs, iks)
```
